# Optimizing a Trainium2 kernel written in Bass

```python
import math
import jax
import jax.numpy as jnp
from jax import lax
import numpy as np

D_MODEL = 1024
BATCH = 4
SEQ = 8192
DEPTH = 4

HEAD_DIM = 64
NSA_HEADS = 8
NSA_GROUPS = 2
CMP_LEN = 32
CMP_STRIDE = 16
CMP_HIDDEN = 256
SEL_BLOCK = 64
SEL_TOP_N = 16
NSA_WINDOW = 512
NSA_Q_BLOCK = 64
SWA_HEADS = 8
SWA_KV_HEADS = 2
SWA_WINDOW = 128
SWA_Q_BLOCK = 128
REL_BUCKETS = 32
REL_MAX_DIST = 128
N_HEADS_TOTAL = NSA_HEADS + SWA_HEADS
N_EXPERTS = 32
TOP_K = 4
D_EXPERT = 1024
SWIGLU_ALPHA = 1.702
SWIGLU_LIMIT = 7.0
MOE_BLOCK = 256
RMS_EPS = 1e-5

NSA_Q_W = NSA_HEADS * HEAD_DIM
NSA_KV_W = NSA_GROUPS * HEAD_DIM
SWA_Q_W = SWA_HEADS * HEAD_DIM
SWA_KV_W = SWA_KV_HEADS * HEAD_DIM
IN_SPLITS = (NSA_Q_W, 6 * NSA_KV_W, 3 * NSA_HEADS, SWA_Q_W, 2 * SWA_KV_W, 2 * D_MODEL)
D_IN = NSA_Q_W + 6 * NSA_KV_W + 3 * NSA_HEADS + SWA_Q_W + 2 * SWA_KV_W + 2 * D_MODEL

kernel_name = 'hybrid_nsa_swa_sink_moe_adaln'


def rms_norm(x, g):
    xf = x.astype(jnp.float32)
    y = xf * lax.rsqrt(jnp.mean(xf * xf, axis=-1, keepdims=True) + RMS_EPS)
    return (y * g.astype(jnp.float32)).astype(x.dtype)


def masked_softmax(s, mask):
    s = jnp.where(mask, s.astype(jnp.float32), -jnp.inf)
    m = jnp.max(s, axis=-1, keepdims=True)
    m = jnp.where(jnp.isfinite(m), m, 0.0)
    e = jnp.exp(s - m)
    d = jnp.sum(e, axis=-1, keepdims=True)
    return e / jnp.where(d > 0, d, 1.0)


def t5_bucket(dist):
    n = jnp.maximum(dist, 0)
    max_exact = REL_BUCKETS // 2
    log_ratio = jnp.log(jnp.maximum(n, 1).astype(jnp.float32) / max_exact) / math.log(REL_MAX_DIST / max_exact)
    large = max_exact + (log_ratio * (REL_BUCKETS - max_exact)).astype(jnp.int32)
    return jnp.where(n < max_exact, n, jnp.minimum(large, REL_BUCKETS - 1))


def compress(kv, pos, w1, w2):
    B, S, G, dh = kv.shape
    c = CMP_LEN // CMP_STRIDE
    n_chunks = S // CMP_STRIDE
    n_cmp = n_chunks - c + 1
    ch = kv.reshape(B, n_chunks, CMP_STRIDE, G, dh)
    blocks = jnp.concatenate([ch[:, r:r + n_cmp] for r in range(c)], axis=2)
    blocks = blocks + pos[:, None, :]
    flat = jnp.swapaxes(blocks, 2, 3).reshape(B, n_cmp, G, CMP_LEN * dh)
    return jax.nn.gelu(flat @ w1) @ w2


def nsa_attention(q, k_cmp, v_cmp, k_slc, v_slc, k_win, v_win, branch_gate, rel_tab):
    B, S, G, Hg, dh = q.shape
    n_cmp = k_cmp.shape[1]
    n_blk = S // SEL_BLOCK
    n_sel = min(SEL_TOP_N, n_blk)
    r = SEL_BLOCK // CMP_STRIDE
    c = CMP_LEN // CMP_STRIDE
    pad_front = r + c - 2
    scale = HEAD_DIM ** -0.5
    cmp_end = jnp.arange(n_cmp) * CMP_STRIDE + CMP_LEN - 1
    kb = k_slc.reshape(B, n_blk, SEL_BLOCK, G, dh).transpose(0, 3, 1, 2, 4)
    vb = v_slc.reshape(B, n_blk, SEL_BLOCK, G, dh).transpose(0, 3, 1, 2, 4)
    kw = jnp.pad(k_win, ((0, 0), (NSA_WINDOW, 0), (0, 0), (0, 0)))
    vw = jnp.pad(v_win, ((0, 0), (NSA_WINDOW, 0), (0, 0), (0, 0)))
    tab_g = rel_tab.transpose(1, 0, 2)
    bi = jnp.arange(B)[:, None, None, None]
    gi = jnp.arange(G)[None, None, :, None]
    blk_ids = jnp.arange(n_blk)
    n_win = NSA_WINDOW + NSA_Q_BLOCK

    def block(s0):
        t = s0 + jnp.arange(NSA_Q_BLOCK)
        qb = lax.dynamic_slice_in_dim(q, s0, NSA_Q_BLOCK, axis=1)
        s_c = jnp.einsum('bqghd,bngd->bqghn', qb, k_cmp) * scale
        p_c = masked_softmax(s_c, (cmp_end[None, :] <= t[:, None])[None, :, None, None, :])
        o_c = jnp.einsum('bqghn,bngd->bqghd', p_c.astype(v_cmp.dtype), v_cmp)
        imp = jnp.pad(jnp.sum(p_c, axis=3), ((0, 0), (0, 0), (0, 0), (pad_front, r)))
        p_slc = sum(imp[..., pad_front - m - n: pad_front - m - n + r * n_blk: r]
                    for m in range(r) for n in range(c))
        blk_t = (t // SEL_BLOCK)[:, None]
        future = blk_ids[None, :] > blk_t
        forced = (blk_ids[None, :] == 0) | (blk_ids[None, :] == blk_t) | (blk_ids[None, :] == blk_t - 1)
        score = jnp.where(future[None, :, None, :], -jnp.inf,
                          jnp.where(forced[None, :, None, :], jnp.inf, p_slc))
        _, idx = lax.top_k(score, n_sel)
        ks = kb[bi, gi, idx]
        vs = vb[bi, gi, idx]
        kpos = idx[..., None] * SEL_BLOCK + jnp.arange(SEL_BLOCK)
        dist = t[None, :, None, None, None] - kpos
        bias = tab_g[gi[..., None], t5_bucket(dist)]
        s_s = jnp.einsum('bqghd,bqgnkd->bqghnk', qb, ks) * scale + jnp.moveaxis(bias, -1, 3)
        s_s = s_s.reshape(B, NSA_Q_BLOCK, G, Hg, n_sel * SEL_BLOCK)
        m_s = (dist >= 0).reshape(B, NSA_Q_BLOCK, G, 1, n_sel * SEL_BLOCK)
        p_s = masked_softmax(s_s, m_s)
        o_s = jnp.einsum('bqghm,bqgmd->bqghd', p_s.astype(vs.dtype),
                         vs.reshape(B, NSA_Q_BLOCK, G, n_sel * SEL_BLOCK, dh))
        kwb = lax.dynamic_slice_in_dim(kw, s0, n_win, axis=1)
        vwb = lax.dynamic_slice_in_dim(vw, s0, n_win, axis=1)
        kpos_w = s0 - NSA_WINDOW + jnp.arange(n_win)
        dist_w = t[:, None] - kpos_w[None, :]
        mask_w = (kpos_w[None, :] >= 0) & (dist_w >= 0) & (dist_w < NSA_WINDOW)
        bias_w = rel_tab[t5_bucket(dist_w)].transpose(0, 2, 3, 1)
        s_w = jnp.einsum('bqghd,bkgd->bqghk', qb, kwb) * scale + bias_w[None]
        p_w = masked_softmax(s_w, mask_w[None, :, None, None, :])
        o_w = jnp.einsum('bqghk,bkgd->bqghd', p_w.astype(vwb.dtype), vwb)
        return jnp.stack([o_c, o_s, o_w], axis=2)

    starts = jnp.arange(S // NSA_Q_BLOCK, dtype=jnp.int32) * NSA_Q_BLOCK
    o = lax.map(block, starts)
    o = jnp.moveaxis(o, 0, 1).reshape(B, S, 3, G, Hg, dh)
    o = jnp.sum(branch_gate[..., None] * o, axis=2)
    return o.reshape(B, S, G * Hg * dh)


def swa_attention(q, k, v, sinks, rel_tab):
    B, S, KV, Hg, dh = q.shape
    scale = HEAD_DIM ** -0.5
    n_keys = SWA_WINDOW + SWA_Q_BLOCK
    kp = jnp.pad(k, ((0, 0), (SWA_WINDOW, 0), (0, 0), (0, 0)))
    vp = jnp.pad(v, ((0, 0), (SWA_WINDOW, 0), (0, 0), (0, 0)))

    def block(s0):
        t = s0 + jnp.arange(SWA_Q_BLOCK)
        qb = lax.dynamic_slice_in_dim(q, s0, SWA_Q_BLOCK, axis=1)
        kb = lax.dynamic_slice_in_dim(kp, s0, n_keys, axis=1)
        vb = lax.dynamic_slice_in_dim(vp, s0, n_keys, axis=1)
        kpos = s0 - SWA_WINDOW + jnp.arange(n_keys)
        dist = t[:, None] - kpos[None, :]
        mask = (kpos[None, :] >= 0) & (dist >= 0) & (dist < SWA_WINDOW)
        bias = rel_tab[t5_bucket(dist)].transpose(0, 2, 3, 1)
        s = jnp.einsum('bqghd,bkgd->bqghk', qb, kb) * scale + bias[None]
        sink_col = jnp.broadcast_to(sinks[None, None, :, :, None].astype(s.dtype), s.shape[:-1] + (1,))
        s = jnp.concatenate([s, sink_col], axis=-1)
        mask = jnp.concatenate([mask[None, :, None, None, :],
                                jnp.ones((1, SWA_Q_BLOCK, 1, 1, 1), dtype=bool)], axis=-1)
        p = masked_softmax(s, mask)[..., :-1]
        return jnp.einsum('bqghk,bkgd->bqghd', p.astype(vb.dtype), vb)

    starts = jnp.arange(S // SWA_Q_BLOCK, dtype=jnp.int32) * SWA_Q_BLOCK
    o = lax.map(block, starts)
    return jnp.moveaxis(o, 0, 1).reshape(B, S, KV * Hg * dh)


def clamped_swiglu(gu):
    x_glu, x_lin = gu[..., ::2], gu[..., 1::2]
    x_glu = jnp.minimum(x_glu, SWIGLU_LIMIT)
    x_lin = jnp.clip(x_lin, -SWIGLU_LIMIT, SWIGLU_LIMIT)
    return x_glu * jax.nn.sigmoid(SWIGLU_ALPHA * x_glu) * (x_lin + 1.0)


def moe_ffn(h, w_router, b_router, w_up, b_up, w_down, b_down):
    B, S, D = h.shape
    T = B * S
    E = w_up.shape[0]
    ht = h.reshape(T, D)
    logits = (ht @ w_router + b_router).astype(jnp.float32)
    top_val, top_idx = lax.top_k(logits, TOP_K)
    gate = jax.nn.softmax(top_val, axis=-1)
    A = T * TOP_K
    e_flat = top_idx.reshape(A)
    order = jnp.argsort(e_flat)
    e_sorted = e_flat[order]
    tok_sorted = (order // TOP_K).astype(jnp.int32)
    gate_sorted = gate.reshape(A)[order].astype(h.dtype)
    counts = jnp.zeros((E,), jnp.int32).at[e_flat].add(1)
    starts = jnp.cumsum(counts) - counts
    padded = (counts + MOE_BLOCK - 1) // MOE_BLOCK * MOE_BLOCK
    pad_end = jnp.cumsum(padded)
    dest = pad_end[e_sorted] - padded[e_sorted] + jnp.arange(A, dtype=jnp.int32) - starts[e_sorted]
    n_blocks = -(-A // MOE_BLOCK) + E
    slot_tok = jnp.zeros((n_blocks * MOE_BLOCK,), jnp.int32).at[dest].set(tok_sorted)
    slot_gate = jnp.zeros((n_blocks * MOE_BLOCK,), h.dtype).at[dest].set(gate_sorted)
    block_expert = jnp.minimum(
        jnp.searchsorted(pad_end, jnp.arange(n_blocks, dtype=jnp.int32) * MOE_BLOCK, side='right'),
        E - 1).astype(jnp.int32)

    def body(y, blk):
        tok, g, e = blk
        xb = ht[tok]
        hid = clamped_swiglu(xb @ w_up[e] + b_up[e])
        out = hid @ w_down[e] + b_down[e]
        return y.at[tok].add(out * g[:, None]), None

    y, _ = lax.scan(body, jnp.zeros_like(ht),
                    (slot_tok.reshape(n_blocks, MOE_BLOCK), slot_gate.reshape(n_blocks, MOE_BLOCK), block_expert))
    return y.reshape(B, S, D)


def setup_inputs(seed: int = 0) -> dict:
    key = jax.random.key(seed)
    ks = jax.random.split(key, 24)
    f32 = jnp.float32

    def nrm(k, shape, scale):
        return jax.random.normal(k, shape, f32) * scale

    L = DEPTH
    E = N_EXPERTS
    return {
        'x': nrm(ks[0], (BATCH, SEQ, D_MODEL), 1.0),
        'c': nrm(ks[1], (BATCH, D_MODEL), 1.0),
        'w_ada': nrm(ks[2], (L, D_MODEL, 6 * D_MODEL), 0.5 * D_MODEL ** -0.5),
        'b_ada': nrm(ks[3], (L, 6 * D_MODEL), 0.02),
        'g_mix': 1.0 + nrm(ks[4], (L, D_MODEL), 0.05),
        'g_ffn': 1.0 + nrm(ks[5], (L, D_MODEL), 0.05),
        'g_final': 1.0 + nrm(ks[6], (D_MODEL,), 0.05),
        'w_in': nrm(ks[7], (L, D_MODEL, D_IN), D_MODEL ** -0.5),
        'cmp_pos': nrm(ks[8], (L, 2, CMP_LEN, HEAD_DIM), 0.1),
        'cmp_w1': nrm(ks[9], (L, 2, CMP_LEN * HEAD_DIM, CMP_HIDDEN), (CMP_LEN * HEAD_DIM) ** -0.5),
        'cmp_w2': nrm(ks[10], (L, 2, CMP_HIDDEN, HEAD_DIM), CMP_HIDDEN ** -0.5),
        'sinks': nrm(ks[11], (L, SWA_HEADS), 0.5),
        'rel_tab': nrm(ks[12], (REL_BUCKETS, N_HEADS_TOTAL), 0.2),
        'w_branch': nrm(ks[13], (L, 2, NSA_Q_W, D_MODEL), NSA_Q_W ** -0.5),
        'w_out': nrm(ks[14], (L, D_MODEL, D_MODEL), D_MODEL ** -0.5),
        'w_router': nrm(ks[15], (L, D_MODEL, E), D_MODEL ** -0.5),
        'b_router': nrm(ks[16], (L, E), 0.01),
        'w_up': nrm(ks[17], (L, E, D_MODEL, 2 * D_EXPERT), D_MODEL ** -0.5),
        'b_up': nrm(ks[18], (L, E, 2 * D_EXPERT), 0.01),
        'w_down': nrm(ks[19], (L, E, D_EXPERT, D_MODEL), D_EXPERT ** -0.5),
        'b_down': nrm(ks[20], (L, E, D_MODEL), 0.01),
    }


def reference(x, c, w_ada, b_ada, g_mix, g_ffn, g_final, w_in, cmp_pos, cmp_w1, cmp_w2, sinks, rel_tab,
              w_branch, w_out, w_router, b_router, w_up, b_up, w_down, b_down):
    B, S, D = x.shape
    G, Hg = NSA_GROUPS, NSA_HEADS // NSA_GROUPS
    KV, Hs = SWA_KV_HEADS, SWA_HEADS // SWA_KV_HEADS
    nsa_tab = rel_tab[:, :NSA_HEADS].reshape(REL_BUCKETS, G, Hg)
    swa_tab = rel_tab[:, NSA_HEADS:].reshape(REL_BUCKETS, KV, Hs)
    cond = jax.nn.silu(c)
    offs = [int(v) for v in np.cumsum(IN_SPLITS)[:-1]]
    for l in range(DEPTH):
        mod = (cond @ w_ada[l] + b_ada[l])[:, None, :]
        sh1, sc1, ga1, sh2, sc2, ga2 = jnp.split(mod, 6, axis=-1)
        h = rms_norm(x, g_mix[l]) * (1.0 + sc1) + sh1
        z = h @ w_in[l]
        q_n, kv_n, gate_n, q_s, kv_s, gate_m = jnp.split(z, offs, axis=-1)
        k_c, v_c, k_sl, v_sl, k_w, v_w = [a.reshape(B, S, G, HEAD_DIM) for a in jnp.split(kv_n, 6, axis=-1)]
        k_c = compress(k_c, cmp_pos[l, 0], cmp_w1[l, 0], cmp_w2[l, 0])
        v_c = compress(v_c, cmp_pos[l, 1], cmp_w1[l, 1], cmp_w2[l, 1])
        o_n = nsa_attention(q_n.reshape(B, S, G, Hg, HEAD_DIM), k_c, v_c, k_sl, v_sl, k_w, v_w,
                            jax.nn.sigmoid(gate_n).reshape(B, S, 3, G, Hg), nsa_tab)
        k_s, v_s = jnp.split(kv_s, 2, axis=-1)
        o_s = swa_attention(q_s.reshape(B, S, KV, Hs, HEAD_DIM), k_s.reshape(B, S, KV, HEAD_DIM),
                            v_s.reshape(B, S, KV, HEAD_DIM), sinks[l].reshape(KV, Hs), swa_tab)
        g_a, g_b = jnp.split(jax.nn.sigmoid(gate_m), 2, axis=-1)
        merged = g_a * (o_n @ w_branch[l, 0]) + g_b * (o_s @ w_branch[l, 1])
        x = x + ga1 * (merged @ w_out[l])
        h = rms_norm(x, g_ffn[l]) * (1.0 + sc2) + sh2
        x = x + ga2 * moe_ffn(h, w_router[l], b_router[l], w_up[l], b_up[l], w_down[l], b_down[l])
    return rms_norm(x, g_final)
```

```python
import math
import numpy as np
import concourse.bass as bass
import concourse.mybir as mybir
from concourse.bass_utils import run_bass_kernel_spmd
from contextlib import ExitStack

F32 = mybir.dt.float32
BF16 = mybir.dt.bfloat16
AF = mybir.ActivationFunctionType
ALU = mybir.AluOpType

D = 1024
DEPTH = 4
NEXP = 32
NEG = -30000.0
FM_COLS = 3744
TM_COLS = 384
RMS_EPS = 1e-5


class Buf:
    def __init__(self, name, t):
        self.name = name
        self.t = t
        self.regs = {}
        self.dsem = None

    def __getitem__(self, idx):
        return self.t[idx]


class KB:
    def __init__(self, nc, es):
        self.nc = nc
        self.es = es
        self.engs = {"pe": nc.tensor, "dve": nc.vector, "act": nc.scalar, "pool": nc.gpsimd, "sp": nc.sync}
        self.esem = {}
        self.cnt = {}
        self.seen = {}
        for e in self.engs:
            self.esem[e] = es.enter_context(nc.semaphore("es_" + e))
            self.cnt[e] = 0
            self.seen[e] = {}
        self.dtot = {}
        self.dsems = []
        self.semcache = {}
        self.uid = 0
        self.nsem = 5
        self.ninst = 0

    def sb(self, es, name, shape, dt):
        self.uid += 1
        return Buf(name, es.enter_context(self.nc.sbuf_tensor("%s_u%d" % (name, self.uid), list(shape), dt)))

    def ps(self, es, name, shape, dt=F32):
        return Buf(name, es.enter_context(self.nc.psum_tensor(name, list(shape), dt)))

    dbg = False

    def dram(self, name, shape, dt, kind="Internal"):
        if kind == "Internal" and self.dbg:
            kind = "ExternalOutput"
        return Buf(name, self.nc.dram_tensor(name, list(shape), dt, kind=kind).ap())

    def _collect(self, b, key, is_write, waits):
        regs = b.regs
        keys = list(regs.keys()) if key == "*" else [k for k in (key, "*") if k in regs]
        for k in keys:
            r = regs[k]
            if r["w"] is not None:
                self._addwait(r["w"], waits)
            if is_write:
                for sem, val in r["r"].items():
                    self._addwait((sem, val), waits)

    def _addwait(self, ev, waits):
        sem, val = ev
        if id(sem) in self.dtot:
            val = self.dtot[id(sem)]
        k = id(sem)
        if k not in waits or waits[k][1] < val:
            waits[k] = (sem, val)

    def _mark(self, b, key, is_write, ev):
        regs = b.regs
        if is_write:
            if key == "*":
                regs.clear()
            regs[key] = {"w": ev, "r": {}}
        else:
            r = regs.setdefault(key, {"w": None, "r": {}})
            r["r"][ev[0]] = ev[1]

    @staticmethod
    def _norm(lst):
        out = []
        for x in lst:
            if isinstance(x, tuple):
                out.append(x)
            else:
                out.append((x, "*"))
        return out

    def _emit_waits(self, eng, reads, writes):
        waits = {}
        for b, key in reads:
            self._collect(b, key, False, waits)
        for b, key in writes:
            self._collect(b, key, True, waits)
        E = self.engs[eng]
        own = id(self.esem[eng])
        for k, (sem, val) in waits.items():
            if k == own and eng == "pe":
                continue
            if self.seen[eng].get(k, 0) >= val:
                continue
            E.wait_ge(sem, val)
            self.seen[eng][k] = val

    def op(self, eng, fn, reads=(), writes=()):
        reads = self._norm(reads)
        writes = self._norm(writes)
        self._emit_waits(eng, reads, writes)
        inst = fn(self.engs[eng])
        self.cnt[eng] += 1
        self.ninst += 1
        inst.then_inc(self.esem[eng], 1)
        ev = (self.esem[eng], self.cnt[eng])
        for b, key in reads:
            self._mark(b, key, False, ev)
        for b, key in writes:
            self._mark(b, key, True, ev)

    def dma(self, q, out_ap, in_ap, reads, writes, owner, **kw):
        reads = self._norm(reads)
        writes = self._norm(writes)
        self._emit_waits(q, reads, writes)
        if owner.dsem is None:
            if owner.name in self.semcache:
                owner.dsem = self.semcache[owner.name]
            else:
                owner.dsem = self.es.enter_context(self.nc.semaphore("ds_" + owner.name))
                self.semcache[owner.name] = owner.dsem
                self.dtot[id(owner.dsem)] = 0
                self.dsems.append(owner.dsem)
                self.nsem += 1
        inst = self.engs[q].dma_start(out=out_ap, in_=in_ap, **kw)
        inst.then_inc(owner.dsem, 16)
        self.ninst += 1
        self.dtot[id(owner.dsem)] += 16
        ev = (owner.dsem, self.dtot[id(owner.dsem)])
        for b, key in reads:
            self._mark(b, key, False, ev)
        for b, key in writes:
            self._mark(b, key, True, ev)

    def load(self, dst, dst_ap, src, src_ap, skey="*", dkey="*", q="sp", **kw):
        self.dma(q, dst_ap, src_ap, [(src, skey)], [(dst, dkey)], dst, **kw)

    def loadc(self, dst, dst_ap, src, src_ap, skey="*", dkey="*", **kw):
        kw.setdefault("max_dma_last_dim", 4096)
        self.dma("pool", dst_ap, src_ap, [(src, skey)], [(dst, dkey)], dst, **kw)

    def store(self, dst, dst_ap, src, src_ap, skey="*", dkey="*", q="sp", **kw):
        self.dma(q, dst_ap, src_ap, [(src, skey)], [(dst, dkey)], src, **kw)

    def barrier(self):
        for eng, E in self.engs.items():
            for e2, sem in self.esem.items():
                if e2 == eng or self.cnt[e2] == 0:
                    continue
                if self.seen[eng].get(id(sem), 0) >= self.cnt[e2]:
                    continue
                E.wait_ge(sem, self.cnt[e2])
                self.seen[eng][id(sem)] = self.cnt[e2]
            for sem in self.dsems:
                tot = self.dtot[id(sem)]
                if tot == 0 or self.seen[eng].get(id(sem), 0) >= tot:
                    continue
                E.wait_ge(sem, tot)
                self.seen[eng][id(sem)] = tot

    def dump(self, name, buf, ap, shape, dt):
        if not self.dbg:
            return
        d = self.dram("dbg_" + name, shape, dt, kind="ExternalOutput")
        self.store(d, d.t, buf, ap)

    def wait_all(self, eng, bufs):
        waits = {}
        for b in bufs:
            self._collect(b, "*", True, waits)
        E = self.engs[eng]
        for k, (sem, val) in waits.items():
            E.wait_ge(sem, val)


def t5_bucket_np(n):
    n = np.maximum(n, 0)
    lr = np.log(np.maximum(n, 1).astype(np.float32) / np.float32(16)) / np.float32(math.log(128 / 16))
    large = 16 + (lr.astype(np.float32) * np.float32(16)).astype(np.int32)
    return np.where(n < 16, n, np.minimum(large, 31))


def near_table(tab_col, width=1024, off=384):
    kp = np.arange(128)[:, None]
    y = np.arange(width)[None, :]
    x = y - kp - off
    out = tab_col[t5_bucket_np(x)]
    return np.where(x >= 0, out, np.float32(NEG)).astype(np.float32)


def win_mask(width, win, off=384):
    kp = np.arange(128)[:, None]
    y = np.arange(width)[None, :]
    x = y - kp - off
    return np.where(x >= win, np.float32(NEG), np.float32(0)).astype(np.float32)


def build(T, L, dbg=False):
    NQ = T // 512
    NKC = T // 128
    NB = T // 64
    NCMP = T // 16 - 1
    NCC = (NCMP + 127) // 128
    NG = T // 1024
    assert NB <= 128 and NCMP <= 512

    nc = bass.Bass("TRN2", target_bir_lowering=False)
    es = ExitStack()
    k = KB(nc, es)
    k.dbg = dbg

    def ext(name, shape, dt=F32):
        return k.dram(name, shape, dt, kind="ExternalInput")

    xT_in = ext("xT", [D, T])
    cT_in = ext("cT", [128, 8])
    w_ada = ext("w_ada", [L, D, 6 * D])
    b_adaT = ext("b_adaT", [L, 128, 48])
    gvec = ext("gvec", [128, (2 * L + 1) * 8])
    w_inR = ext("w_inR", [L, D, FM_COLS + TM_COLS])
    posT = ext("posT", [L, 2, 64, 32])
    w1R = ext("w1R", [L, 2, 64, 32 * 256])
    w2 = ext("w2", [L, 2, 256, 64])
    sinksB = ext("sinksB", [L, 128, 8])
    tabN = ext("tabN", [128, 8, 1024])
    tabS = ext("tabS", [128, 8, 1024])
    mskW = ext("mskW", [128, 1408])
    mskS = ext("mskS", [128, 1024])
    b31 = ext("b31", [128, 16])
    wmap = ext("wmap", [NCC * 128, NB + 1])
    ftab = ext("ftab", [128, 2 * NB])
    emat = ext("emat", [NB, T])
    selg = ext("selg", [32, 24 * 64])
    identF = ext("identF", [128, 128])
    w_brR = ext("w_brR", [L, 2, 128, 4 * D])
    w_out = ext("w_out", [L, D, D])
    w_router = ext("w_router", [L, D, NEXP])
    b_router = ext("b_router", [L, 1, NEXP])
    w_upR = ext("w_upR", [L, NEXP, D, 2 * D])
    b_upR = ext("b_upR", [L, NEXP, 128, 16])
    w_down = ext("w_down", [L, NEXP, D, D])
    b_down = ext("b_down", [L, NEXP, 1, D])
    outT = k.dram("outT", [D, T], F32, kind="ExternalOutput")

    xresA = k.dram("xresA", [8, 128, T], F32)
    xresB = k.dram("xresB", [8, 128, T], F32)
    ZFM = k.dram("ZFM", [29, 128, T], BF16)
    ZTM = k.dram("ZTM", [T, TM_COLS], BF16)
    LNG = k.dram("LNG", [32, T], F32)
    OCSD = k.dram("OCSD", [4, 128, T], F32)
    ONB = k.dram("ONB", [4, 128, T], BF16)
    dbgs = {}

    xT_v = xT_in.t.rearrange("(c p) t -> p c t", p=128)
    outT_v = outT.t.rearrange("(c p) t -> p c t", p=128)

    cs = es
    ones_bf = k.sb(cs, "ones_bf", [128, 128], BF16)
    ones_f = k.sb(cs, "ones_f", [128, 128], F32)
    neg1_f = k.sb(cs, "neg1_f", [128, 64], F32)
    ident_f = k.sb(cs, "ident_f", [128, 128], F32)
    ident_bf = k.sb(cs, "ident_bf", [128, 128], BF16)
    epsc = k.sb(cs, "epsc", [128, 1], F32)
    modT = k.sb(cs, "modT", [128, L * 48], F32)
    A1 = k.sb(cs, "A1", [128, L * 8], F32)
    A2 = k.sb(cs, "A2", [128, L * 8], F32)
    gv = k.sb(cs, "gv", [128, (2 * L + 1) * 8], F32)
    b31t = k.sb(cs, "b31t", [128, 16], F32)
    PS = [k.ps(cs, "psb%d" % i, [128, 512]) for i in range(8)]
    psi = [0]

    psa = [0]

    def psum():
        p = PS[psi[0] % 6]
        psi[0] += 1
        return p

    def psum_acc():
        p = PS[6 + psa[0] % 2]
        psa[0] += 1
        return p

    k.op("dve", lambda e: e.memset(ones_bf[:], 1.0), writes=[ones_bf])
    k.op("dve", lambda e: e.memset(ones_f[:], 1.0), writes=[ones_f])
    k.op("dve", lambda e: e.memset(neg1_f[:], -1.0), writes=[neg1_f])
    k.op("dve", lambda e: e.memset(epsc[:], RMS_EPS), writes=[epsc])
    k.load(ident_f, ident_f[:], identF, identF[:, :])
    k.loadc(ident_bf, ident_bf[:], identF, identF[:, :])
    k.load(gv, gv[:], gvec, gvec[:, :])
    k.load(b31t, b31t[:], b31, b31[:, :])

    with ExitStack() as s0:
        ct = k.sb(s0, "ct", [128, 8], F32)
        sg = k.sb(s0, "sgc", [128, 8], F32)
        cond = k.sb(s0, "cond", [128, 8], F32)
        bad = k.sb(s0, "bad", [128, 48], F32)
        wa = [k.sb(s0, "wa%d" % i, [128, 8, 768], F32) for i in range(2)]
        k.load(ct, ct[:], cT_in, cT_in[:, :])
        k.op("act", lambda e: e.activation(out=sg[:], in_=ct[:], func=AF.Sigmoid), reads=[ct], writes=[sg])
        k.op("dve", lambda e: e.tensor_tensor(out=cond[:], in0=ct[:], in1=sg[:], op=ALU.mult), reads=[ct, sg], writes=[cond])
        for l in range(L):
            pm = psum()
            for blk in range(8):
                wt = wa[blk % 2]
                k.load(wt, wt[:], w_ada, w_ada[l, :, blk * 768:(blk + 1) * 768].rearrange("(k p) c -> p k c", p=128))
                for jj in range(6):
                    j = blk * 6 + jj
                    for kk in range(8):
                        k.op("pe", lambda e, wt=wt, jj=jj, kk=kk, j=j: e.matmul(
                            pm[:, j:j + 1], wt[:, kk, jj * 128:(jj + 1) * 128], cond[:, kk:kk + 1],
                            start=(kk == 0), stop=(kk == 7)), reads=[wt, cond], writes=[pm])
            k.load(bad, bad[:], b_adaT, b_adaT[l, :, :])
            k.op("dve", lambda e, l=l: e.tensor_tensor(out=modT[:, l * 48:(l + 1) * 48], in0=pm[:, 0:48], in1=bad[:], op=ALU.add),
                 reads=[pm, bad], writes=[modT])
            k.op("dve", lambda e, l=l: e.scalar_tensor_tensor(
                out=A1[:, l * 8:(l + 1) * 8], in0=modT[:, l * 48 + 8:l * 48 + 16], scalar=1.0,
                in1=gv[:, l * 8:(l + 1) * 8], op0=ALU.add, op1=ALU.mult), reads=[modT, gv], writes=[A1])
            k.op("dve", lambda e, l=l: e.scalar_tensor_tensor(
                out=A2[:, l * 8:(l + 1) * 8], in0=modT[:, l * 48 + 32:l * 48 + 40], scalar=1.0,
                in1=gv[:, (L + l) * 8:(L + l + 1) * 8], op0=ALU.add, op1=ALU.mult), reads=[modT, gv], writes=[A2])

    k.barrier()
    k.dump("modT", modT, modT[:], [128, L * 48], F32)
    k.dump("A1", A1, A1[:], [128, L * 8], F32)

    def mcol(l, kind, c):
        j = l * 48 + kind * 8 + c
        return modT[:, j:j + 1]

    def norm_tile(X, Hout, scl, shf, sq, rstd, tmp, Hf=None):
        pm = psum()
        for c in range(8):
            k.op("act", lambda e, c=c: e.activation(out=sq[:, c, :], in_=X[:, c, :], func=AF.Square),
                 reads=[(X, c)], writes=[(sq, c)])
            k.op("pe", lambda e, c=c: e.matmul(pm[:, :], ones_bf[:, :], sq[:, c, :], start=(c == 0), stop=(c == 7)),
                 reads=[(sq, c), ones_bf], writes=[pm])
        k.op("act", lambda e: e.activation(out=rstd[:], in_=pm[:, :], func=AF.Sqrt, bias=epsc[:, 0:1], scale=1.0 / D),
             reads=[pm, epsc], writes=[rstd])
        k.op("dve", lambda e: e.reciprocal(out=rstd[:], in_=rstd[:]), reads=[rstd], writes=[rstd])
        for c in range(8):
            k.op("dve", lambda e, c=c: e.tensor_tensor(out=tmp[:, c % 2, :], in0=X[:, c, :], in1=rstd[:], op=ALU.mult),
                 reads=[(X, c), rstd], writes=[(tmp, c % 2)])
            if shf is not None:
                k.op("act", lambda e, c=c: e.activation(out=Hout[:, c, :], in_=tmp[:, c % 2, :], func=AF.Identity,
                                                         scale=scl(c), bias=shf(c)),
                     reads=[(tmp, c % 2), modT, A1, A2], writes=[(Hout, c)])
                if Hf is not None:
                    k.op("act", lambda e, c=c: e.activation(out=Hf[:, c, :], in_=tmp[:, c % 2, :], func=AF.Identity,
                                                             scale=scl(c), bias=shf(c)),
                         reads=[(tmp, c % 2), modT, A1, A2], writes=[(Hf, c)])
            else:
                k.op("act", lambda e, c=c: e.activation(out=Hout[:, c, :], in_=tmp[:, c % 2, :], func=AF.Identity, scale=scl(c)),
                     reads=[(tmp, c % 2), gv], writes=[(Hout, c)])

    xcur_v = xT_v
    xcur_b = xT_in

    for l in range(L):
        k.barrier()
        with ExitStack() as sA:
            Win = k.sb(sA, "Win", [128, 8, FM_COLS + TM_COLS], BF16)
            for kk in range(8):
                for h0 in range(0, FM_COLS + TM_COLS, 1376):
                    k.loadc(Win, Win[:, kk, h0:h0 + 1376], w_inR, w_inR[l, kk * 128:(kk + 1) * 128, h0:h0 + 1376], dkey=(kk, h0))
            Xs = [k.sb(sA, "Xa%d" % i, [128, 8, 512], F32) for i in range(1)]
            sq = k.sb(sA, "sqA", [128, 8, 512], BF16)
            rstd = k.sb(sA, "rstdA", [128, 512], F32)
            tmp = k.sb(sA, "tmpA", [128, 2, 512], F32)
            Hs = [k.sb(sA, "Ha%d" % i, [128, 8, 512], BF16) for i in range(1)]
            ZT = [k.sb(sA, "ZT%d" % i, [128, 29, 512], BF16) for i in range(1)]
            VT = [k.sb(sA, "VT%d" % i, [128, 4, TM_COLS], BF16) for i in range(2)]
            GN = k.sb(sA, "GN", [32, 512], F32)
            GN2 = [k.sb(sA, "GN2%d" % i, [32, 512], F32) for i in range(2)]
            for qi in range(NQ):
                qs = slice(qi * 512, (qi + 1) * 512)
                X = Xs[0]
                H = Hs[0]
                Z = ZT[0]
                V = VT[qi % 2]
                G2 = GN2[qi % 2]
                k.load(X, X[:], xcur_b, xcur_v[:, :, qs], skey=qi)
                norm_tile(X, H, lambda c: A1[:, l * 8 + c:l * 8 + c + 1], lambda c: mcol(l, 0, c), sq, rstd, tmp)
                if qi == 0 and l == 0:
                    k.dump("H0", H, H[:], [128, 8, 512], BF16)
                    k.dump("rstd0", rstd, rstd[:], [128, 512], F32)
                    k.dump("X0", X, X[:], [128, 8, 512], F32)
                for j in range(30):
                    w = 128 if j < 29 else 32
                    pm = psum()
                    for kk in range(8):
                        k.op("pe", lambda e, kk=kk, j=j, w=w, pm=pm: e.matmul(
                            pm[0:w, :], Win[:, kk, j * 128:j * 128 + w], H[:, kk, :], start=(kk == 0), stop=(kk == 7)),
                            reads=[Win, (H, kk)], writes=[pm])
                    if j < 4 or 8 <= j < 12:
                        k.op("act", lambda e, j=j, pm=pm: e.activation(out=Z[:, j, :], in_=pm[:, :], func=AF.Copy, scale=0.125),
                             reads=[pm], writes=[(Z, j)])
                    elif j < 13:
                        k.op("dve", lambda e, j=j, pm=pm: e.tensor_copy(out=Z[:, j, :], in_=pm[:, :]), reads=[pm], writes=[(Z, j)])
                    elif j < 29:
                        k.op("act", lambda e, j=j, pm=pm: e.activation(out=Z[:, j, :], in_=pm[:, :], func=AF.Sigmoid),
                             reads=[pm], writes=[(Z, j)])
                    else:
                        k.op("act", lambda e, pm=pm: e.activation(out=GN[:, :], in_=pm[0:32, :], func=AF.Sigmoid),
                             reads=[pm], writes=[GN])
                        k.op("act", lambda e: e.activation(out=G2[:, :], in_=GN[:, :], func=AF.Ln), reads=[GN], writes=[G2])
                for q4 in range(4):
                    pm = psum()
                    for kk in range(8):
                        k.op("pe", lambda e, kk=kk, q4=q4, pm=pm: e.matmul(
                            pm[:, 0:TM_COLS], H[:, kk, q4 * 128:(q4 + 1) * 128], Win[:, kk, FM_COLS:FM_COLS + TM_COLS],
                            start=(kk == 0), stop=(kk == 7)), reads=[Win, (H, kk)], writes=[pm])
                    k.op("dve", lambda e, q4=q4, pm=pm: e.tensor_copy(out=V[:, q4, :], in_=pm[:, 0:TM_COLS]),
                         reads=[pm], writes=[(V, q4)])
                k.store(ZFM, ZFM.t[:, :, qs].rearrange("j p t -> p j t"), Z, Z[:], dkey=qi)
                k.store(ZTM, ZTM.t[qs, :].rearrange("(q p) c -> p q c", p=128), V, V[:], dkey=qi)
                k.store(LNG, LNG.t[:, qs], G2, G2[:], dkey=qi)

        k.barrier()
        with ExitStack() as sC:
            KCMP = k.sb(sC, "KCMP", [128, 512], BF16)
            VCMP = k.sb(sC, "VCMP", [128, NCC, 2, 65], BF16)
            k.op("dve", lambda e: e.memset(KCMP[:], 0.0), writes=[KCMP])
            k.op("dve", lambda e: e.memset(VCMP[:], 0.0), writes=[VCMP])
            with ExitStack() as sB:
                RT = k.sb(sB, "RT", [128, T], BF16)
                W1d = k.sb(sB, "W1d", [128, 32, 256], BF16)
                posd = k.sb(sB, "posd", [128, 32], BF16)
                W2t = k.sb(sB, "W2t", [128, 2, 64], BF16)
                posb = k.sb(sB, "posb", [128, 1], F32)
                U = k.sb(sB, "U", [128, 512], F32)
                U2 = k.sb(sB, "U2", [128, 512], F32)
                SG = k.sb(sB, "SGB", [128, 512], F32)
                HT = k.sb(sB, "HT", [128, 2, 512], BF16)
                for kv in range(2):
                    k.load(RT, RT[:], ZFM, ZFM.t[4 + kv, :, :])
                    for half in range(2):
                        k.loadc(W1d, W1d[half * 64:(half + 1) * 64, :, :], w1R,
                                w1R[l, kv, :, :].rearrange("d (l h) -> d l h", h=256), dkey=half)
                        k.loadc(posd, posd[half * 64:(half + 1) * 64, :], posT, posT[l, kv, :, :], dkey=half)
                    k.loadc(W2t, W2t[:], w2, w2[l, kv, :, :].rearrange("(c p) d -> p c d", p=128))
                    for g in range(2):
                        gs = slice(64 * g, 64 * g + 64)
                        for hc in range(2):
                            pb = psum()
                            for ll in range(32):
                                k.op("pe", lambda e, ll=ll, hc=hc, gs=gs, pb=pb: e.matmul(
                                    pb[:, 0:1], W1d[gs, ll, hc * 128:(hc + 1) * 128], posd[gs, ll:ll + 1],
                                    start=(ll == 0), stop=(ll == 31)), reads=[W1d, posd], writes=[pb])
                            k.op("dve", lambda e, pb=pb: e.tensor_copy(out=posb[:], in_=pb[:, 0:1]), reads=[pb], writes=[posb])
                            pm = psum()
                            for ll in range(32):
                                k.op("pe", lambda e, ll=ll, hc=hc, gs=gs, pm=pm: e.matmul(
                                    pm[:, 0:NCMP], W1d[gs, ll, hc * 128:(hc + 1) * 128],
                                    RT[gs, ll:ll + 16 * (NCMP - 1) + 1:16],
                                    start=(ll == 0), stop=(ll == 31)), reads=[W1d, RT], writes=[pm])
                            k.op("act", lambda e, pm=pm: e.activation(out=U[:, 0:NCMP], in_=pm[:, 0:NCMP], func=AF.Identity, bias=posb[:, 0:1]),
                                 reads=[pm, posb], writes=[U])
                            k.op("dve", lambda e: e.tensor_tensor(out=U2[:, 0:NCMP], in0=U[:, 0:NCMP], in1=U[:, 0:NCMP], op=ALU.mult),
                                 reads=[U], writes=[U2])
                            k.op("dve", lambda e: e.tensor_scalar(out=U2[:, 0:NCMP], in0=U2[:, 0:NCMP], scalar1=0.044715, scalar2=1.0,
                                                                  op0=ALU.mult, op1=ALU.add), reads=[U2], writes=[U2])
                            k.op("dve", lambda e: e.tensor_tensor(out=U2[:, 0:NCMP], in0=U2[:, 0:NCMP], in1=U[:, 0:NCMP], op=ALU.mult),
                                 reads=[U2, U], writes=[U2])
                            k.op("act", lambda e: e.activation(out=SG[:, 0:NCMP], in_=U2[:, 0:NCMP], func=AF.Sigmoid, scale=1.5957691216057308),
                                 reads=[U2], writes=[SG])
                            k.op("dve", lambda e, hc=hc: e.tensor_tensor(out=HT[:, hc, 0:NCMP], in0=U[:, 0:NCMP], in1=SG[:, 0:NCMP], op=ALU.mult),
                                 reads=[U, SG], writes=[(HT, hc)])
                        if kv == 0:
                            pm = psum()
                            for hc in range(2):
                                k.op("pe", lambda e, hc=hc, pm=pm: e.matmul(pm[0:64, 0:NCMP], W2t[:, hc, :], HT[:, hc, 0:NCMP],
                                                                            start=(hc == 0), stop=(hc == 1)),
                                     reads=[W2t, (HT, hc)], writes=[pm])
                            k.op("dve", lambda e, gs=gs, pm=pm: e.tensor_copy(out=KCMP[gs, 0:NCMP], in_=pm[0:64, 0:NCMP]),
                                 reads=[pm], writes=[(KCMP, g)])
                        else:
                            for c in range(NCC):
                                n0 = c * 128
                                nn = min(128, NCMP - n0)
                                pm = psum()
                                for hc in range(2):
                                    k.op("pe", lambda e, hc=hc, pm=pm, n0=n0, nn=nn: e.matmul(
                                        pm[0:nn, 0:64], HT[:, hc, n0:n0 + nn], W2t[:, hc, :], start=(hc == 0), stop=(hc == 1)),
                                        reads=[W2t, (HT, hc)], writes=[pm])
                                k.op("dve", lambda e, pm=pm, c=c, g=g, nn=nn: e.tensor_copy(out=VCMP[0:nn, c, g, 0:64], in_=pm[0:nn, 0:64]),
                                     reads=[pm], writes=[(VCMP, (c, g))])
                                k.op("dve", lambda e, c=c, g=g, nn=nn: e.memset(VCMP[0:nn, c, g, 64:65], 1.0), writes=[(VCMP, (c, g, 1))])

            k.barrier()
            KSL = k.sb(sC, "KSL", [128, T], BF16)
            VSL = k.sb(sC, "VSL", [128, NKC, 2, 65], BF16)
            TABN = k.sb(sC, "TABN", [128, 8, 1024], BF16)
            EM = k.sb(sC, "EM", [128, T], BF16)
            WT = k.sb(sC, "WTm", [128, NCC, NB + 1], BF16)
            FT = k.sb(sC, "FT", [128, 2 * NB], F32)
            SELG = k.sb(sC, "SELG", [32, 24 * 64], F32)
            k.load(KSL, KSL[:], ZFM, ZFM.t[6, :, :])
            k.op("pool", lambda e: e.memset(VSL[:], 1.0), writes=[VSL])
            for g in range(2):
                k.load(VSL, VSL[:, :, g, 0:64], ZTM, ZTM.t[:, g * 64:(g + 1) * 64].rearrange("(c p) d -> p c d", p=128), dkey=g)
            k.loadc(TABN, TABN[:], tabN, tabN[:, :, :])
            k.loadc(EM, EM[0:NB, :], emat, emat[:, :])
            k.loadc(WT, WT[:], wmap, wmap.t.rearrange("(c p) j -> p c j", p=128))
            k.load(FT, FT[:], ftab, ftab[:, :])
            k.load(SELG, SELG[:], selg, selg[:, :])
            QTs = [k.sb(sC, "QT%d" % i, [128, 4, 512], BF16) for i in range(2)]
            LGs = [k.sb(sC, "LG%d" % i, [32, 512], F32) for i in range(2)]
            EC = k.sb(sC, "EC", [128, 4, NCC, 512], BF16)
            ETs = [k.sb(sC, "ET%d" % i, [128, 512], BF16) for i in range(4)]
            PENT = k.sb(sC, "PENT", [128, 2, 512], BF16)
            OCS = [k.sb(sC, "OCS%d" % i, [128, 4, 512], F32) for i in range(2)]
            drow = k.sb(sC, "drow", [128, 512], F32)
            lnrow = k.sb(sC, "lnrow", [128, 512], F32)
            EB = k.sb(sC, "EB", [64, 512], F32)
            TMPF = k.sb(sC, "TMPF", [128, 512], F32)
            acc = k.sb(sC, "acc", [128, 128], F32)
            sc2 = k.sb(sC, "sc2", [128, 128], F32)
            m8a = k.sb(sC, "m8a", [128, 8], F32)
            m8b = k.sb(sC, "m8b", [128, 8], F32)
            rden = k.sb(sC, "rden", [128, 1], F32)
            pen = k.sb(sC, "pen", [128, 128], F32)

            def finalize(Ops, dest, g, hg, first, LGT, r=None, sink_ap=None, out_bf=False):
                gs = slice(64 * g, 64 * g + 64)
                k.op("dve", lambda e: e.tensor_scalar_max(out=drow[64:65, :], in0=Ops[64:65, :], scalar1=1e-30),
                     reads=[Ops], writes=[drow])
                if sink_ap is None:
                    k.op("act", lambda e: e.activation(out=lnrow[64:65, :], in_=drow[64:65, :], func=AF.Ln), reads=[drow], writes=[lnrow])
                else:
                    k.op("act", lambda e: e.activation(out=lnrow[64:65, :], in_=drow[64:65, :], func=AF.Ln, bias=sink_ap),
                         reads=[drow], writes=[lnrow])
                pb = psum()
                if r is not None:
                    k.op("pe", lambda e: e.matmul(pb[0:64, :], SELG[0:32, r * 64:(r + 1) * 64], LGT[0:32, :], start=True, stop=False),
                         reads=[SELG, LGT], writes=[pb])
                k.op("pe", lambda e: e.matmul(pb[0:64, :], neg1_f[64:65, 0:64], lnrow[64:65, :], start=(r is None), stop=True),
                     reads=[neg1_f, lnrow], writes=[pb])
                k.op("act", lambda e: e.activation(out=EB[:, :], in_=pb[0:64, :], func=AF.Exp), reads=[pb], writes=[EB])
                if first:
                    k.op("dve", lambda e: e.tensor_tensor(out=dest[gs, hg, :], in0=Ops[0:64, :], in1=EB[:, :], op=ALU.mult),
                         reads=[Ops, EB], writes=[(dest, (g, hg))])
                else:
                    k.op("dve", lambda e: e.tensor_tensor(out=TMPF[gs, :], in0=Ops[0:64, :], in1=EB[:, :], op=ALU.mult),
                         reads=[Ops, EB], writes=[TMPF])
                    k.op("pool", lambda e: e.tensor_tensor(out=dest[gs, hg, :], in0=dest[gs, hg, :], in1=TMPF[gs, :], op=ALU.add),
                         reads=[TMPF, (dest, (g, hg))], writes=[(dest, (g, hg))])

            def attn_chunks(chunks, Kt, Vt, Qt, g, hg, extra, bias_far):
                gs = slice(64 * g, 64 * g + 64)
                Ops = psum_acc()
                pend = []
                n = len(chunks)
                for i, (kc, dl) in enumerate(chunks):
                    pm = psum()
                    ex = extra(kc, dl)
                    k.op("pe", lambda e, pm=pm, kc=kc, ex=ex: e.matmul(pm[:, :], Kt[gs, kc * 128:(kc + 1) * 128], Qt[gs, hg, :],
                                                                          start=True, stop=(len(ex) == 0)),
                         reads=[Kt, Qt], writes=[pm])
                    for xi, (la, ra, bufs) in enumerate(ex):
                        k.op("pe", lambda e, pm=pm, la=la, ra=ra, xi=xi, ex=ex: e.matmul(pm[:, :], la, ra, start=False, stop=(xi == len(ex) - 1)),
                             reads=bufs, writes=[pm])
                    ET = ETs[i % 4]
                    bf = bias_far(dl)
                    if bf is None:
                        k.op("act", lambda e, pm=pm, ET=ET: e.activation(out=ET[:, :], in_=pm[:, :], func=AF.Exp), reads=[pm], writes=[ET])
                    else:
                        k.op("act", lambda e, pm=pm, ET=ET, bf=bf: e.activation(out=ET[:, :], in_=pm[:, :], func=AF.Exp, bias=bf),
                             reads=[pm, b31t], writes=[ET])
                    pend.append((kc, ET, i))
                    if len(pend) > 2:
                        kc2, ET2, i2 = pend.pop(0)
                        k.op("pe", lambda e, kc2=kc2, ET2=ET2, i2=i2: e.matmul(Ops[0:65, :], Vt[:, kc2, g, 0:65], ET2[:, :],
                                                                                 start=(i2 == 0), stop=(i2 == n - 1)),
                             reads=[Vt, ET2], writes=[Ops])
                for kc2, ET2, i2 in pend:
                    k.op("pe", lambda e, kc2=kc2, ET2=ET2, i2=i2: e.matmul(Ops[0:65, :], Vt[:, kc2, g, 0:65], ET2[:, :],
                                                                             start=(i2 == 0), stop=(i2 == n - 1)),
                         reads=[Vt, ET2], writes=[Ops])
                return Ops

            for qi in range(NQ):
                q0 = qi * 512
                qs = slice(q0, q0 + 512)
                QT = QTs[qi % 2]
                LGT = LGs[qi % 2]
                OC = OCS[qi % 2]
                k.load(QT, QT[:], ZFM, ZFM.t[0:4, :, qs].rearrange("j p t -> p j t"), skey=qi)
                k.load(LGT, LGT[:], LNG, LNG.t[:, qs], skey=qi)
                NCV = min(NCC, (q0 + 480) // 2048 + 1)
                for g in range(2):
                    gs = slice(64 * g, 64 * g + 64)
                    for hg in range(4):
                        for c in range(NCV):
                            pm = psum()
                            k.op("pe", lambda e, pm=pm, c=c: e.matmul(pm[:, :], KCMP[gs, c * 128:(c + 1) * 128], QT[gs, hg, :], start=True, stop=True),
                                 reads=[KCMP, QT], writes=[pm])
                            k.op("act", lambda e, pm=pm, c=c: e.activation(out=EC[:, hg, c, :], in_=pm[:, :], func=AF.Exp),
                                 reads=[pm], writes=[(EC, (hg, c))])
                            if 2048 * c + 2063 > q0:
                                base = q0 - 2048 * c - 31
                                k.op("pool", lambda e, c=c, base=base: e.affine_select(
                                    out=EC[:, hg, c, :], in_=EC[:, hg, c, :], pattern=[[1, 512]], compare_op=ALU.is_ge,
                                    fill=0.0, base=base, channel_multiplier=-16), reads=[(EC, (hg, c))], writes=[(EC, (hg, c))])
                        Ops = psum_acc()
                        for c in range(NCV):
                            k.op("pe", lambda e, c=c: e.matmul(Ops[0:65, :], VCMP[:, c, g, 0:65], EC[:, hg, c, :], start=(c == 0), stop=(c == NCV - 1)),
                                 reads=[VCMP, (EC, (hg, c))], writes=[Ops])
                        finalize(Ops, OC, g, hg, True, LGT, r=0 * 8 + g * 4 + hg)
                    for q4 in range(4):
                        t128 = qi * 4 + q4
                        for hg in range(4):
                            pi = psum()
                            for c in range(NCV):
                                k.op("pe", lambda e, c=c, pi=pi: e.matmul(pi[:, 0:NB + 1], EC[:, hg, c, q4 * 128:(q4 + 1) * 128], WT[:, c, :],
                                                                          start=(c == 0), stop=(c == NCV - 1)),
                                     reads=[WT, (EC, (hg, c))], writes=[pi])
                            k.op("dve", lambda e, pi=pi: e.tensor_scalar_max(out=rden[:], in0=pi[:, NB:NB + 1], scalar1=1e-30), reads=[pi], writes=[rden])
                            k.op("dve", lambda e: e.reciprocal(out=rden[:], in_=rden[:]), reads=[rden], writes=[rden])
                            if hg == 0:
                                k.op("dve", lambda e, pi=pi: e.tensor_scalar(out=acc[:, 0:NB], in0=pi[:, 0:NB], scalar1=rden[:, 0:1], scalar2=None, op0=ALU.mult),
                                     reads=[pi, rden], writes=[acc])
                            else:
                                k.op("dve", lambda e, pi=pi: e.scalar_tensor_tensor(out=acc[:, 0:NB], in0=pi[:, 0:NB], scalar=rden[:, 0:1], in1=acc[:, 0:NB],
                                                                                    op0=ALU.mult, op1=ALU.add), reads=[pi, rden, acc], writes=[acc])
                        fo = NB - 2 * t128
                        k.op("dve", lambda e, fo=fo: e.tensor_tensor(out=acc[:, 0:NB], in0=acc[:, 0:NB], in1=FT[:, fo:fo + NB], op=ALU.add),
                             reads=[acc, FT], writes=[acc])
                        k.op("dve", lambda e: e.tensor_scalar_add(out=acc[:, 0:1], in0=acc[:, 0:1], scalar1=100.0), reads=[acc], writes=[acc])
                        k.op("dve", lambda e: e.max(out=m8a[:], in_=acc[:, 0:NB]), reads=[acc], writes=[m8a])
                        k.op("dve", lambda e: e.match_replace(out=sc2[:, 0:NB], in_to_replace=m8a[:], in_values=acc[:, 0:NB], imm_value=-1e30),
                             reads=[acc, m8a], writes=[sc2])
                        k.op("dve", lambda e: e.max(out=m8b[:], in_=sc2[:, 0:NB]), reads=[sc2], writes=[m8b])
                        k.op("dve", lambda e: e.tensor_scalar(out=pen[:, 0:NB], in0=acc[:, 0:NB], scalar1=m8b[:, 7:8], scalar2=1.0,
                                                              op0=ALU.is_ge, op1=ALU.subtract), reads=[acc, m8b], writes=[pen])
                        pt = psum()
                        k.op("pe", lambda e, pt=pt: e.transpose(pt[0:NB, 0:128], pen[:, 0:NB], ident_f[:, :]), reads=[pen, ident_f], writes=[pt])
                        k.op("act", lambda e, pt=pt, q4=q4: e.activation(out=PENT[0:NB, g, q4 * 128:(q4 + 1) * 128], in_=pt[0:NB, 0:128],
                                                                          func=AF.Copy, scale=-NEG),
                             reads=[pt], writes=[(PENT, (g, q4))])
                    for hg in range(4):
                        h = g * 4 + hg
                        chunks = [(kc, q0 - 128 * kc) for kc in range(0, 4 * qi + 4)]

                        def extra(kc, dl, h=h, g=g):
                            ex = [(EM[0:NB, kc * 128:(kc + 1) * 128], PENT[0:NB, g, :], [EM, PENT])]
                            if dl <= 128:
                                ex.append((ident_bf[:, :], TABN[:, h, dl + 384:dl + 384 + 512], [ident_bf, TABN]))
                            return ex

                        Ops = attn_chunks(chunks, KSL, VSL, QT, g, hg, extra,
                                          lambda dl, h=h: (b31t[:, h:h + 1] if dl >= 256 else None))
                        finalize(Ops, OC, g, hg, False, LGT, r=1 * 8 + g * 4 + hg)
                k.store(OCSD, OCSD.t[:, :, qs].rearrange("j p t -> p j t"), OC, OC[:], dkey=qi)

        k.barrier()
        with ExitStack() as sD:
            KW = k.sb(sD, "KW", [128, T], BF16)
            VW = k.sb(sD, "VW", [128, NKC, 2, 65], BF16)
            TABN = k.sb(sD, "TABN2", [128, 8, 1024], BF16)
            MW = k.sb(sD, "MW", [128, 1408], BF16)
            SELG = k.sb(sD, "SELG2", [32, 24 * 64], F32)
            k.load(KW, KW[:], ZFM, ZFM.t[7, :, :])
            k.op("pool", lambda e: e.memset(VW[:], 1.0), writes=[VW])
            for g in range(2):
                k.load(VW, VW[:, :, g, 0:64], ZTM, ZTM.t[:, 128 + g * 64:128 + (g + 1) * 64].rearrange("(c p) d -> p c d", p=128), dkey=g)
            k.loadc(TABN, TABN[:], tabN, tabN[:, :, :])
            k.loadc(MW, MW[:], mskW, mskW[:, :])
            k.load(SELG, SELG[:], selg, selg[:, :])
            QTs = [k.sb(sD, "QTw%d" % i, [128, 4, 512], BF16) for i in range(2)]
            LGs = [k.sb(sD, "LGw%d" % i, [32, 512], F32) for i in range(2)]
            OCs = [k.sb(sD, "OCw%d" % i, [128, 4, 512], F32) for i in range(2)]
            ONs = [k.sb(sD, "ONs%d" % i, [128, 4, 512], BF16) for i in range(2)]
            ETs = [k.sb(sD, "ETw%d" % i, [128, 512], BF16) for i in range(4)]
            drow = k.sb(sD, "droww", [128, 512], F32)
            lnrow = k.sb(sD, "lnroww", [128, 512], F32)
            EB = k.sb(sD, "EBw", [64, 512], F32)
            TMPF = k.sb(sD, "TMPFw", [128, 512], F32)
            for qi in range(NQ):
                q0 = qi * 512
                qs = slice(q0, q0 + 512)
                QT, LGT, OC, ON = QTs[qi % 2], LGs[qi % 2], OCs[qi % 2], ONs[qi % 2]
                k.load(QT, QT[:], ZFM, ZFM.t[0:4, :, qs].rearrange("j p t -> p j t"), skey=qi)
                k.load(LGT, LGT[:], LNG, LNG.t[:, qs], skey=qi)
                k.load(OC, OC[:], OCSD, OCSD.t[:, :, qs].rearrange("j p t -> p j t"), skey=qi)
                for g in range(2):
                    for hg in range(4):
                        h = g * 4 + hg
                        chunks = [(kc, q0 - 128 * kc) for kc in range(max(0, 4 * qi - 4), 4 * qi + 4)]

                        def extra(kc, dl, h=h):
                            ex = []
                            if dl <= 128:
                                ex.append((ident_bf[:, :], TABN[:, h, dl + 384:dl + 384 + 512], [ident_bf, TABN]))
                            if dl >= 128:
                                ex.append((ident_bf[:, :], MW[:, dl + 384:dl + 384 + 512], [ident_bf, MW]))
                            return ex

                        Ops = attn_chunks(chunks, KW, VW, QT, g, hg, extra, lambda dl, h=h: (b31t[:, h:h + 1] if dl >= 256 else None))
                        finalize(Ops, OC, g, hg, False, LGT, r=2 * 8 + g * 4 + hg)
                k.op("act", lambda e: e.copy(out=ON[:], in_=OC[:]), reads=[OC], writes=[ON])
                k.store(ONB, ONB.t[:, :, qs].rearrange("j p t -> p j t"), ON, ON[:], dkey=qi)

        k.barrier()
        with ExitStack() as sD:
            KS = k.sb(sD, "KS", [128, T], BF16)
            VS = k.sb(sD, "VS", [128, NKC, 2, 65], BF16)
            TABS = k.sb(sD, "TABS", [128, 8, 1024], BF16)
            MS = k.sb(sD, "MS", [128, 1024], BF16)
            WB = k.sb(sD, "WB", [128, 2, 4, D], BF16)
            WO = k.sb(sD, "WO", [128, 8, D], BF16)
            snk = k.sb(sD, "snk", [128, 8], F32)
            esnk = k.sb(sD, "esnk", [128, 8], F32)
            k.load(KS, KS[:], ZFM, ZFM.t[12, :, :])
            k.op("pool", lambda e: e.memset(VS[:], 1.0), writes=[VS])
            for g in range(2):
                k.load(VS, VS[:, :, g, 0:64], ZTM, ZTM.t[:, 256 + g * 64:256 + (g + 1) * 64].rearrange("(c p) d -> p c d", p=128), dkey=g)
            k.loadc(TABS, TABS[:], tabS, tabS[:, :, :])
            k.loadc(MS, MS[:], mskS, mskS[:, :])
            for b in range(2):
                for hg in range(4):
                    k.loadc(WB, WB[:, b, hg, :], w_brR, w_brR[l, b, :, hg * D:(hg + 1) * D], dkey=(b, hg))
            for kk in range(8):
                k.loadc(WO, WO[:, kk, :], w_out, w_out[l, kk * 128:(kk + 1) * 128, :], dkey=kk)
            k.load(snk, snk[:], sinksB, sinksB[l, :, :])
            k.op("act", lambda e: e.activation(out=esnk[:], in_=snk[:], func=AF.Exp), reads=[snk], writes=[esnk])
            QS = k.sb(sD, "QSs", [128, 4, 512], BF16)
            GM = k.sb(sD, "GM", [128, 16, 512], BF16)
            X = k.sb(sD, "Xc", [128, 8, 512], F32)
            ONb = k.sb(sD, "ONb", [128, 4, 512], BF16)
            OSb = k.sb(sD, "OSb", [128, 4, 512], BF16)
            MG = k.sb(sD, "MG", [128, 8, 512], BF16)
            M1 = k.sb(sD, "M1", [128, 512], F32)
            M2 = k.sb(sD, "M2", [128, 512], F32)
            ETs = [k.sb(sD, "ETs%d" % i, [128, 512], BF16) for i in range(4)]
            drow = k.sb(sD, "drows", [128, 512], F32)
            lnrow = k.sb(sD, "lnrows", [128, 512], F32)
            EB = k.sb(sD, "EBs", [64, 512], F32)
            TMPF = k.sb(sD, "TMPFs", [128, 512], F32)
            for qi in range(NQ):
                q0 = qi * 512
                qs = slice(q0, q0 + 512)
                k.load(QS, QS[:], ZFM, ZFM.t[8:12, :, qs].rearrange("j p t -> p j t"), skey=qi)
                k.load(GM, GM[:], ZFM, ZFM.t[13:29, :, qs].rearrange("j p t -> p j t"), skey=qi)
                k.load(ONb, ONb[:], ONB, ONB.t[:, :, qs].rearrange("j p t -> p j t"), skey=qi)
                k.load(X, X[:], xcur_b, xcur_v[:, :, qs], skey=qi)
                for g in range(2):
                    for hg in range(4):
                        h = g * 4 + hg
                        chunks = [(kc, q0 - 128 * kc) for kc in range(max(0, 4 * qi - 1), 4 * qi + 4)]

                        def extra(kc, dl, h=h):
                            ex = [(ident_bf[:, :], TABS[:, h, dl + 384:dl + 384 + 512], [ident_bf, TABS])]
                            if dl >= -256:
                                ex.append((ident_bf[:, :], MS[:, dl + 384:dl + 384 + 512], [ident_bf, MS]))
                            return ex

                        Ops = attn_chunks(chunks, KS, VS, QS, g, hg, extra, lambda dl: None)
                        finalize(Ops, OSb, g, hg, True, None, r=None, sink_ap=esnk[64:65, h:h + 1])
                for cc in range(8):
                    pa = psum()
                    pb = psum()
                    for hg in range(4):
                        k.op("pe", lambda e, hg=hg, cc=cc, pa=pa: e.matmul(pa[:, :], WB[:, 0, hg, cc * 128:(cc + 1) * 128], ONb[:, hg, :],
                                                                           start=(hg == 0), stop=(hg == 3)), reads=[WB, ONb], writes=[pa])
                    for hg in range(4):
                        k.op("pe", lambda e, hg=hg, cc=cc, pb=pb: e.matmul(pb[:, :], WB[:, 1, hg, cc * 128:(cc + 1) * 128], OSb[:, hg, :],
                                                                           start=(hg == 0), stop=(hg == 3)), reads=[WB, OSb], writes=[pb])
                    k.op("dve", lambda e, cc=cc, pa=pa: e.tensor_tensor(out=M1[:, :], in0=pa[:, :], in1=GM[:, cc, :], op=ALU.mult),
                         reads=[pa, GM], writes=[M1])
                    k.op("dve", lambda e, cc=cc, pb=pb: e.tensor_tensor(out=M2[:, :], in0=pb[:, :], in1=GM[:, 8 + cc, :], op=ALU.mult),
                         reads=[pb, GM], writes=[M2])
                    k.op("pool", lambda e, cc=cc: e.tensor_tensor(out=MG[:, cc, :], in0=M1[:, :], in1=M2[:, :], op=ALU.add),
                         reads=[M1, M2], writes=[(MG, cc)])
                for co in range(8):
                    pm = psum()
                    for cc in range(8):
                        k.op("pe", lambda e, cc=cc, co=co, pm=pm: e.matmul(pm[:, :], WO[:, cc, co * 128:(co + 1) * 128], MG[:, cc, :],
                                                                           start=(cc == 0), stop=(cc == 7)), reads=[WO, (MG, cc)], writes=[pm])
                    k.op("dve", lambda e, co=co, pm=pm: e.scalar_tensor_tensor(out=X[:, co, :], in0=pm[:, :], scalar=mcol(l, 2, co), in1=X[:, co, :],
                                                                                op0=ALU.mult, op1=ALU.add), reads=[pm, modT, (X, co)], writes=[(X, co)])
                k.store(xresA, xresA.t[:, :, qs].rearrange("c p t -> p c t"), X, X[:], dkey=qi)

        k.barrier()
        with ExitStack() as sE:
            WU = [k.sb(sE, "WU%d" % i, [128, 8, 2 * D], BF16) for i in range(2)]
            WDn = [k.sb(sE, "WD%d" % i, [128, 8, D], BF16) for i in range(1)]
            BU = [k.sb(sE, "BU%d" % i, [128, 16], F32) for i in range(2)]
            BD = [k.sb(sE, "BD%d" % i, [1, D], BF16) for i in range(2)]
            WR = k.sb(sE, "WR", [128, 8, NEXP], F32)
            BR = k.sb(sE, "BR", [1, NEXP], F32)
            XT = k.sb(sE, "XTm", [128, 8, 512], F32)
            HF = XT
            H2 = k.sb(sE, "H2", [128, 8, 1024], BF16)
            sq = k.sb(sE, "sqM", [128, 8, 512], BF16)
            rstd = k.sb(sE, "rstdM", [128, 512], F32)
            tmp = k.sb(sE, "tmpM", [128, 2, 512], F32)
            YACC = k.sb(sE, "YACC", [128, 8, D], F32)
            GATES = k.sb(sE, "GATES", [128, 8, NEXP], F32)
            LGm = k.sb(sE, "LGm", [128, NEXP], F32)
            Pm = k.sb(sE, "Pm", [128, NEXP], F32)
            Mk = k.sb(sE, "Mk", [128, NEXP], F32)
            m8 = k.sb(sE, "m8", [128, 8], F32)
            e4 = k.sb(sE, "e4", [128, 4], F32)
            nmx = k.sb(sE, "nmx", [128, 1], F32)
            den = k.sb(sE, "den", [128, 1], F32)
            G1 = [k.sb(sE, "G1%d" % i, [128, 512], F32) for i in range(2)]
            S1 = [k.sb(sE, "S1%d" % i, [128, 512], F32) for i in range(2)]
            L1 = [k.sb(sE, "L1%d" % i, [128, 512], F32) for i in range(2)]
            HID = k.sb(sE, "HID", [128, 8, 512], BF16)
            k.load(WR, WR[:], w_router, w_router[l, :, :].rearrange("(k p) e -> p k e", p=128))
            k.load(BR, BR[:], b_router, b_router[l, :, :])
            for gi in range(NG):
                t0 = gi * 1024
                for hf in range(2):
                    qs = slice(t0 + hf * 512, t0 + hf * 512 + 512)
                    k.load(XT, XT[:], xresA, xresA.t[:, :, qs].rearrange("c p t -> p c t"), skey=(t0 + hf * 512) // 512)
                    H2half = _HalfView(H2, hf)
                    norm_tile(XT, H2half, lambda c: A2[:, l * 8 + c:l * 8 + c + 1], lambda c: mcol(l, 3, c), sq, rstd, tmp, Hf=HF)
                    for q4 in range(4):
                        sub = hf * 4 + q4
                        pm = psum()
                        for kk in range(8):
                            k.op("pe", lambda e, kk=kk, q4=q4, pm=pm: e.matmul(pm[:, 0:NEXP], HF[:, kk, q4 * 128:(q4 + 1) * 128], WR[:, kk, :],
                                                                               start=(kk == 0), stop=False), reads=[(HF, kk), WR], writes=[pm])
                        k.op("pe", lambda e, pm=pm: e.matmul(pm[:, 0:NEXP], ones_f[0:1, 0:128], BR[0:1, :], start=False, stop=True),
                             reads=[ones_f, BR], writes=[pm])
                        k.op("dve", lambda e, pm=pm: e.tensor_copy(out=LGm[:], in_=pm[:, 0:NEXP]), reads=[pm], writes=[LGm])
                        k.op("dve", lambda e: e.max(out=m8[:], in_=LGm[:]), reads=[LGm], writes=[m8])
                        k.op("dve", lambda e: e.tensor_scalar(out=nmx[:], in0=m8[:, 0:1], scalar1=-1.0, scalar2=None, op0=ALU.mult), reads=[m8], writes=[nmx])
                        k.op("act", lambda e: e.activation(out=e4[:], in_=m8[:, 0:4], func=AF.Exp, bias=nmx[:, 0:1]), reads=[m8, nmx], writes=[e4])
                        k.op("dve", lambda e: e.reduce_sum(out=den[:], in_=e4[:], axis=mybir.AxisListType.X), reads=[e4], writes=[den])
                        k.op("dve", lambda e: e.reciprocal(out=den[:], in_=den[:]), reads=[den], writes=[den])
                        k.op("act", lambda e: e.activation(out=Pm[:], in_=LGm[:], func=AF.Exp, bias=nmx[:, 0:1]), reads=[LGm, nmx], writes=[Pm])
                        k.op("dve", lambda e: e.tensor_scalar(out=Mk[:], in0=LGm[:], scalar1=m8[:, 3:4], scalar2=None, op0=ALU.is_ge), reads=[LGm, m8], writes=[Mk])
                        k.op("dve", lambda e, sub=sub: e.scalar_tensor_tensor(out=GATES[:, sub, :], in0=Pm[:], scalar=den[:, 0:1], in1=Mk[:],
                                                                               op0=ALU.mult, op1=ALU.mult), reads=[Pm, den, Mk], writes=[(GATES, sub)])
                for ex in range(NEXP):
                    wu, wd, bu, bd = WU[ex % 2], WDn[0], BU[ex % 2], BD[ex % 2]
                    for kk in range(8):
                        k.loadc(wu, wu[:, kk, :], w_upR, w_upR[l, ex, kk * 128:(kk + 1) * 128, :], dkey=kk)
                    for kk in range(8):
                        k.loadc(wd, wd[:, kk, :], w_down, w_down[l, ex, kk * 128:(kk + 1) * 128, :], dkey=kk)
                    k.load(bu, bu[:], b_upR, b_upR[l, ex, :, :])
                    k.loadc(bd, bd[:], b_down, b_down[l, ex, :, :])
                    for hf in range(2):
                        hs = slice(hf * 512, hf * 512 + 512)
                        for jc in range(8):
                            pg = psum()
                            pl = psum()
                            g1, s1, l1 = G1[jc % 2], S1[jc % 2], L1[jc % 2]
                            for kk in range(8):
                                k.op("pe", lambda e, kk=kk, jc=jc, pg=pg: e.matmul(pg[:, :], wu[:, kk, jc * 128:(jc + 1) * 128], H2[:, kk, hs],
                                                                                   start=(kk == 0), stop=(kk == 7)), reads=[(wu, kk), H2], writes=[pg])
                            for kk in range(8):
                                k.op("pe", lambda e, kk=kk, jc=jc, pl=pl: e.matmul(pl[:, :], wu[:, kk, D + jc * 128:D + (jc + 1) * 128], H2[:, kk, hs],
                                                                                   start=(kk == 0), stop=(kk == 7)), reads=[(wu, kk), H2], writes=[pl])
                            k.op("dve", lambda e, jc=jc, pg=pg, g1=g1: e.tensor_scalar(out=g1[:], in0=pg[:, :], scalar1=bu[:, jc:jc + 1], scalar2=7.0,
                                                                                       op0=ALU.add, op1=ALU.min), reads=[pg, bu], writes=[g1])
                            k.op("act", lambda e, g1=g1, s1=s1: e.activation(out=s1[:], in_=g1[:], func=AF.Sigmoid, scale=1.702), reads=[g1], writes=[s1])
                            k.op("dve", lambda e, jc=jc, pl=pl, l1=l1: e.tensor_scalar(out=l1[:], in0=pl[:, :], scalar1=bu[:, 8 + jc:9 + jc], scalar2=7.0,
                                                                                       op0=ALU.add, op1=ALU.min), reads=[pl, bu], writes=[l1])
                            k.op("pool", lambda e, l1=l1: e.tensor_scalar(out=l1[:], in0=l1[:], scalar1=-7.0, scalar2=1.0, op0=ALU.max, op1=ALU.add),
                                 reads=[l1], writes=[l1])
                            k.op("pool", lambda e, g1=g1, s1=s1: e.tensor_tensor(out=g1[:], in0=g1[:], in1=s1[:], op=ALU.mult), reads=[g1, s1], writes=[g1])
                            k.op("dve", lambda e, jc=jc, g1=g1, l1=l1: e.tensor_tensor(out=HID[:, jc, :], in0=g1[:], in1=l1[:], op=ALU.mult),
                                 reads=[g1, l1], writes=[(HID, jc)])
                        for q4 in range(4):
                            sub = hf * 4 + q4
                            for ch in range(2):
                                py = psum()
                                for jc in range(8):
                                    k.op("pe", lambda e, jc=jc, q4=q4, ch=ch, py=py: e.matmul(py[:, :], HID[:, jc, q4 * 128:(q4 + 1) * 128], wd[:, jc, ch * 512:(ch + 1) * 512],
                                                                                              start=(jc == 0), stop=False), reads=[(HID, jc), (wd, jc)], writes=[py])
                                k.op("pe", lambda e, ch=ch, py=py: e.matmul(py[:, :], ones_bf[0:1, 0:128], bd[0:1, ch * 512:(ch + 1) * 512], start=False, stop=True),
                                     reads=[ones_bf, bd], writes=[py])
                                if ex == 0:
                                    k.op("dve", lambda e, sub=sub, ch=ch, py=py: e.tensor_scalar(out=YACC[:, sub, ch * 512:(ch + 1) * 512], in0=py[:, :],
                                                                                                 scalar1=GATES[:, sub, ex:ex + 1], scalar2=None, op0=ALU.mult),
                                         reads=[py, (GATES, sub)], writes=[(YACC, (sub, ch))])
                                else:
                                    k.op("dve", lambda e, sub=sub, ch=ch, py=py, ex=ex: e.scalar_tensor_tensor(
                                        out=YACC[:, sub, ch * 512:(ch + 1) * 512], in0=py[:, :], scalar=GATES[:, sub, ex:ex + 1],
                                        in1=YACC[:, sub, ch * 512:(ch + 1) * 512], op0=ALU.mult, op1=ALU.add),
                                        reads=[py, (GATES, sub), (YACC, (sub, ch))], writes=[(YACC, (sub, ch))])
                for hf in range(2):
                    qs = slice(t0 + hf * 512, t0 + hf * 512 + 512)
                    k.load(XT, XT[:], xresA, xresA.t[:, :, qs].rearrange("c p t -> p c t"), skey=(t0 + hf * 512) // 512)
                    for cc in range(8):
                        pm = psum()
                        for q4 in range(4):
                            sub = hf * 4 + q4
                            k.op("pe", lambda e, q4=q4, sub=sub, cc=cc, pm=pm: e.transpose(pm[:, q4 * 128:(q4 + 1) * 128], YACC[:, sub, cc * 128:(cc + 1) * 128], ident_f[:, :]),
                                 reads=[YACC, ident_f], writes=[pm])
                        k.op("dve", lambda e, cc=cc, pm=pm: e.scalar_tensor_tensor(out=XT[:, cc, :], in0=pm[:, :], scalar=mcol(l, 5, cc), in1=XT[:, cc, :],
                                                                                    op0=ALU.mult, op1=ALU.add), reads=[pm, modT, (XT, cc)], writes=[(XT, cc)])
                    k.store(xresB, xresB.t[:, :, qs].rearrange("c p t -> p c t"), XT, XT[:], dkey=(t0 + hf * 512) // 512)
        xcur_b = xresB
        xcur_v = xresB.t.rearrange("c p t -> p c t")

    k.barrier()
    with ExitStack() as sF:
        Xs = [k.sb(sF, "Xf%d" % i, [128, 8, 512], F32) for i in range(2)]
        Os = [k.sb(sF, "Of%d" % i, [128, 8, 512], F32) for i in range(2)]
        sq = k.sb(sF, "sqF", [128, 8, 512], BF16)
        rstd = k.sb(sF, "rstdF", [128, 512], F32)
        tmp = k.sb(sF, "tmpF", [128, 2, 512], F32)
        for qi in range(NQ):
            qs = slice(qi * 512, (qi + 1) * 512)
            X, O = Xs[qi % 2], Os[qi % 2]
            k.load(X, X[:], xcur_b, xcur_v[:, :, qs], skey=qi)
            norm_tile(X, O, lambda c: gv[:, 2 * L * 8 + c:2 * L * 8 + c + 1], None, sq, rstd, tmp)
            k.store(outT, outT_v[:, :, qs], O, O[:], dkey=qi)
        k.wait_all("sp", [outT])
    es.close()
    return nc, k


class _HalfView:
    def __init__(self, base, hf):
        self.base = base
        self.hf = hf
        self.name = base.name
        self.regs = base.regs
        self.dsem = None

    def __getitem__(self, idx):
        p, c, t = idx
        assert t == slice(None, None, None)
        return self.base.t[p, c, self.hf * 512:(self.hf + 1) * 512]


def prep_shared(inp, T, L):
    f = np.float32
    NB = T // 64
    NCMP = T // 16 - 1
    NCC = (NCMP + 127) // 128
    sh = {}
    sh["w_ada"] = np.ascontiguousarray(inp["w_ada"][:L], f)
    sh["b_adaT"] = np.ascontiguousarray(inp["b_ada"][:L].reshape(L, 48, 128).transpose(0, 2, 1), f)
    gl = [inp["g_mix"][l] for l in range(L)] + [inp["g_ffn"][l] for l in range(L)] + [inp["g_final"]]
    sh["gvec"] = np.ascontiguousarray(np.concatenate([np.asarray(v, f).reshape(8, 128).T for v in gl], axis=1), f)
    w_in = np.asarray(inp["w_in"][:L], f)
    o_qn, o_kv, o_gn, o_qs, o_kvs, o_gm = 0, 512, 1280, 1304, 1816, 2072
    cols = []
    for hg in range(4):
        for g in range(2):
            h = g * 4 + hg
            cols += list(range(o_qn + h * 64, o_qn + (h + 1) * 64))
    cols += list(range(o_kv + 0, o_kv + 128))
    cols += list(range(o_kv + 128, o_kv + 256))
    cols += list(range(o_kv + 256, o_kv + 384))
    cols += list(range(o_kv + 512, o_kv + 640))
    for hg in range(4):
        for g in range(2):
            h = g * 4 + hg
            cols += list(range(o_qs + h * 64, o_qs + (h + 1) * 64))
    cols += list(range(o_kvs, o_kvs + 128))
    cols += list(range(o_gm, o_gm + 2048))
    cols += list(range(o_gn, o_gn + 24))
    fm = w_in[:, :, cols]
    fm = np.concatenate([fm, np.zeros((L, D, 8), f)], axis=2)
    tmc = list(range(o_kv + 384, o_kv + 512)) + list(range(o_kv + 640, o_kv + 768)) + list(range(o_kvs + 128, o_kvs + 256))
    sh["w_inR"] = np.ascontiguousarray(np.concatenate([fm, w_in[:, :, tmc]], axis=2), f)
    assert sh["w_inR"].shape[2] == FM_COLS + TM_COLS
    sh["posT"] = np.ascontiguousarray(np.asarray(inp["cmp_pos"][:L], f).transpose(0, 1, 3, 2))
    sh["w1R"] = np.ascontiguousarray(np.asarray(inp["cmp_w1"][:L], f).reshape(L, 2, 32, 64, 256).transpose(0, 1, 3, 2, 4).reshape(L, 2, 64, 32 * 256))
    sh["w2"] = np.ascontiguousarray(inp["cmp_w2"][:L], f)
    sh["sinksB"] = np.ascontiguousarray(np.broadcast_to(np.asarray(inp["sinks"][:L], f)[:, None, :], (L, 128, 8)))
    rel = np.asarray(inp["rel_tab"], f)
    sh["tabN"] = np.ascontiguousarray(np.stack([near_table(rel[:, h]) for h in range(8)], axis=1))
    sh["tabS"] = np.ascontiguousarray(np.stack([near_table(rel[:, 8 + h]) for h in range(8)], axis=1))
    sh["mskW"] = win_mask(1408, 512)
    sh["mskS"] = win_mask(1024, 128)
    sh["b31"] = np.ascontiguousarray(np.broadcast_to(rel[31][None, :], (128, 16)))
    wm = np.zeros((NCC * 128, NB + 1), f)
    for j in range(NB):
        for m in range(4):
            for n in range(2):
                i = 4 * j - m - n
                if 0 <= i < NCMP:
                    wm[i, j] += 1.0
    wm[:NCMP, NB] = 1.0
    sh["wmap"] = wm
    ft = np.zeros((128, 2 * NB), f)
    for qp in range(128):
        s = 1 if qp >= 64 else 0
        for y in range(2 * NB):
            x = y - NB
            if x == s or x == s - 1:
                ft[qp, y] = 100.0
            elif x > s:
                ft[qp, y] = -100.0
    sh["ftab"] = ft
    em = np.zeros((NB, T), f)
    for j in range(NB):
        em[j, j * 64:(j + 1) * 64] = 1.0
    sh["emat"] = em
    sg = np.zeros((32, 24 * 64), f)
    for r in range(24):
        sg[r, r * 64:(r + 1) * 64] = 1.0
    sh["selg"] = sg
    sh["identF"] = np.eye(128, dtype=f)
    wb = np.asarray(inp["w_branch"][:L], f).reshape(L, 2, 2, 4, 64, D)
    sh["w_brR"] = np.ascontiguousarray(wb.transpose(0, 1, 2, 4, 3, 5).reshape(L, 2, 128, 4 * D))
    sh["w_out"] = np.ascontiguousarray(inp["w_out"][:L], f)
    sh["w_router"] = np.ascontiguousarray(inp["w_router"][:L], f)
    sh["b_router"] = np.ascontiguousarray(np.asarray(inp["b_router"][:L], f)[:, None, :])
    wu = np.asarray(inp["w_up"][:L], f)
    sh["w_upR"] = np.ascontiguousarray(np.concatenate([wu[..., 0::2], wu[..., 1::2]], axis=-1))
    bu = np.asarray(inp["b_up"][:L], f)
    bu = np.concatenate([bu[..., 0::2], bu[..., 1::2]], axis=-1)
    sh["b_upR"] = np.ascontiguousarray(bu.reshape(L, NEXP, 16, 128).transpose(0, 1, 3, 2))
    sh["w_down"] = np.ascontiguousarray(inp["w_down"][:L], f)
    sh["b_down"] = np.ascontiguousarray(np.asarray(inp["b_down"][:L], f)[:, :, None, :])
    return sh


def run_model(inp, T, L, n_cores=8, dbg=False):
    x = np.asarray(inp["x"], np.float32)
    c = np.asarray(inp["c"], np.float32)
    B = x.shape[0]
    sh = prep_shared(inp, T, L)
    nc, kb = build(T, L, dbg)
    in_maps = []
    for core in range(n_cores):
        b = core % B
        m = dict(sh)
        m["xT"] = np.ascontiguousarray(x[b].T)
        m["cT"] = np.ascontiguousarray(c[b].reshape(8, 128).T)
        in_maps.append(m)
    res = run_bass_kernel_spmd(nc, in_maps, core_ids=list(range(n_cores)))
    out = np.stack([np.ascontiguousarray(res.results[b]["outT"].T) for b in range(B)], axis=0)
    if dbg:
        return out.astype(np.float32), res.results[0]
    return out.astype(np.float32)


def kernel(**inputs):
    return run_model(inputs, 8192, DEPTH, 8)
```

```python
import math
import numpy as np
import concourse.bass as bass
import concourse.mybir as mybir
from concourse.bass_utils import run_bass_kernel_spmd
from contextlib import ExitStack

F32 = mybir.dt.float32
BF16 = mybir.dt.bfloat16
AF = mybir.ActivationFunctionType
ALU = mybir.AluOpType

D = 1024
DEPTH = 4
NEXP = 32
NEG = -30000.0
FM_COLS = 3744
TM_COLS = 384
RMS_EPS = 1e-5


class Buf:
    def __init__(self, name, t):
        self.name = name
        self.t = t
        self.regs = {}
        self.dsem = None

    def __getitem__(self, idx):
        return self.t[idx]


class KB:
    def __init__(self, nc, es):
        self.nc = nc
        self.es = es
        self.engs = {"pe": nc.tensor, "dve": nc.vector, "act": nc.scalar, "pool": nc.gpsimd, "sp": nc.sync}
        self.esem = {}
        self.cnt = {}
        self.seen = {}
        for e in self.engs:
            self.esem[e] = es.enter_context(nc.semaphore("es_" + e))
            self.cnt[e] = 0
            self.seen[e] = {}
        self.dtot = {}
        self.dsems = []
        self.semcache = {}
        self.uid = 0
        self.nsem = 5
        self.ninst = 0

    def sb(self, es, name, shape, dt):
        self.uid += 1
        return Buf(name, es.enter_context(self.nc.sbuf_tensor("%s_u%d" % (name, self.uid), list(shape), dt)))

    def ps(self, es, name, shape, dt=F32):
        return Buf(name, es.enter_context(self.nc.psum_tensor(name, list(shape), dt)))

    dbg = False

    def dram(self, name, shape, dt, kind="Internal"):
        if kind == "Internal" and self.dbg:
            kind = "ExternalOutput"
        return Buf(name, self.nc.dram_tensor(name, list(shape), dt, kind=kind).ap())

    def _collect(self, b, key, is_write, waits):
        regs = b.regs
        keys = list(regs.keys()) if key == "*" else [k for k in (key, "*") if k in regs]
        for k in keys:
            r = regs[k]
            if r["w"] is not None:
                self._addwait(r["w"], waits)
            if is_write:
                for sem, val in r["r"].items():
                    self._addwait((sem, val), waits)

    def _addwait(self, ev, waits):
        sem, val = ev
        if id(sem) in self.dtot:
            val = self.dtot[id(sem)]
        k = id(sem)
        if k not in waits or waits[k][1] < val:
            waits[k] = (sem, val)

    def _mark(self, b, key, is_write, ev):
        regs = b.regs
        if is_write:
            if key == "*":
                regs.clear()
            regs[key] = {"w": ev, "r": {}}
        else:
            r = regs.setdefault(key, {"w": None, "r": {}})
            r["r"][ev[0]] = ev[1]

    @staticmethod
    def _norm(lst):
        out = []
        for x in lst:
            if isinstance(x, tuple):
                out.append(x)
            else:
                out.append((x, "*"))
        return out

    def _emit_waits(self, eng, reads, writes):
        waits = {}
        for b, key in reads:
            self._collect(b, key, False, waits)
        for b, key in writes:
            self._collect(b, key, True, waits)
        E = self.engs[eng]
        own = id(self.esem[eng])
        for k, (sem, val) in waits.items():
            if k == own and eng == "pe":
                continue
            if self.seen[eng].get(k, 0) >= val:
                continue
            E.wait_ge(sem, val)
            self.seen[eng][k] = val

    def op(self, eng, fn, reads=(), writes=()):
        reads = self._norm(reads)
        writes = self._norm(writes)
        self._emit_waits(eng, reads, writes)
        inst = fn(self.engs[eng])
        self.cnt[eng] += 1
        self.ninst += 1
        inst.then_inc(self.esem[eng], 1)
        ev = (self.esem[eng], self.cnt[eng])
        for b, key in reads:
            self._mark(b, key, False, ev)
        for b, key in writes:
            self._mark(b, key, True, ev)

    def dma(self, q, out_ap, in_ap, reads, writes, owner, **kw):
        reads = self._norm(reads)
        writes = self._norm(writes)
        self._emit_waits(q, reads, writes)
        if owner.dsem is None:
            if owner.name in self.semcache:
                owner.dsem = self.semcache[owner.name]
            else:
                owner.dsem = self.es.enter_context(self.nc.semaphore("ds_" + owner.name))
                self.semcache[owner.name] = owner.dsem
                self.dtot[id(owner.dsem)] = 0
                self.dsems.append(owner.dsem)
                self.nsem += 1
        inst = self.engs[q].dma_start(out=out_ap, in_=in_ap, **kw)
        inst.then_inc(owner.dsem, 16)
        self.ninst += 1
        self.dtot[id(owner.dsem)] += 16
        ev = (owner.dsem, self.dtot[id(owner.dsem)])
        for b, key in reads:
            self._mark(b, key, False, ev)
        for b, key in writes:
            self._mark(b, key, True, ev)

    def load(self, dst, dst_ap, src, src_ap, skey="*", dkey="*", q="sp", **kw):
        self.dma(q, dst_ap, src_ap, [(src, skey)], [(dst, dkey)], dst, **kw)

    def loadc(self, dst, dst_ap, src, src_ap, skey="*", dkey="*", **kw):
        kw.setdefault("max_dma_last_dim", 4096)
        self.dma("pool", dst_ap, src_ap, [(src, skey)], [(dst, dkey)], dst, **kw)

    def store(self, dst, dst_ap, src, src_ap, skey="*", dkey="*", q="sp", **kw):
        self.dma(q, dst_ap, src_ap, [(src, skey)], [(dst, dkey)], src, **kw)

    def barrier(self):
        for eng, E in self.engs.items():
            for e2, sem in self.esem.items():
                if e2 == eng or self.cnt[e2] == 0:
                    continue
                if self.seen[eng].get(id(sem), 0) >= self.cnt[e2]:
                    continue
                E.wait_ge(sem, self.cnt[e2])
                self.seen[eng][id(sem)] = self.cnt[e2]
            for sem in self.dsems:
                tot = self.dtot[id(sem)]
                if tot == 0 or self.seen[eng].get(id(sem), 0) >= tot:
                    continue
                E.wait_ge(sem, tot)
                self.seen[eng][id(sem)] = tot

    def dump(self, name, buf, ap, shape, dt):
        if not self.dbg:
            return
        d = self.dram("dbg_" + name, shape, dt, kind="ExternalOutput")
        self.store(d, d.t, buf, ap)

    def wait_all(self, eng, bufs):
        waits = {}
        for b in bufs:
            self._collect(b, "*", True, waits)
        E = self.engs[eng]
        for k, (sem, val) in waits.items():
            E.wait_ge(sem, val)


def t5_bucket_np(n):
    n = np.maximum(n, 0)
    lr = np.log(np.maximum(n, 1).astype(np.float32) / np.float32(16)) / np.float32(math.log(128 / 16))
    large = 16 + (lr.astype(np.float32) * np.float32(16)).astype(np.int32)
    return np.where(n < 16, n, np.minimum(large, 31))


def near_table(tab_col, width=1024, off=384):
    kp = np.arange(128)[:, None]
    y = np.arange(width)[None, :]
    x = y - kp - off
    out = tab_col[t5_bucket_np(x)]
    return np.where(x >= 0, out, np.float32(NEG)).astype(np.float32)


def win_mask(width, win, off=384):
    kp = np.arange(128)[:, None]
    y = np.arange(width)[None, :]
    x = y - kp - off
    return np.where(x >= win, np.float32(NEG), np.float32(0)).astype(np.float32)


def build(T, L, dbg=False):
    NQ = T // 512
    NKC = T // 128
    NB = T // 64
    NCMP = T // 16 - 1
    NCC = (NCMP + 127) // 128
    NG = T // 1024
    assert NB <= 128 and NCMP <= 512

    nc = bass.Bass("TRN2", target_bir_lowering=False)
    es = ExitStack()
    k = KB(nc, es)
    k.dbg = dbg

    def ext(name, shape, dt=F32):
        return k.dram(name, shape, dt, kind="ExternalInput")

    xT_in = ext("xT", [D, T])
    cT_in = ext("cT", [128, 8])
    w_ada = ext("w_ada", [L, D, 6 * D])
    b_adaT = ext("b_adaT", [L, 128, 48])
    gvec = ext("gvec", [128, (2 * L + 1) * 8])
    w_inR = ext("w_inR", [L, D, FM_COLS + TM_COLS])
    posT = ext("posT", [L, 2, 64, 32])
    w1R = ext("w1R", [L, 2, 64, 32 * 256])
    w2 = ext("w2", [L, 2, 256, 64])
    sinksB = ext("sinksB", [L, 128, 8])
    tabN = ext("tabN", [128, 8, 1024])
    tabS = ext("tabS", [128, 8, 1024])
    mskW = ext("mskW", [128, 1408])
    mskS = ext("mskS", [128, 1024])
    b31 = ext("b31", [128, 16])
    wmap = ext("wmap", [NCC * 128, NB + 1])
    ftab = ext("ftab", [128, 2 * NB])
    emat = ext("emat", [NB, T])
    selg = ext("selg", [32, 24 * 64])
    identF = ext("identF", [128, 128])
    w_brR = ext("w_brR", [L, 2, 128, 4 * D])
    w_out = ext("w_out", [L, D, D])
    w_router = ext("w_router", [L, D, NEXP])
    b_router = ext("b_router", [L, 1, NEXP])
    w_upR = ext("w_upR", [L, NEXP, D, 2 * D])
    b_upR = ext("b_upR", [L, NEXP, 128, 16])
    w_down = ext("w_down", [L, NEXP, D, D])
    b_down = ext("b_down", [L, NEXP, 1, D])
    outT = k.dram("outT", [D, T], F32, kind="ExternalOutput")

    xresA = k.dram("xresA", [8, 128, T], F32)
    xresB = k.dram("xresB", [8, 128, T], F32)
    ZFM = k.dram("ZFM", [29, 128, T], BF16)
    ZTM = k.dram("ZTM", [T, TM_COLS], BF16)
    LNG = k.dram("LNG", [32, T], F32)
    OCSD = k.dram("OCSD", [4, 128, T], F32)
    ONB = k.dram("ONB", [4, 128, T], BF16)
    dbgs = {}

    xT_v = xT_in.t.rearrange("(c p) t -> p c t", p=128)
    outT_v = outT.t.rearrange("(c p) t -> p c t", p=128)

    cs = es
    ones_bf = k.sb(cs, "ones_bf", [128, 128], BF16)
    ones_f = k.sb(cs, "ones_f", [128, 128], F32)
    neg1_f = k.sb(cs, "neg1_f", [128, 64], F32)
    ident_f = k.sb(cs, "ident_f", [128, 128], F32)
    ident_bf = k.sb(cs, "ident_bf", [128, 128], BF16)
    epsc = k.sb(cs, "epsc", [128, 1], F32)
    modT = k.sb(cs, "modT", [128, L * 48], F32)
    A1 = k.sb(cs, "A1", [128, L * 8], F32)
    A2 = k.sb(cs, "A2", [128, L * 8], F32)
    gv = k.sb(cs, "gv", [128, (2 * L + 1) * 8], F32)
    b31t = k.sb(cs, "b31t", [128, 16], F32)
    PS = [k.ps(cs, "psb%d" % i, [128, 512]) for i in range(8)]
    psi = [0]

    psa = [0]

    def psum():
        p = PS[psi[0] % 6]
        psi[0] += 1
        return p

    def psum_acc():
        p = PS[6 + psa[0] % 2]
        psa[0] += 1
        return p

    k.op("dve", lambda e: e.memset(ones_bf[:], 1.0), writes=[ones_bf])
    k.op("dve", lambda e: e.memset(ones_f[:], 1.0), writes=[ones_f])
    k.op("dve", lambda e: e.memset(neg1_f[:], -1.0), writes=[neg1_f])
    k.op("dve", lambda e: e.memset(epsc[:], RMS_EPS), writes=[epsc])
    k.load(ident_f, ident_f[:], identF, identF[:, :])
    k.loadc(ident_bf, ident_bf[:], identF, identF[:, :])
    k.load(gv, gv[:], gvec, gvec[:, :])
    k.load(b31t, b31t[:], b31, b31[:, :])

    with ExitStack() as s0:
        ct = k.sb(s0, "ct", [128, 8], F32)
        sg = k.sb(s0, "sgc", [128, 8], F32)
        cond = k.sb(s0, "cond", [128, 8], F32)
        bad = k.sb(s0, "bad", [128, 48], F32)
        wa = [k.sb(s0, "wa%d" % i, [128, 8, 768], F32) for i in range(2)]
        k.load(ct, ct[:], cT_in, cT_in[:, :])
        k.op("act", lambda e: e.activation(out=sg[:], in_=ct[:], func=AF.Sigmoid), reads=[ct], writes=[sg])
        k.op("dve", lambda e: e.tensor_tensor(out=cond[:], in0=ct[:], in1=sg[:], op=ALU.mult), reads=[ct, sg], writes=[cond])
        for l in range(L):
            pm = psum()
            for blk in range(8):
                wt = wa[blk % 2]
                k.load(wt, wt[:], w_ada, w_ada[l, :, blk * 768:(blk + 1) * 768].rearrange("(k p) c -> p k c", p=128))
                for jj in range(6):
                    j = blk * 6 + jj
                    for kk in range(8):
                        k.op("pe", lambda e, wt=wt, jj=jj, kk=kk, j=j: e.matmul(
                            pm[:, j:j + 1], wt[:, kk, jj * 128:(jj + 1) * 128], cond[:, kk:kk + 1],
                            start=(kk == 0), stop=(kk == 7)), reads=[wt, cond], writes=[pm])
            k.load(bad, bad[:], b_adaT, b_adaT[l, :, :])
            k.op("dve", lambda e, l=l: e.tensor_tensor(out=modT[:, l * 48:(l + 1) * 48], in0=pm[:, 0:48], in1=bad[:], op=ALU.add),
                 reads=[pm, bad], writes=[modT])
            k.op("dve", lambda e, l=l: e.scalar_tensor_tensor(
                out=A1[:, l * 8:(l + 1) * 8], in0=modT[:, l * 48 + 8:l * 48 + 16], scalar=1.0,
                in1=gv[:, l * 8:(l + 1) * 8], op0=ALU.add, op1=ALU.mult), reads=[modT, gv], writes=[A1])
            k.op("dve", lambda e, l=l: e.scalar_tensor_tensor(
                out=A2[:, l * 8:(l + 1) * 8], in0=modT[:, l * 48 + 32:l * 48 + 40], scalar=1.0,
                in1=gv[:, (L + l) * 8:(L + l + 1) * 8], op0=ALU.add, op1=ALU.mult), reads=[modT, gv], writes=[A2])

    k.barrier()
    k.dump("modT", modT, modT[:], [128, L * 48], F32)
    k.dump("A1", A1, A1[:], [128, L * 8], F32)

    def mcol(l, kind, c):
        j = l * 48 + kind * 8 + c
        return modT[:, j:j + 1]

    def norm_tile(X, Hout, scl, shf, sq, rstd, tmp, Hf=None):
        pm = psum()
        for c in range(8):
            k.op("act", lambda e, c=c: e.activation(out=sq[:, c, :], in_=X[:, c, :], func=AF.Square),
                 reads=[(X, c)], writes=[(sq, c)])
            k.op("pe", lambda e, c=c: e.matmul(pm[:, :], ones_bf[:, :], sq[:, c, :], start=(c == 0), stop=(c == 7)),
                 reads=[(sq, c), ones_bf], writes=[pm])
        k.op("act", lambda e: e.activation(out=rstd[:], in_=pm[:, :], func=AF.Sqrt, bias=epsc[:, 0:1], scale=1.0 / D),
             reads=[pm, epsc], writes=[rstd])
        k.op("dve", lambda e: e.reciprocal(out=rstd[:], in_=rstd[:]), reads=[rstd], writes=[rstd])
        for c in range(8):
            k.op("dve", lambda e, c=c: e.tensor_tensor(out=tmp[:, c % 2, :], in0=X[:, c, :], in1=rstd[:], op=ALU.mult),
                 reads=[(X, c), rstd], writes=[(tmp, c % 2)])
            if shf is not None:
                k.op("act", lambda e, c=c: e.activation(out=Hout[:, c, :], in_=tmp[:, c % 2, :], func=AF.Identity,
                                                         scale=scl(c), bias=shf(c)),
                     reads=[(tmp, c % 2), modT, A1, A2], writes=[(Hout, c)])
                if Hf is not None:
                    k.op("act", lambda e, c=c: e.activation(out=Hf[:, c, :], in_=tmp[:, c % 2, :], func=AF.Identity,
                                                             scale=scl(c), bias=shf(c)),
                         reads=[(tmp, c % 2), modT, A1, A2], writes=[(Hf, c)])
            else:
                k.op("act", lambda e, c=c: e.activation(out=Hout[:, c, :], in_=tmp[:, c % 2, :], func=AF.Identity, scale=scl(c)),
                     reads=[(tmp, c % 2), gv], writes=[(Hout, c)])

    xcur_v = xT_v
    xcur_b = xT_in

    for l in range(L):
        k.barrier()
        with ExitStack() as sA:
            Win = k.sb(sA, "Win", [128, 8, FM_COLS + TM_COLS], BF16)
            for kk in range(8):
                for h0 in range(0, FM_COLS + TM_COLS, 1376):
                    k.loadc(Win, Win[:, kk, h0:h0 + 1376], w_inR, w_inR[l, kk * 128:(kk + 1) * 128, h0:h0 + 1376], dkey=(kk, h0))
            Xs = [k.sb(sA, "Xa%d" % i, [128, 8, 512], F32) for i in range(1)]
            sq = k.sb(sA, "sqA", [128, 8, 512], BF16)
            rstd = k.sb(sA, "rstdA", [128, 512], F32)
            tmp = k.sb(sA, "tmpA", [128, 2, 512], F32)
            Hs = [k.sb(sA, "Ha%d" % i, [128, 8, 512], BF16) for i in range(1)]
            ZT = [k.sb(sA, "ZT%d" % i, [128, 29, 512], BF16) for i in range(1)]
            VT = [k.sb(sA, "VT%d" % i, [128, 4, TM_COLS], BF16) for i in range(2)]
            GN = k.sb(sA, "GN", [32, 512], F32)
            GN2 = [k.sb(sA, "GN2%d" % i, [32, 512], F32) for i in range(2)]
            for qi in range(NQ):
                qs = slice(qi * 512, (qi + 1) * 512)
                X = Xs[0]
                H = Hs[0]
                Z = ZT[0]
                V = VT[qi % 2]
                G2 = GN2[qi % 2]
                k.load(X, X[:], xcur_b, xcur_v[:, :, qs], skey=qi)
                norm_tile(X, H, lambda c: A1[:, l * 8 + c:l * 8 + c + 1], lambda c: mcol(l, 0, c), sq, rstd, tmp)
                if qi == 0 and l == 0:
                    k.dump("H0", H, H[:], [128, 8, 512], BF16)
                    k.dump("rstd0", rstd, rstd[:], [128, 512], F32)
                    k.dump("X0", X, X[:], [128, 8, 512], F32)
                for j in range(30):
                    w = 128 if j < 29 else 32
                    pm = psum()
                    for kk in range(8):
                        k.op("pe", lambda e, kk=kk, j=j, w=w, pm=pm: e.matmul(
                            pm[0:w, :], Win[:, kk, j * 128:j * 128 + w], H[:, kk, :], start=(kk == 0), stop=(kk == 7)),
                            reads=[Win, (H, kk)], writes=[pm])
                    if j < 4 or 8 <= j < 12:
                        k.op("act", lambda e, j=j, pm=pm: e.activation(out=Z[:, j, :], in_=pm[:, :], func=AF.Copy, scale=0.125),
                             reads=[pm], writes=[(Z, j)])
                    elif j < 13:
                        k.op("dve", lambda e, j=j, pm=pm: e.tensor_copy(out=Z[:, j, :], in_=pm[:, :]), reads=[pm], writes=[(Z, j)])
                    elif j < 29:
                        k.op("act", lambda e, j=j, pm=pm: e.activation(out=Z[:, j, :], in_=pm[:, :], func=AF.Sigmoid),
                             reads=[pm], writes=[(Z, j)])
                    else:
                        k.op("act", lambda e, pm=pm: e.activation(out=GN[:, :], in_=pm[0:32, :], func=AF.Sigmoid),
                             reads=[pm], writes=[GN])
                        k.op("act", lambda e: e.activation(out=G2[:, :], in_=GN[:, :], func=AF.Ln), reads=[GN], writes=[G2])
                for q4 in range(4):
                    pm = psum()
                    for kk in range(8):
                        k.op("pe", lambda e, kk=kk, q4=q4, pm=pm: e.matmul(
                            pm[:, 0:TM_COLS], H[:, kk, q4 * 128:(q4 + 1) * 128], Win[:, kk, FM_COLS:FM_COLS + TM_COLS],
                            start=(kk == 0), stop=(kk == 7)), reads=[Win, (H, kk)], writes=[pm])
                    k.op("dve", lambda e, q4=q4, pm=pm: e.tensor_copy(out=V[:, q4, :], in_=pm[:, 0:TM_COLS]),
                         reads=[pm], writes=[(V, q4)])
                k.store(ZFM, ZFM.t[:, :, qs].rearrange("j p t -> p j t"), Z, Z[:], dkey=qi)
                k.store(ZTM, ZTM.t[qs, :].rearrange("(q p) c -> p q c", p=128), V, V[:], dkey=qi)
                k.store(LNG, LNG.t[:, qs], G2, G2[:], dkey=qi)

        k.barrier()
        with ExitStack() as sC:
            KCMP = k.sb(sC, "KCMP", [128, 512], BF16)
            VCMP = k.sb(sC, "VCMP", [128, NCC, 2, 65], BF16)
            k.op("dve", lambda e: e.memset(KCMP[:], 0.0), writes=[KCMP])
            k.op("dve", lambda e: e.memset(VCMP[:], 0.0), writes=[VCMP])
            with ExitStack() as sB:
                RT = k.sb(sB, "RT", [128, T], BF16)
                W1d = k.sb(sB, "W1d", [128, 32, 256], BF16)
                posd = k.sb(sB, "posd", [128, 32], BF16)
                W2t = k.sb(sB, "W2t", [128, 2, 64], BF16)
                posb = k.sb(sB, "posb", [128, 1], F32)
                U = k.sb(sB, "U", [128, 512], F32)
                U2 = k.sb(sB, "U2", [128, 512], F32)
                SG = k.sb(sB, "SGB", [128, 512], F32)
                HT = k.sb(sB, "HT", [128, 2, 512], BF16)
                for kv in range(2):
                    k.load(RT, RT[:], ZFM, ZFM.t[4 + kv, :, :])
                    for half in range(2):
                        k.loadc(W1d, W1d[half * 64:(half + 1) * 64, :, :], w1R,
                                w1R[l, kv, :, :].rearrange("d (l h) -> d l h", h=256), dkey=half)
                        k.loadc(posd, posd[half * 64:(half + 1) * 64, :], posT, posT[l, kv, :, :], dkey=half)
                    k.loadc(W2t, W2t[:], w2, w2[l, kv, :, :].rearrange("(c p) d -> p c d", p=128))
                    for g in range(2):
                        gs = slice(64 * g, 64 * g + 64)
                        for hc in range(2):
                            pb = psum()
                            for ll in range(32):
                                k.op("pe", lambda e, ll=ll, hc=hc, gs=gs, pb=pb: e.matmul(
                                    pb[:, 0:1], W1d[gs, ll, hc * 128:(hc + 1) * 128], posd[gs, ll:ll + 1],
                                    start=(ll == 0), stop=(ll == 31)), reads=[W1d, posd], writes=[pb])
                            k.op("dve", lambda e, pb=pb: e.tensor_copy(out=posb[:], in_=pb[:, 0:1]), reads=[pb], writes=[posb])
                            pm = psum()
                            for ll in range(32):
                                k.op("pe", lambda e, ll=ll, hc=hc, gs=gs, pm=pm: e.matmul(
                                    pm[:, 0:NCMP], W1d[gs, ll, hc * 128:(hc + 1) * 128],
                                    RT[gs, ll:ll + 16 * (NCMP - 1) + 1:16],
                                    start=(ll == 0), stop=(ll == 31)), reads=[W1d, RT], writes=[pm])
                            k.op("act", lambda e, pm=pm: e.activation(out=U[:, 0:NCMP], in_=pm[:, 0:NCMP], func=AF.Identity, bias=posb[:, 0:1]),
                                 reads=[pm, posb], writes=[U])
                            k.op("dve", lambda e: e.tensor_tensor(out=U2[:, 0:NCMP], in0=U[:, 0:NCMP], in1=U[:, 0:NCMP], op=ALU.mult),
                                 reads=[U], writes=[U2])
                            k.op("dve", lambda e: e.tensor_scalar(out=U2[:, 0:NCMP], in0=U2[:, 0:NCMP], scalar1=0.044715, scalar2=1.0,
                                                                  op0=ALU.mult, op1=ALU.add), reads=[U2], writes=[U2])
                            k.op("dve", lambda e: e.tensor_tensor(out=U2[:, 0:NCMP], in0=U2[:, 0:NCMP], in1=U[:, 0:NCMP], op=ALU.mult),
                                 reads=[U2, U], writes=[U2])
                            k.op("act", lambda e: e.activation(out=SG[:, 0:NCMP], in_=U2[:, 0:NCMP], func=AF.Sigmoid, scale=1.5957691216057308),
                                 reads=[U2], writes=[SG])
                            k.op("dve", lambda e, hc=hc: e.tensor_tensor(out=HT[:, hc, 0:NCMP], in0=U[:, 0:NCMP], in1=SG[:, 0:NCMP], op=ALU.mult),
                                 reads=[U, SG], writes=[(HT, hc)])
                        if kv == 0:
                            pm = psum()
                            for hc in range(2):
                                k.op("pe", lambda e, hc=hc, pm=pm: e.matmul(pm[0:64, 0:NCMP], W2t[:, hc, :], HT[:, hc, 0:NCMP],
                                                                            start=(hc == 0), stop=(hc == 1)),
                                     reads=[W2t, (HT, hc)], writes=[pm])
                            k.op("dve", lambda e, gs=gs, pm=pm: e.tensor_copy(out=KCMP[gs, 0:NCMP], in_=pm[0:64, 0:NCMP]),
                                 reads=[pm], writes=[(KCMP, g)])
                        else:
                            for c in range(NCC):
                                n0 = c * 128
                                nn = min(128, NCMP - n0)
                                pm = psum()
                                for hc in range(2):
                                    k.op("pe", lambda e, hc=hc, pm=pm, n0=n0, nn=nn: e.matmul(
                                        pm[0:nn, 0:64], HT[:, hc, n0:n0 + nn], W2t[:, hc, :], start=(hc == 0), stop=(hc == 1)),
                                        reads=[W2t, (HT, hc)], writes=[pm])
                                k.op("dve", lambda e, pm=pm, c=c, g=g, nn=nn: e.tensor_copy(out=VCMP[0:nn, c, g, 0:64], in_=pm[0:nn, 0:64]),
                                     reads=[pm], writes=[(VCMP, (c, g))])
                                k.op("dve", lambda e, c=c, g=g, nn=nn: e.memset(VCMP[0:nn, c, g, 64:65], 1.0), writes=[(VCMP, (c, g, 1))])

            k.barrier()
            KSL = k.sb(sC, "KSL", [128, T], BF16)
            VSL = k.sb(sC, "VSL", [128, NKC, 2, 65], BF16)
            TABN = k.sb(sC, "TABN", [128, 8, 1024], BF16)
            EM = k.sb(sC, "EM", [128, T], BF16)
            WT = k.sb(sC, "WTm", [128, NCC, NB + 1], BF16)
            FT = k.sb(sC, "FT", [128, 2 * NB], F32)
            SELG = k.sb(sC, "SELG", [32, 24 * 64], F32)
            k.load(KSL, KSL[:], ZFM, ZFM.t[6, :, :])
            k.op("pool", lambda e: e.memset(VSL[:], 1.0), writes=[VSL])
            for g in range(2):
                k.load(VSL, VSL[:, :, g, 0:64], ZTM, ZTM.t[:, g * 64:(g + 1) * 64].rearrange("(c p) d -> p c d", p=128), dkey=g)
            k.loadc(TABN, TABN[:], tabN, tabN[:, :, :])
            k.loadc(EM, EM[0:NB, :], emat, emat[:, :])
            k.loadc(WT, WT[:], wmap, wmap.t.rearrange("(c p) j -> p c j", p=128))
            k.load(FT, FT[:], ftab, ftab[:, :])
            k.load(SELG, SELG[:], selg, selg[:, :])
            QTs = [k.sb(sC, "QT%d" % i, [128, 4, 512], BF16) for i in range(2)]
            LGs = [k.sb(sC, "LG%d" % i, [32, 512], F32) for i in range(2)]
            EC = k.sb(sC, "EC", [128, 4, NCC, 512], BF16)
            ETs = [k.sb(sC, "ET%d" % i, [128, 512], BF16) for i in range(4)]
            PENT = k.sb(sC, "PENT", [128, 2, 512], BF16)
            OCS = [k.sb(sC, "OCS%d" % i, [128, 4, 512], F32) for i in range(2)]
            drow = k.sb(sC, "drow", [128, 512], F32)
            lnrow = k.sb(sC, "lnrow", [128, 512], F32)
            EB = k.sb(sC, "EB", [64, 512], F32)
            TMPF = k.sb(sC, "TMPF", [128, 512], F32)
            acc = k.sb(sC, "acc", [128, 128], F32)
            sc2 = k.sb(sC, "sc2", [128, 128], F32)
            m8a = k.sb(sC, "m8a", [128, 8], F32)
            m8b = k.sb(sC, "m8b", [128, 8], F32)
            rden = k.sb(sC, "rden", [128, 1], F32)
            pen = k.sb(sC, "pen", [128, 128], F32)

            def finalize(Ops, dest, g, hg, first, LGT, r=None, sink_ap=None, out_bf=False):
                gs = slice(64 * g, 64 * g + 64)
                k.op("dve", lambda e: e.tensor_scalar_max(out=drow[64:65, :], in0=Ops[64:65, :], scalar1=1e-30),
                     reads=[Ops], writes=[drow])
                if sink_ap is None:
                    k.op("act", lambda e: e.activation(out=lnrow[64:65, :], in_=drow[64:65, :], func=AF.Ln), reads=[drow], writes=[lnrow])
                else:
                    k.op("act", lambda e: e.activation(out=lnrow[64:65, :], in_=drow[64:65, :], func=AF.Ln, bias=sink_ap),
                         reads=[drow], writes=[lnrow])
                pb = psum()
                if r is not None:
                    k.op("pe", lambda e: e.matmul(pb[0:64, :], SELG[0:32, r * 64:(r + 1) * 64], LGT[0:32, :], start=True, stop=False),
                         reads=[SELG, LGT], writes=[pb])
                k.op("pe", lambda e: e.matmul(pb[0:64, :], neg1_f[64:65, 0:64], lnrow[64:65, :], start=(r is None), stop=True),
                     reads=[neg1_f, lnrow], writes=[pb])
                k.op("act", lambda e: e.activation(out=EB[:, :], in_=pb[0:64, :], func=AF.Exp), reads=[pb], writes=[EB])
                if first:
                    k.op("dve", lambda e: e.tensor_tensor(out=dest[gs, hg, :], in0=Ops[0:64, :], in1=EB[:, :], op=ALU.mult),
                         reads=[Ops, EB], writes=[(dest, (g, hg))])
                else:
                    k.op("dve", lambda e: e.tensor_tensor(out=TMPF[gs, :], in0=Ops[0:64, :], in1=EB[:, :], op=ALU.mult),
                         reads=[Ops, EB], writes=[TMPF])
                    k.op("pool", lambda e: e.tensor_tensor(out=dest[gs, hg, :], in0=dest[gs, hg, :], in1=TMPF[gs, :], op=ALU.add),
                         reads=[TMPF, (dest, (g, hg))], writes=[(dest, (g, hg))])

            def attn_chunks(chunks, Kt, Vt, Qt, g, hg, extra, bias_far):
                gs = slice(64 * g, 64 * g + 64)
                Ops = psum_acc()
                pend = []
                n = len(chunks)
                for i, (kc, dl) in enumerate(chunks):
                    pm = psum()
                    ex = extra(kc, dl)
                    k.op("pe", lambda e, pm=pm, kc=kc, ex=ex: e.matmul(pm[:, :], Kt[gs, kc * 128:(kc + 1) * 128], Qt[gs, hg, :],
                                                                          start=True, stop=(len(ex) == 0)),
                         reads=[Kt, Qt], writes=[pm])
                    for xi, (la, ra, bufs) in enumerate(ex):
                        k.op("pe", lambda e, pm=pm, la=la, ra=ra, xi=xi, ex=ex: e.matmul(pm[:, :], la, ra, start=False, stop=(xi == len(ex) - 1)),
                             reads=bufs, writes=[pm])
                    ET = ETs[i % 4]
                    bf = bias_far(dl)
                    if bf is None:
                        k.op("act", lambda e, pm=pm, ET=ET: e.activation(out=ET[:, :], in_=pm[:, :], func=AF.Exp), reads=[pm], writes=[ET])
                    else:
                        k.op("act", lambda e, pm=pm, ET=ET, bf=bf: e.activation(out=ET[:, :], in_=pm[:, :], func=AF.Exp, bias=bf),
                             reads=[pm, b31t], writes=[ET])
                    pend.append((kc, ET, i))
                    if len(pend) > 2:
                        kc2, ET2, i2 = pend.pop(0)
                        k.op("pe", lambda e, kc2=kc2, ET2=ET2, i2=i2: e.matmul(Ops[0:65, :], Vt[:, kc2, g, 0:65], ET2[:, :],
                                                                                 start=(i2 == 0), stop=(i2 == n - 1)),
                             reads=[Vt, ET2], writes=[Ops])
                for kc2, ET2, i2 in pend:
                    k.op("pe", lambda e, kc2=kc2, ET2=ET2, i2=i2: e.matmul(Ops[0:65, :], Vt[:, kc2, g, 0:65], ET2[:, :],
                                                                             start=(i2 == 0), stop=(i2 == n - 1)),
                         reads=[Vt, ET2], writes=[Ops])
                return Ops

            for qi in range(NQ):
                q0 = qi * 512
                qs = slice(q0, q0 + 512)
                QT = QTs[qi % 2]
                LGT = LGs[qi % 2]
                OC = OCS[qi % 2]
                k.load(QT, QT[:], ZFM, ZFM.t[0:4, :, qs].rearrange("j p t -> p j t"), skey=qi)
                k.load(LGT, LGT[:], LNG, LNG.t[:, qs], skey=qi)
                NCV = min(NCC, (q0 + 480) // 2048 + 1)
                for g in range(2):
                    gs = slice(64 * g, 64 * g + 64)
                    for hg in range(4):
                        for c in range(NCV):
                            pm = psum()
                            k.op("pe", lambda e, pm=pm, c=c: e.matmul(pm[:, :], KCMP[gs, c * 128:(c + 1) * 128], QT[gs, hg, :], start=True, stop=True),
                                 reads=[KCMP, QT], writes=[pm])
                            k.op("act", lambda e, pm=pm, c=c: e.activation(out=EC[:, hg, c, :], in_=pm[:, :], func=AF.Exp),
                                 reads=[pm], writes=[(EC, (hg, c))])
                            if 2048 * c + 2063 > q0:
                                base = q0 - 2048 * c - 31
                                k.op("pool", lambda e, c=c, base=base: e.affine_select(
                                    out=EC[:, hg, c, :], in_=EC[:, hg, c, :], pattern=[[1, 512]], compare_op=ALU.is_ge,
                                    fill=0.0, base=base, channel_multiplier=-16), reads=[(EC, (hg, c))], writes=[(EC, (hg, c))])
                        Ops = psum_acc()
                        for c in range(NCV):
                            k.op("pe", lambda e, c=c: e.matmul(Ops[0:65, :], VCMP[:, c, g, 0:65], EC[:, hg, c, :], start=(c == 0), stop=(c == NCV - 1)),
                                 reads=[VCMP, (EC, (hg, c))], writes=[Ops])
                        finalize(Ops, OC, g, hg, True, LGT, r=0 * 8 + g * 4 + hg)
                    for q4 in range(4):
                        t128 = qi * 4 + q4
                        for hg in range(4):
                            pi = psum()
                            for c in range(NCV):
                                k.op("pe", lambda e, c=c, pi=pi: e.matmul(pi[:, 0:NB + 1], EC[:, hg, c, q4 * 128:(q4 + 1) * 128], WT[:, c, :],
                                                                          start=(c == 0), stop=(c == NCV - 1)),
                                     reads=[WT, (EC, (hg, c))], writes=[pi])
                            k.op("dve", lambda e, pi=pi: e.tensor_scalar_max(out=rden[:], in0=pi[:, NB:NB + 1], scalar1=1e-30), reads=[pi], writes=[rden])
                            k.op("dve", lambda e: e.reciprocal(out=rden[:], in_=rden[:]), reads=[rden], writes=[rden])
                            if hg == 0:
                                k.op("dve", lambda e, pi=pi: e.tensor_scalar(out=acc[:, 0:NB], in0=pi[:, 0:NB], scalar1=rden[:, 0:1], scalar2=None, op0=ALU.mult),
                                     reads=[pi, rden], writes=[acc])
                            else:
                                k.op("dve", lambda e, pi=pi: e.scalar_tensor_tensor(out=acc[:, 0:NB], in0=pi[:, 0:NB], scalar=rden[:, 0:1], in1=acc[:, 0:NB],
                                                                                    op0=ALU.mult, op1=ALU.add), reads=[pi, rden, acc], writes=[acc])
                        fo = NB - 2 * t128
                        k.op("dve", lambda e, fo=fo: e.tensor_tensor(out=acc[:, 0:NB], in0=acc[:, 0:NB], in1=FT[:, fo:fo + NB], op=ALU.add),
                             reads=[acc, FT], writes=[acc])
                        k.op("dve", lambda e: e.tensor_scalar_add(out=acc[:, 0:1], in0=acc[:, 0:1], scalar1=100.0), reads=[acc], writes=[acc])
                        k.op("dve", lambda e: e.max(out=m8a[:], in_=acc[:, 0:NB]), reads=[acc], writes=[m8a])
                        k.op("dve", lambda e: e.match_replace(out=sc2[:, 0:NB], in_to_replace=m8a[:], in_values=acc[:, 0:NB], imm_value=-1e30),
                             reads=[acc, m8a], writes=[sc2])
                        k.op("dve", lambda e: e.max(out=m8b[:], in_=sc2[:, 0:NB]), reads=[sc2], writes=[m8b])
                        k.op("dve", lambda e: e.tensor_scalar(out=pen[:, 0:NB], in0=acc[:, 0:NB], scalar1=m8b[:, 7:8], scalar2=1.0,
                                                              op0=ALU.is_ge, op1=ALU.subtract), reads=[acc, m8b], writes=[pen])
                        pt = psum()
                        k.op("pe", lambda e, pt=pt: e.transpose(pt[0:NB, 0:128], pen[:, 0:NB], ident_f[:, :]), reads=[pen, ident_f], writes=[pt])
                        k.op("act", lambda e, pt=pt, q4=q4: e.activation(out=PENT[0:NB, g, q4 * 128:(q4 + 1) * 128], in_=pt[0:NB, 0:128],
                                                                          func=AF.Copy, scale=-NEG),
                             reads=[pt], writes=[(PENT, (g, q4))])
                    for hg in range(4):
                        h = g * 4 + hg
                        chunks = [(kc, q0 - 128 * kc) for kc in range(0, 4 * qi + 4)]

                        def extra(kc, dl, h=h, g=g):
                            ex = [(EM[0:NB, kc * 128:(kc + 1) * 128], PENT[0:NB, g, :], [EM, PENT])]
                            if dl <= 128:
                                ex.append((ident_bf[:, :], TABN[:, h, dl + 384:dl + 384 + 512], [ident_bf, TABN]))
                            return ex

                        Ops = attn_chunks(chunks, KSL, VSL, QT, g, hg, extra,
                                          lambda dl, h=h: (b31t[:, h:h + 1] if dl >= 256 else None))
                        finalize(Ops, OC, g, hg, False, LGT, r=1 * 8 + g * 4 + hg)
                k.store(OCSD, OCSD.t[:, :, qs].rearrange("j p t -> p j t"), OC, OC[:], dkey=qi)

        k.barrier()
        with ExitStack() as sD:
            KW = k.sb(sD, "KW", [128, T], BF16)
            VW = k.sb(sD, "VW", [128, NKC, 2, 65], BF16)
            TABN = k.sb(sD, "TABN2", [128, 8, 1024], BF16)
            MW = k.sb(sD, "MW", [128, 1408], BF16)
            SELG = k.sb(sD, "SELG2", [32, 24 * 64], F32)
            k.load(KW, KW[:], ZFM, ZFM.t[7, :, :])
            k.op("pool", lambda e: e.memset(VW[:], 1.0), writes=[VW])
            for g in range(2):
                k.load(VW, VW[:, :, g, 0:64], ZTM, ZTM.t[:, 128 + g * 64:128 + (g + 1) * 64].rearrange("(c p) d -> p c d", p=128), dkey=g)
            k.loadc(TABN, TABN[:], tabN, tabN[:, :, :])
            k.loadc(MW, MW[:], mskW, mskW[:, :])
            k.load(SELG, SELG[:], selg, selg[:, :])
            QTs = [k.sb(sD, "QTw%d" % i, [128, 4, 512], BF16) for i in range(2)]
            LGs = [k.sb(sD, "LGw%d" % i, [32, 512], F32) for i in range(2)]
            OCs = [k.sb(sD, "OCw%d" % i, [128, 4, 512], F32) for i in range(2)]
            ONs = [k.sb(sD, "ONs%d" % i, [128, 4, 512], BF16) for i in range(2)]
            ETs = [k.sb(sD, "ETw%d" % i, [128, 512], BF16) for i in range(4)]
            drow = k.sb(sD, "droww", [128, 512], F32)
            lnrow = k.sb(sD, "lnroww", [128, 512], F32)
            EB = k.sb(sD, "EBw", [64, 512], F32)
            TMPF = k.sb(sD, "TMPFw", [128, 512], F32)
            for qi in range(NQ):
                q0 = qi * 512
                qs = slice(q0, q0 + 512)
                QT, LGT, OC, ON = QTs[qi % 2], LGs[qi % 2], OCs[qi % 2], ONs[qi % 2]
                k.load(QT, QT[:], ZFM, ZFM.t[0:4, :, qs].rearrange("j p t -> p j t"), skey=qi)
                k.load(LGT, LGT[:], LNG, LNG.t[:, qs], skey=qi)
                k.load(OC, OC[:], OCSD, OCSD.t[:, :, qs].rearrange("j p t -> p j t"), skey=qi)
                for g in range(2):
                    for hg in range(4):
                        h = g * 4 + hg
                        chunks = [(kc, q0 - 128 * kc) for kc in range(max(0, 4 * qi - 4), 4 * qi + 4)]

                        def extra(kc, dl, h=h):
                            ex = []
                            if dl <= 128:
                                ex.append((ident_bf[:, :], TABN[:, h, dl + 384:dl + 384 + 512], [ident_bf, TABN]))
                            if dl >= 128:
                                ex.append((ident_bf[:, :], MW[:, dl + 384:dl + 384 + 512], [ident_bf, MW]))
                            return ex

                        Ops = attn_chunks(chunks, KW, VW, QT, g, hg, extra, lambda dl, h=h: (b31t[:, h:h + 1] if dl >= 256 else None))
                        finalize(Ops, OC, g, hg, False, LGT, r=2 * 8 + g * 4 + hg)
                k.op("act", lambda e: e.copy(out=ON[:], in_=OC[:]), reads=[OC], writes=[ON])
                k.store(ONB, ONB.t[:, :, qs].rearrange("j p t -> p j t"), ON, ON[:], dkey=qi)

        k.barrier()
        with ExitStack() as sD:
            KS = k.sb(sD, "KS", [128, T], BF16)
            VS = k.sb(sD, "VS", [128, NKC, 2, 65], BF16)
            TABS = k.sb(sD, "TABS", [128, 8, 1024], BF16)
            MS = k.sb(sD, "MS", [128, 1024], BF16)
            WB = k.sb(sD, "WB", [128, 2, 4, D], BF16)
            WO = k.sb(sD, "WO", [128, 8, D], BF16)
            snk = k.sb(sD, "snk", [128, 8], F32)
            esnk = k.sb(sD, "esnk", [128, 8], F32)
            k.load(KS, KS[:], ZFM, ZFM.t[12, :, :])
            k.op("pool", lambda e: e.memset(VS[:], 1.0), writes=[VS])
            for g in range(2):
                k.load(VS, VS[:, :, g, 0:64], ZTM, ZTM.t[:, 256 + g * 64:256 + (g + 1) * 64].rearrange("(c p) d -> p c d", p=128), dkey=g)
            k.loadc(TABS, TABS[:], tabS, tabS[:, :, :])
            k.loadc(MS, MS[:], mskS, mskS[:, :])
            for b in range(2):
                for hg in range(4):
                    k.loadc(WB, WB[:, b, hg, :], w_brR, w_brR[l, b, :, hg * D:(hg + 1) * D], dkey=(b, hg))
            for kk in range(8):
                k.loadc(WO, WO[:, kk, :], w_out, w_out[l, kk * 128:(kk + 1) * 128, :], dkey=kk)
            k.load(snk, snk[:], sinksB, sinksB[l, :, :])
            k.op("act", lambda e: e.activation(out=esnk[:], in_=snk[:], func=AF.Exp), reads=[snk], writes=[esnk])
            QS = k.sb(sD, "QSs", [128, 4, 512], BF16)
            GM = k.sb(sD, "GM", [128, 16, 512], BF16)
            X = k.sb(sD, "Xc", [128, 8, 512], F32)
            ONb = k.sb(sD, "ONb", [128, 4, 512], BF16)
            OSb = k.sb(sD, "OSb", [128, 4, 512], BF16)
            MG = k.sb(sD, "MG", [128, 8, 512], BF16)
            M1 = k.sb(sD, "M1", [128, 512], F32)
            M2 = k.sb(sD, "M2", [128, 512], F32)
            ETs = [k.sb(sD, "ETs%d" % i, [128, 512], BF16) for i in range(4)]
            drow = k.sb(sD, "drows", [128, 512], F32)
            lnrow = k.sb(sD, "lnrows", [128, 512], F32)
            EB = k.sb(sD, "EBs", [64, 512], F32)
            TMPF = k.sb(sD, "TMPFs", [128, 512], F32)
            for qi in range(NQ):
                q0 = qi * 512
                qs = slice(q0, q0 + 512)
                k.load(QS, QS[:], ZFM, ZFM.t[8:12, :, qs].rearrange("j p t -> p j t"), skey=qi)
                k.load(GM, GM[:], ZFM, ZFM.t[13:29, :, qs].rearrange("j p t -> p j t"), skey=qi)
                k.load(ONb, ONb[:], ONB, ONB.t[:, :, qs].rearrange("j p t -> p j t"), skey=qi)
                k.load(X, X[:], xcur_b, xcur_v[:, :, qs], skey=qi)
                for g in range(2):
                    for hg in range(4):
                        h = g * 4 + hg
                        chunks = [(kc, q0 - 128 * kc) for kc in range(max(0, 4 * qi - 1), 4 * qi + 4)]

                        def extra(kc, dl, h=h):
                            ex = [(ident_bf[:, :], TABS[:, h, dl + 384:dl + 384 + 512], [ident_bf, TABS])]
                            if dl >= -256:
                                ex.append((ident_bf[:, :], MS[:, dl + 384:dl + 384 + 512], [ident_bf, MS]))
                            return ex

                        Ops = attn_chunks(chunks, KS, VS, QS, g, hg, extra, lambda dl: None)
                        finalize(Ops, OSb, g, hg, True, None, r=None, sink_ap=esnk[64:65, h:h + 1])
                for cc in range(8):
                    pa = psum()
                    pb = psum()
                    for hg in range(4):
                        k.op("pe", lambda e, hg=hg, cc=cc, pa=pa: e.matmul(pa[:, :], WB[:, 0, hg, cc * 128:(cc + 1) * 128], ONb[:, hg, :],
                                                                           start=(hg == 0), stop=(hg == 3)), reads=[WB, ONb], writes=[pa])
                    for hg in range(4):
                        k.op("pe", lambda e, hg=hg, cc=cc, pb=pb: e.matmul(pb[:, :], WB[:, 1, hg, cc * 128:(cc + 1) * 128], OSb[:, hg, :],
                                                                           start=(hg == 0), stop=(hg == 3)), reads=[WB, OSb], writes=[pb])
                    k.op("dve", lambda e, cc=cc, pa=pa: e.tensor_tensor(out=M1[:, :], in0=pa[:, :], in1=GM[:, cc, :], op=ALU.mult),
                         reads=[pa, GM], writes=[M1])
                    k.op("dve", lambda e, cc=cc, pb=pb: e.tensor_tensor(out=M2[:, :], in0=pb[:, :], in1=GM[:, 8 + cc, :], op=ALU.mult),
                         reads=[pb, GM], writes=[M2])
                    k.op("pool", lambda e, cc=cc: e.tensor_tensor(out=MG[:, cc, :], in0=M1[:, :], in1=M2[:, :], op=ALU.add),
                         reads=[M1, M2], writes=[(MG, cc)])
                for co in range(8):
                    pm = psum()
                    for cc in range(8):
                        k.op("pe", lambda e, cc=cc, co=co, pm=pm: e.matmul(pm[:, :], WO[:, cc, co * 128:(co + 1) * 128], MG[:, cc, :],
                                                                           start=(cc == 0), stop=(cc == 7)), reads=[WO, (MG, cc)], writes=[pm])
                    k.op("dve", lambda e, co=co, pm=pm: e.scalar_tensor_tensor(out=X[:, co, :], in0=pm[:, :], scalar=mcol(l, 2, co), in1=X[:, co, :],
                                                                                op0=ALU.mult, op1=ALU.add), reads=[pm, modT, (X, co)], writes=[(X, co)])
                k.store(xresA, xresA.t[:, :, qs].rearrange("c p t -> p c t"), X, X[:], dkey=qi)

        k.barrier()
        with ExitStack() as sE:
            WU = [k.sb(sE, "WU%d" % i, [128, 8, 2 * D], BF16) for i in range(2)]
            WDn = [k.sb(sE, "WD%d" % i, [128, 8, D], BF16) for i in range(2)]
            BU = [k.sb(sE, "BU%d" % i, [128, 16], F32) for i in range(2)]
            BD = [k.sb(sE, "BD%d" % i, [1, D], BF16) for i in range(2)]
            WR = k.sb(sE, "WR", [128, 8, NEXP], F32)
            BR = k.sb(sE, "BR", [1, NEXP], F32)
            H2 = k.sb(sE, "H2", [128, 8, 1024], BF16)
            YACC = k.sb(sE, "YACC", [128, 8, D], F32)
            GATES = k.sb(sE, "GATES", [128, 8, NEXP], F32)
            k.load(WR, WR[:], w_router, w_router[l, :, :].rearrange("(k p) e -> p k e", p=128))
            k.load(BR, BR[:], b_router, b_router[l, :, :])
            for gi in range(NG):
                t0 = gi * 1024
                k.barrier()
                with ExitStack() as sP:
                    XT = k.sb(sP, "XTm", [128, 8, 512], F32)
                    HF = XT
                    sq = k.sb(sP, "sqM", [128, 8, 512], BF16)
                    rstd = k.sb(sP, "rstdM", [128, 512], F32)
                    tmp = k.sb(sP, "tmpM", [128, 2, 512], F32)
                    LGm = k.sb(sP, "LGm", [128, NEXP], F32)
                    Pm = k.sb(sP, "Pm", [128, NEXP], F32)
                    Mk = k.sb(sP, "Mk", [128, NEXP], F32)
                    m8 = k.sb(sP, "m8", [128, 8], F32)
                    e4 = k.sb(sP, "e4", [128, 4], F32)
                    nmx = k.sb(sP, "nmx", [128, 1], F32)
                    den = k.sb(sP, "den", [128, 1], F32)
                    for hf in range(2):
                        qs = slice(t0 + hf * 512, t0 + hf * 512 + 512)
                        k.load(XT, XT[:], xresA, xresA.t[:, :, qs].rearrange("c p t -> p c t"), skey=(t0 + hf * 512) // 512)
                        H2half = _HalfView(H2, hf)
                        norm_tile(XT, H2half, lambda c: A2[:, l * 8 + c:l * 8 + c + 1], lambda c: mcol(l, 3, c), sq, rstd, tmp, Hf=HF)
                        for q4 in range(4):
                            sub = hf * 4 + q4
                            pm = psum()
                            for kk in range(8):
                                k.op("pe", lambda e, kk=kk, q4=q4, pm=pm: e.matmul(pm[:, 0:NEXP], HF[:, kk, q4 * 128:(q4 + 1) * 128], WR[:, kk, :],
                                                                                   start=(kk == 0), stop=False), reads=[(HF, kk), WR], writes=[pm])
                            k.op("pe", lambda e, pm=pm: e.matmul(pm[:, 0:NEXP], ones_f[0:1, 0:128], BR[0:1, :], start=False, stop=True),
                                 reads=[ones_f, BR], writes=[pm])
                            k.op("dve", lambda e, pm=pm: e.tensor_copy(out=LGm[:], in_=pm[:, 0:NEXP]), reads=[pm], writes=[LGm])
                            k.op("dve", lambda e: e.max(out=m8[:], in_=LGm[:]), reads=[LGm], writes=[m8])
                            k.op("dve", lambda e: e.tensor_scalar(out=nmx[:], in0=m8[:, 0:1], scalar1=-1.0, scalar2=None, op0=ALU.mult), reads=[m8], writes=[nmx])
                            k.op("act", lambda e: e.activation(out=e4[:], in_=m8[:, 0:4], func=AF.Exp, bias=nmx[:, 0:1]), reads=[m8, nmx], writes=[e4])
                            k.op("dve", lambda e: e.reduce_sum(out=den[:], in_=e4[:], axis=mybir.AxisListType.X), reads=[e4], writes=[den])
                            k.op("dve", lambda e: e.reciprocal(out=den[:], in_=den[:]), reads=[den], writes=[den])
                            k.op("act", lambda e: e.activation(out=Pm[:], in_=LGm[:], func=AF.Exp, bias=nmx[:, 0:1]), reads=[LGm, nmx], writes=[Pm])
                            k.op("dve", lambda e: e.tensor_scalar(out=Mk[:], in0=LGm[:], scalar1=m8[:, 3:4], scalar2=None, op0=ALU.is_ge), reads=[LGm, m8], writes=[Mk])
                            k.op("dve", lambda e, sub=sub: e.scalar_tensor_tensor(out=GATES[:, sub, :], in0=Pm[:], scalar=den[:, 0:1], in1=Mk[:],
                                                                                   op0=ALU.mult, op1=ALU.mult), reads=[Pm, den, Mk], writes=[(GATES, sub)])
                k.barrier()
                with ExitStack() as sX:
                    STG = [k.sb(sX, "STG%d" % i, [128, 2 * D], F32) for i in range(2)]
                    G1 = [k.sb(sX, "G1%d" % i, [128, 512], F32) for i in range(2)]
                    S1 = [k.sb(sX, "S1%d" % i, [128, 512], F32) for i in range(2)]
                    L1 = [k.sb(sX, "L1%d" % i, [128, 512], F32) for i in range(2)]
                    HID = k.sb(sX, "HID", [128, 8, 1024], BF16)
                    sti = [0]

                    def emit_load(ex, chunk):
                        st = STG[sti[0] % 2]
                        sti[0] += 1
                        if chunk == 0:
                            k.load(BU[ex % 2], BU[ex % 2][:], b_upR, b_upR[l, ex, :, :])
                            k.loadc(BD[ex % 2], BD[ex % 2][:], b_down, b_down[l, ex, :, :])
                        if chunk < 8:
                            kk = chunk
                            wu = WU[ex % 2]
                            k.load(st, st[:, :], w_upR, w_upR[l, ex, kk * 128:(kk + 1) * 128, :])
                            k.op("act", lambda e: e.copy(out=wu[:, kk, :], in_=st[:, :]), reads=[st], writes=[(wu, kk)])
                        else:
                            kk = chunk - 8
                            wd = WDn[ex % 2]
                            k.load(st, st[:, 0:D], w_down, w_down[l, ex, kk * 128:(kk + 1) * 128, :])
                            k.op("pool", lambda e: e.tensor_copy(out=wd[:, kk, :], in_=st[:, 0:D]), reads=[st], writes=[(wd, kk)])

                    for ch in range(16):
                        emit_load(0, ch)
                    for ex in range(NEXP):
                        wu, wd, bu, bd = WU[ex % 2], WDn[ex % 2], BU[ex % 2], BD[ex % 2]
                        for hf in range(2):
                            hs = slice(hf * 512, hf * 512 + 512)
                            for jc in range(8):
                                pg = psum()
                                pl = psum()
                                g1, s1, l1 = G1[jc % 2], S1[jc % 2], L1[jc % 2]
                                for kk in range(8):
                                    k.op("pe", lambda e, kk=kk, jc=jc, pg=pg: e.matmul(pg[:, :], wu[:, kk, jc * 128:(jc + 1) * 128], H2[:, kk, hs],
                                                                                       start=(kk == 0), stop=(kk == 7)), reads=[(wu, kk), H2], writes=[pg])
                                for kk in range(8):
                                    k.op("pe", lambda e, kk=kk, jc=jc, pl=pl: e.matmul(pl[:, :], wu[:, kk, D + jc * 128:D + (jc + 1) * 128], H2[:, kk, hs],
                                                                                       start=(kk == 0), stop=(kk == 7)), reads=[(wu, kk), H2], writes=[pl])
                                k.op("dve", lambda e, jc=jc, pg=pg, g1=g1: e.tensor_scalar(out=g1[:], in0=pg[:, :], scalar1=bu[:, jc:jc + 1], scalar2=7.0,
                                                                                           op0=ALU.add, op1=ALU.min), reads=[pg, bu], writes=[g1])
                                k.op("act", lambda e, g1=g1, s1=s1: e.activation(out=s1[:], in_=g1[:], func=AF.Sigmoid, scale=1.702), reads=[g1], writes=[s1])
                                k.op("dve", lambda e, jc=jc, pl=pl, l1=l1: e.tensor_scalar(out=l1[:], in0=pl[:, :], scalar1=bu[:, 8 + jc:9 + jc], scalar2=7.0,
                                                                                           op0=ALU.add, op1=ALU.min), reads=[pl, bu], writes=[l1])
                                k.op("pool", lambda e, l1=l1: e.tensor_scalar(out=l1[:], in0=l1[:], scalar1=-7.0, scalar2=1.0, op0=ALU.max, op1=ALU.add),
                                     reads=[l1], writes=[l1])
                                k.op("pool", lambda e, g1=g1, s1=s1: e.tensor_tensor(out=g1[:], in0=g1[:], in1=s1[:], op=ALU.mult), reads=[g1, s1], writes=[g1])
                                k.op("dve", lambda e, jc=jc, g1=g1, l1=l1, hs=hs: e.tensor_tensor(out=HID[:, jc, hs], in0=g1[:], in1=l1[:], op=ALU.mult),
                                     reads=[g1, l1], writes=[(HID, (jc, hf))])
                                if ex + 1 < NEXP:
                                    emit_load(ex + 1, hf * 8 + jc)
                        for hf in range(2):
                            for q4 in range(4):
                                sub = hf * 4 + q4
                                for ch in range(2):
                                    py = psum()
                                    for jc in range(8):
                                        k.op("pe", lambda e, jc=jc, q4=q4, ch=ch, py=py, hf=hf: e.matmul(
                                            py[:, :], HID[:, jc, hf * 512 + q4 * 128:hf * 512 + (q4 + 1) * 128], wd[:, jc, ch * 512:(ch + 1) * 512],
                                            start=(jc == 0), stop=False), reads=[(HID, (jc, hf)), (wd, jc)], writes=[py])
                                    k.op("pe", lambda e, ch=ch, py=py: e.matmul(py[:, :], ones_bf[0:1, 0:128], bd[0:1, ch * 512:(ch + 1) * 512], start=False, stop=True),
                                         reads=[ones_bf, bd], writes=[py])
                                    if ex == 0:
                                        k.op("dve", lambda e, sub=sub, ch=ch, py=py: e.tensor_scalar(out=YACC[:, sub, ch * 512:(ch + 1) * 512], in0=py[:, :],
                                                                                                     scalar1=GATES[:, sub, ex:ex + 1], scalar2=None, op0=ALU.mult),
                                             reads=[py, (GATES, sub)], writes=[(YACC, (sub, ch))])
                                    else:
                                        k.op("dve", lambda e, sub=sub, ch=ch, py=py, ex=ex: e.scalar_tensor_tensor(
                                            out=YACC[:, sub, ch * 512:(ch + 1) * 512], in0=py[:, :], scalar=GATES[:, sub, ex:ex + 1],
                                            in1=YACC[:, sub, ch * 512:(ch + 1) * 512], op0=ALU.mult, op1=ALU.add),
                                            reads=[py, (GATES, sub), (YACC, (sub, ch))], writes=[(YACC, (sub, ch))])
                k.barrier()
                with ExitStack() as sQ:
                    XTs = [k.sb(sQ, "XTe%d" % i, [128, 8, 512], F32) for i in range(2)]
                    for hf in range(2):
                        XT = XTs[hf]
                        qs = slice(t0 + hf * 512, t0 + hf * 512 + 512)
                        k.load(XT, XT[:], xresA, xresA.t[:, :, qs].rearrange("c p t -> p c t"), skey=(t0 + hf * 512) // 512)
                        for cc in range(8):
                            pm = psum()
                            for q4 in range(4):
                                sub = hf * 4 + q4
                                k.op("pe", lambda e, q4=q4, sub=sub, cc=cc, pm=pm: e.transpose(pm[:, q4 * 128:(q4 + 1) * 128], YACC[:, sub, cc * 128:(cc + 1) * 128], ident_f[:, :]),
                                     reads=[YACC, ident_f], writes=[pm])
                            k.op("dve", lambda e, cc=cc, pm=pm, XT=XT: e.scalar_tensor_tensor(out=XT[:, cc, :], in0=pm[:, :], scalar=mcol(l, 5, cc), in1=XT[:, cc, :],
                                                                                        op0=ALU.mult, op1=ALU.add), reads=[pm, modT, (XT, cc)], writes=[(XT, cc)])
                        k.store(xresB, xresB.t[:, :, qs].rearrange("c p t -> p c t"), XT, XT[:], dkey=(t0 + hf * 512) // 512)
        xcur_b = xresB
        xcur_v = xresB.t.rearrange("c p t -> p c t")

    k.barrier()
    with ExitStack() as sF:
        Xs = [k.sb(sF, "Xf%d" % i, [128, 8, 512], F32) for i in range(2)]
        Os = [k.sb(sF, "Of%d" % i, [128, 8, 512], F32) for i in range(2)]
        sq = k.sb(sF, "sqF", [128, 8, 512], BF16)
        rstd = k.sb(sF, "rstdF", [128, 512], F32)
        tmp = k.sb(sF, "tmpF", [128, 2, 512], F32)
        for qi in range(NQ):
            qs = slice(qi * 512, (qi + 1) * 512)
            X, O = Xs[qi % 2], Os[qi % 2]
            k.load(X, X[:], xcur_b, xcur_v[:, :, qs], skey=qi)
            norm_tile(X, O, lambda c: gv[:, 2 * L * 8 + c:2 * L * 8 + c + 1], None, sq, rstd, tmp)
            k.store(outT, outT_v[:, :, qs], O, O[:], dkey=qi)
        k.wait_all("sp", [outT])
    es.close()
    return nc, k


class _HalfView:
    def __init__(self, base, hf):
        self.base = base
        self.hf = hf
        self.name = base.name
        self.regs = base.regs
        self.dsem = None

    def __getitem__(self, idx):
        p, c, t = idx
        assert t == slice(None, None, None)
        return self.base.t[p, c, self.hf * 512:(self.hf + 1) * 512]


def prep_shared(inp, T, L):
    f = np.float32
    NB = T // 64
    NCMP = T // 16 - 1
    NCC = (NCMP + 127) // 128
    sh = {}
    sh["w_ada"] = np.ascontiguousarray(inp["w_ada"][:L], f)
    sh["b_adaT"] = np.ascontiguousarray(inp["b_ada"][:L].reshape(L, 48, 128).transpose(0, 2, 1), f)
    gl = [inp["g_mix"][l] for l in range(L)] + [inp["g_ffn"][l] for l in range(L)] + [inp["g_final"]]
    sh["gvec"] = np.ascontiguousarray(np.concatenate([np.asarray(v, f).reshape(8, 128).T for v in gl], axis=1), f)
    w_in = np.asarray(inp["w_in"][:L], f)
    o_qn, o_kv, o_gn, o_qs, o_kvs, o_gm = 0, 512, 1280, 1304, 1816, 2072
    cols = []
    for hg in range(4):
        for g in range(2):
            h = g * 4 + hg
            cols += list(range(o_qn + h * 64, o_qn + (h + 1) * 64))
    cols += list(range(o_kv + 0, o_kv + 128))
    cols += list(range(o_kv + 128, o_kv + 256))
    cols += list(range(o_kv + 256, o_kv + 384))
    cols += list(range(o_kv + 512, o_kv + 640))
    for hg in range(4):
        for g in range(2):
            h = g * 4 + hg
            cols += list(range(o_qs + h * 64, o_qs + (h + 1) * 64))
    cols += list(range(o_kvs, o_kvs + 128))
    cols += list(range(o_gm, o_gm + 2048))
    cols += list(range(o_gn, o_gn + 24))
    fm = w_in[:, :, cols]
    fm = np.concatenate([fm, np.zeros((L, D, 8), f)], axis=2)
    tmc = list(range(o_kv + 384, o_kv + 512)) + list(range(o_kv + 640, o_kv + 768)) + list(range(o_kvs + 128, o_kvs + 256))
    sh["w_inR"] = np.ascontiguousarray(np.concatenate([fm, w_in[:, :, tmc]], axis=2), f)
    assert sh["w_inR"].shape[2] == FM_COLS + TM_COLS
    sh["posT"] = np.ascontiguousarray(np.asarray(inp["cmp_pos"][:L], f).transpose(0, 1, 3, 2))
    sh["w1R"] = np.ascontiguousarray(np.asarray(inp["cmp_w1"][:L], f).reshape(L, 2, 32, 64, 256).transpose(0, 1, 3, 2, 4).reshape(L, 2, 64, 32 * 256))
    sh["w2"] = np.ascontiguousarray(inp["cmp_w2"][:L], f)
    sh["sinksB"] = np.ascontiguousarray(np.broadcast_to(np.asarray(inp["sinks"][:L], f)[:, None, :], (L, 128, 8)))
    rel = np.asarray(inp["rel_tab"], f)
    sh["tabN"] = np.ascontiguousarray(np.stack([near_table(rel[:, h]) for h in range(8)], axis=1))
    sh["tabS"] = np.ascontiguousarray(np.stack([near_table(rel[:, 8 + h]) for h in range(8)], axis=1))
    sh["mskW"] = win_mask(1408, 512)
    sh["mskS"] = win_mask(1024, 128)
    sh["b31"] = np.ascontiguousarray(np.broadcast_to(rel[31][None, :], (128, 16)))
    wm = np.zeros((NCC * 128, NB + 1), f)
    for j in range(NB):
        for m in range(4):
            for n in range(2):
                i = 4 * j - m - n
                if 0 <= i < NCMP:
                    wm[i, j] += 1.0
    wm[:NCMP, NB] = 1.0
    sh["wmap"] = wm
    ft = np.zeros((128, 2 * NB), f)
    for qp in range(128):
        s = 1 if qp >= 64 else 0
        for y in range(2 * NB):
            x = y - NB
            if x == s or x == s - 1:
                ft[qp, y] = 100.0
            elif x > s:
                ft[qp, y] = -100.0
    sh["ftab"] = ft
    em = np.zeros((NB, T), f)
    for j in range(NB):
        em[j, j * 64:(j + 1) * 64] = 1.0
    sh["emat"] = em
    sg = np.zeros((32, 24 * 64), f)
    for r in range(24):
        sg[r, r * 64:(r + 1) * 64] = 1.0
    sh["selg"] = sg
    sh["identF"] = np.eye(128, dtype=f)
    wb = np.asarray(inp["w_branch"][:L], f).reshape(L, 2, 2, 4, 64, D)
    sh["w_brR"] = np.ascontiguousarray(wb.transpose(0, 1, 2, 4, 3, 5).reshape(L, 2, 128, 4 * D))
    sh["w_out"] = np.ascontiguousarray(inp["w_out"][:L], f)
    sh["w_router"] = np.ascontiguousarray(inp["w_router"][:L], f)
    sh["b_router"] = np.ascontiguousarray(np.asarray(inp["b_router"][:L], f)[:, None, :])
    wu = np.asarray(inp["w_up"][:L], f)
    sh["w_upR"] = np.ascontiguousarray(np.concatenate([wu[..., 0::2], wu[..., 1::2]], axis=-1))
    bu = np.asarray(inp["b_up"][:L], f)
    bu = np.concatenate([bu[..., 0::2], bu[..., 1::2]], axis=-1)
    sh["b_upR"] = np.ascontiguousarray(bu.reshape(L, NEXP, 16, 128).transpose(0, 1, 3, 2))
    sh["w_down"] = np.ascontiguousarray(inp["w_down"][:L], f)
    sh["b_down"] = np.ascontiguousarray(np.asarray(inp["b_down"][:L], f)[:, :, None, :])
    return sh


def run_model(inp, T, L, n_cores=8, dbg=False):
    x = np.asarray(inp["x"], np.float32)
    c = np.asarray(inp["c"], np.float32)
    B = x.shape[0]
    sh = prep_shared(inp, T, L)
    nc, kb = build(T, L, dbg)
    in_maps = []
    for core in range(n_cores):
        b = core % B
        m = dict(sh)
        m["xT"] = np.ascontiguousarray(x[b].T)
        m["cT"] = np.ascontiguousarray(c[b].reshape(8, 128).T)
        in_maps.append(m)
    res = run_bass_kernel_spmd(nc, in_maps, core_ids=list(range(n_cores)))
    out = np.stack([np.ascontiguousarray(res.results[b]["outT"].T) for b in range(B)], axis=0)
    if dbg:
        return out.astype(np.float32), res.results[0]
    return out.astype(np.float32)


def kernel(**inputs):
    return run_model(inputs, 8192, DEPTH, 8)
```

```python
import math
import numpy as np
import concourse.bass as bass
import concourse.mybir as mybir
from concourse.bass_utils import run_bass_kernel_spmd
from contextlib import ExitStack

F32 = mybir.dt.float32
BF16 = mybir.dt.bfloat16
AF = mybir.ActivationFunctionType
ALU = mybir.AluOpType

D = 1024
DEPTH = 4
NEXP = 32
NEG = -30000.0
FM_COLS = 3744
TM_COLS = 384
RMS_EPS = 1e-5


class Buf:
    def __init__(self, name, t):
        self.name = name
        self.t = t
        self.regs = {}
        self.dsem = None

    def __getitem__(self, idx):
        return self.t[idx]


class KB:
    def __init__(self, nc, es):
        self.nc = nc
        self.es = es
        self.engs = {"pe": nc.tensor, "dve": nc.vector, "act": nc.scalar, "pool": nc.gpsimd, "sp": nc.sync}
        self.esem = {}
        self.cnt = {}
        self.seen = {}
        for e in self.engs:
            self.esem[e] = es.enter_context(nc.semaphore("es_" + e))
            self.cnt[e] = 0
            self.seen[e] = {}
        self.dtot = {}
        self.dsems = []
        self.semcache = {}
        self.uid = 0
        self.nsem = 5
        self.ninst = 0

    def sb(self, es, name, shape, dt):
        self.uid += 1
        return Buf(name, es.enter_context(self.nc.sbuf_tensor("%s_u%d" % (name, self.uid), list(shape), dt)))

    def ps(self, es, name, shape, dt=F32):
        return Buf(name, es.enter_context(self.nc.psum_tensor(name, list(shape), dt)))

    dbg = False

    def dram(self, name, shape, dt, kind="Internal"):
        if kind == "Internal" and self.dbg:
            kind = "ExternalOutput"
        return Buf(name, self.nc.dram_tensor(name, list(shape), dt, kind=kind).ap())

    def _collect(self, b, key, is_write, waits):
        regs = b.regs
        keys = list(regs.keys()) if key == "*" else [k for k in (key, "*") if k in regs]
        for k in keys:
            r = regs[k]
            if r["w"] is not None:
                self._addwait(r["w"], waits)
            if is_write:
                for sem, val in r["r"].items():
                    self._addwait((sem, val), waits)

    def _addwait(self, ev, waits):
        sem, val = ev
        if id(sem) in self.dtot:
            val = self.dtot[id(sem)]
        k = id(sem)
        if k not in waits or waits[k][1] < val:
            waits[k] = (sem, val)

    def _mark(self, b, key, is_write, ev):
        regs = b.regs
        if is_write:
            if key == "*":
                regs.clear()
            regs[key] = {"w": ev, "r": {}}
        else:
            r = regs.setdefault(key, {"w": None, "r": {}})
            r["r"][ev[0]] = ev[1]

    @staticmethod
    def _norm(lst):
        out = []
        for x in lst:
            if isinstance(x, tuple):
                out.append(x)
            else:
                out.append((x, "*"))
        return out

    def _emit_waits(self, eng, reads, writes):
        waits = {}
        for b, key in reads:
            self._collect(b, key, False, waits)
        for b, key in writes:
            self._collect(b, key, True, waits)
        E = self.engs[eng]
        own = id(self.esem[eng])
        for k, (sem, val) in waits.items():
            if k == own and eng == "pe":
                continue
            if self.seen[eng].get(k, 0) >= val:
                continue
            E.wait_ge(sem, val)
            self.seen[eng][k] = val

    def op(self, eng, fn, reads=(), writes=()):
        reads = self._norm(reads)
        writes = self._norm(writes)
        self._emit_waits(eng, reads, writes)
        inst = fn(self.engs[eng])
        self.cnt[eng] += 1
        self.ninst += 1
        inst.then_inc(self.esem[eng], 1)
        ev = (self.esem[eng], self.cnt[eng])
        for b, key in reads:
            self._mark(b, key, False, ev)
        for b, key in writes:
            self._mark(b, key, True, ev)

    def dma(self, q, out_ap, in_ap, reads, writes, owner, **kw):
        reads = self._norm(reads)
        writes = self._norm(writes)
        self._emit_waits(q, reads, writes)
        if owner.dsem is None:
            if owner.name in self.semcache:
                owner.dsem = self.semcache[owner.name]
            else:
                owner.dsem = self.es.enter_context(self.nc.semaphore("ds_" + owner.name))
                self.semcache[owner.name] = owner.dsem
                self.dtot[id(owner.dsem)] = 0
                self.dsems.append(owner.dsem)
                self.nsem += 1
        inst = self.engs[q].dma_start(out=out_ap, in_=in_ap, **kw)
        inst.then_inc(owner.dsem, 16)
        self.ninst += 1
        self.dtot[id(owner.dsem)] += 16
        ev = (owner.dsem, self.dtot[id(owner.dsem)])
        for b, key in reads:
            self._mark(b, key, False, ev)
        for b, key in writes:
            self._mark(b, key, True, ev)

    def load(self, dst, dst_ap, src, src_ap, skey="*", dkey="*", q="sp", **kw):
        self.dma(q, dst_ap, src_ap, [(src, skey)], [(dst, dkey)], dst, **kw)

    def loadc(self, dst, dst_ap, src, src_ap, skey="*", dkey="*", **kw):
        kw.setdefault("max_dma_last_dim", 4096)
        self.dma("pool", dst_ap, src_ap, [(src, skey)], [(dst, dkey)], dst, **kw)

    def store(self, dst, dst_ap, src, src_ap, skey="*", dkey="*", q="sp", **kw):
        self.dma(q, dst_ap, src_ap, [(src, skey)], [(dst, dkey)], src, **kw)

    def barrier(self):
        for eng, E in self.engs.items():
            for e2, sem in self.esem.items():
                if e2 == eng or self.cnt[e2] == 0:
                    continue
                if self.seen[eng].get(id(sem), 0) >= self.cnt[e2]:
                    continue
                E.wait_ge(sem, self.cnt[e2])
                self.seen[eng][id(sem)] = self.cnt[e2]
            for sem in self.dsems:
                tot = self.dtot[id(sem)]
                if tot == 0 or self.seen[eng].get(id(sem), 0) >= tot:
                    continue
                E.wait_ge(sem, tot)
                self.seen[eng][id(sem)] = tot

    def dump(self, name, buf, ap, shape, dt):
        if not self.dbg:
            return
        d = self.dram("dbg_" + name, shape, dt, kind="ExternalOutput")
        self.store(d, d.t, buf, ap)

    def wait_all(self, eng, bufs):
        waits = {}
        for b in bufs:
            self._collect(b, "*", True, waits)
        E = self.engs[eng]
        for k, (sem, val) in waits.items():
            E.wait_ge(sem, val)


def t5_bucket_np(n):
    n = np.maximum(n, 0)
    lr = np.log(np.maximum(n, 1).astype(np.float32) / np.float32(16)) / np.float32(math.log(128 / 16))
    large = 16 + (lr.astype(np.float32) * np.float32(16)).astype(np.int32)
    return np.where(n < 16, n, np.minimum(large, 31))


def near_table(tab_col, width=1024, off=384):
    kp = np.arange(128)[:, None]
    y = np.arange(width)[None, :]
    x = y - kp - off
    out = tab_col[t5_bucket_np(x)]
    return np.where(x >= 0, out, np.float32(NEG)).astype(np.float32)


def win_mask(width, win, off=384):
    kp = np.arange(128)[:, None]
    y = np.arange(width)[None, :]
    x = y - kp - off
    return np.where(x >= win, np.float32(NEG), np.float32(0)).astype(np.float32)


def build(T, L, dbg=False):
    NQ = T // 512
    NKC = T // 128
    NB = T // 64
    NCMP = T // 16 - 1
    NCC = (NCMP + 127) // 128
    NG = T // 1024
    assert NB <= 128 and NCMP <= 512

    nc = bass.Bass("TRN2", target_bir_lowering=False)
    es = ExitStack()
    k = KB(nc, es)
    k.dbg = dbg

    def ext(name, shape, dt=F32):
        return k.dram(name, shape, dt, kind="ExternalInput")

    xT_in = ext("xT", [D, T])
    cT_in = ext("cT", [128, 8])
    w_ada = ext("w_ada", [L, D, 6 * D])
    b_adaT = ext("b_adaT", [L, 128, 48])
    gvec = ext("gvec", [128, (2 * L + 1) * 8])
    w_inR = ext("w_inR", [L, D, FM_COLS + TM_COLS])
    posT = ext("posT", [L, 2, 64, 32])
    w1R = ext("w1R", [L, 2, 64, 32 * 256])
    w2 = ext("w2", [L, 2, 256, 64])
    sinksB = ext("sinksB", [L, 128, 8])
    tabN = ext("tabN", [128, 8, 1024])
    tabS = ext("tabS", [128, 8, 1024])
    mskW = ext("mskW", [128, 1408])
    mskS = ext("mskS", [128, 1024])
    b31 = ext("b31", [128, 16])
    wmap = ext("wmap", [NCC * 128, NB + 1])
    ftab = ext("ftab", [128, 2 * NB])
    emat = ext("emat", [NB, T])
    selg = ext("selg", [32, 24 * 64])
    identF = ext("identF", [128, 128])
    w_brR = ext("w_brR", [L, 2, 128, 4 * D])
    w_out = ext("w_out", [L, D, D])
    w_router = ext("w_router", [L, D, NEXP])
    b_router = ext("b_router", [L, 1, NEXP])
    w_upR = ext("w_upR", [L, NEXP, D, 2 * D])
    b_upR = ext("b_upR", [L, NEXP, 128, 16])
    w_down = ext("w_down", [L, NEXP, D, D])
    b_down = ext("b_down", [L, NEXP, 1, D])
    outT = k.dram("outT", [D, T], F32, kind="ExternalOutput")

    xresA = k.dram("xresA", [8, 128, T], F32)
    xresB = k.dram("xresB", [8, 128, T], F32)
    ZFM = k.dram("ZFM", [29, 128, T], BF16)
    ZTM = k.dram("ZTM", [T, TM_COLS], BF16)
    LNG = k.dram("LNG", [32, T], F32)
    OCSD = k.dram("OCSD", [4, 128, T], F32)
    ONB = k.dram("ONB", [4, 128, T], BF16)
    dbgs = {}

    xT_v = xT_in.t.rearrange("(c p) t -> p c t", p=128)
    outT_v = outT.t.rearrange("(c p) t -> p c t", p=128)

    cs = es
    ones_bf = k.sb(cs, "ones_bf", [128, 128], BF16)
    ones_f = k.sb(cs, "ones_f", [128, 128], F32)
    neg1_f = k.sb(cs, "neg1_f", [128, 64], F32)
    ident_f = k.sb(cs, "ident_f", [128, 128], F32)
    ident_bf = k.sb(cs, "ident_bf", [128, 128], BF16)
    epsc = k.sb(cs, "epsc", [128, 1], F32)
    modT = k.sb(cs, "modT", [128, L * 48], F32)
    A1 = k.sb(cs, "A1", [128, L * 8], F32)
    A2 = k.sb(cs, "A2", [128, L * 8], F32)
    gv = k.sb(cs, "gv", [128, (2 * L + 1) * 8], F32)
    b31t = k.sb(cs, "b31t", [128, 16], F32)
    PS = [k.ps(cs, "psb%d" % i, [128, 512]) for i in range(8)]
    psi = [0]

    psa = [0]

    def psum():
        p = PS[psi[0] % 6]
        psi[0] += 1
        return p

    def psum_acc():
        p = PS[6 + psa[0] % 2]
        psa[0] += 1
        return p

    k.op("dve", lambda e: e.memset(ones_bf[:], 1.0), writes=[ones_bf])
    k.op("dve", lambda e: e.memset(ones_f[:], 1.0), writes=[ones_f])
    k.op("dve", lambda e: e.memset(neg1_f[:], -1.0), writes=[neg1_f])
    k.op("dve", lambda e: e.memset(epsc[:], RMS_EPS), writes=[epsc])
    k.load(ident_f, ident_f[:], identF, identF[:, :])
    k.loadc(ident_bf, ident_bf[:], identF, identF[:, :])
    k.load(gv, gv[:], gvec, gvec[:, :])
    k.load(b31t, b31t[:], b31, b31[:, :])

    with ExitStack() as s0:
        ct = k.sb(s0, "ct", [128, 8], F32)
        sg = k.sb(s0, "sgc", [128, 8], F32)
        cond = k.sb(s0, "cond", [128, 8], F32)
        bad = k.sb(s0, "bad", [128, 48], F32)
        wa = [k.sb(s0, "wa%d" % i, [128, 8, 768], F32) for i in range(2)]
        k.load(ct, ct[:], cT_in, cT_in[:, :])
        k.op("act", lambda e: e.activation(out=sg[:], in_=ct[:], func=AF.Sigmoid), reads=[ct], writes=[sg])
        k.op("dve", lambda e: e.tensor_tensor(out=cond[:], in0=ct[:], in1=sg[:], op=ALU.mult), reads=[ct, sg], writes=[cond])
        for l in range(L):
            pm = psum()
            for blk in range(8):
                wt = wa[blk % 2]
                k.load(wt, wt[:], w_ada, w_ada[l, :, blk * 768:(blk + 1) * 768].rearrange("(k p) c -> p k c", p=128))
                for jj in range(6):
                    j = blk * 6 + jj
                    for kk in range(8):
                        k.op("pe", lambda e, wt=wt, jj=jj, kk=kk, j=j: e.matmul(
                            pm[:, j:j + 1], wt[:, kk, jj * 128:(jj + 1) * 128], cond[:, kk:kk + 1],
                            start=(kk == 0), stop=(kk == 7)), reads=[wt, cond], writes=[pm])
            k.load(bad, bad[:], b_adaT, b_adaT[l, :, :])
            k.op("dve", lambda e, l=l: e.tensor_tensor(out=modT[:, l * 48:(l + 1) * 48], in0=pm[:, 0:48], in1=bad[:], op=ALU.add),
                 reads=[pm, bad], writes=[modT])
            k.op("dve", lambda e, l=l: e.scalar_tensor_tensor(
                out=A1[:, l * 8:(l + 1) * 8], in0=modT[:, l * 48 + 8:l * 48 + 16], scalar=1.0,
                in1=gv[:, l * 8:(l + 1) * 8], op0=ALU.add, op1=ALU.mult), reads=[modT, gv], writes=[A1])
            k.op("dve", lambda e, l=l: e.scalar_tensor_tensor(
                out=A2[:, l * 8:(l + 1) * 8], in0=modT[:, l * 48 + 32:l * 48 + 40], scalar=1.0,
                in1=gv[:, (L + l) * 8:(L + l + 1) * 8], op0=ALU.add, op1=ALU.mult), reads=[modT, gv], writes=[A2])

    k.barrier()
    k.dump("modT", modT, modT[:], [128, L * 48], F32)
    k.dump("A1", A1, A1[:], [128, L * 8], F32)

    def mcol(l, kind, c):
        j = l * 48 + kind * 8 + c
        return modT[:, j:j + 1]

    def norm_tile(X, Hout, scl, shf, sq, rstd, tmp, Hf=None):
        pm = psum()
        for c in range(8):
            k.op("act", lambda e, c=c: e.activation(out=sq[:, c, :], in_=X[:, c, :], func=AF.Square),
                 reads=[(X, c)], writes=[(sq, c)])
            k.op("pe", lambda e, c=c: e.matmul(pm[:, :], ones_bf[:, :], sq[:, c, :], start=(c == 0), stop=(c == 7)),
                 reads=[(sq, c), ones_bf], writes=[pm])
        k.op("act", lambda e: e.activation(out=rstd[:], in_=pm[:, :], func=AF.Sqrt, bias=epsc[:, 0:1], scale=1.0 / D),
             reads=[pm, epsc], writes=[rstd])
        k.op("dve", lambda e: e.reciprocal(out=rstd[:], in_=rstd[:]), reads=[rstd], writes=[rstd])
        for c in range(8):
            k.op("dve", lambda e, c=c: e.tensor_tensor(out=tmp[:, c % 2, :], in0=X[:, c, :], in1=rstd[:], op=ALU.mult),
                 reads=[(X, c), rstd], writes=[(tmp, c % 2)])
            if shf is not None:
                k.op("act", lambda e, c=c: e.activation(out=Hout[:, c, :], in_=tmp[:, c % 2, :], func=AF.Identity,
                                                         scale=scl(c), bias=shf(c)),
                     reads=[(tmp, c % 2), modT, A1, A2], writes=[(Hout, c)])
                if Hf is not None:
                    k.op("act", lambda e, c=c: e.activation(out=Hf[:, c, :], in_=tmp[:, c % 2, :], func=AF.Identity,
                                                             scale=scl(c), bias=shf(c)),
                         reads=[(tmp, c % 2), modT, A1, A2], writes=[(Hf, c)])
            else:
                k.op("act", lambda e, c=c: e.activation(out=Hout[:, c, :], in_=tmp[:, c % 2, :], func=AF.Identity, scale=scl(c)),
                     reads=[(tmp, c % 2), gv], writes=[(Hout, c)])

    xcur_v = xT_v
    xcur_b = xT_in

    for l in range(L):
        k.barrier()
        with ExitStack() as sA:
            Win = k.sb(sA, "Win", [128, 8, FM_COLS + TM_COLS], BF16)
            for kk in range(8):
                for h0 in range(0, FM_COLS + TM_COLS, 1376):
                    k.loadc(Win, Win[:, kk, h0:h0 + 1376], w_inR, w_inR[l, kk * 128:(kk + 1) * 128, h0:h0 + 1376], dkey=(kk, h0))
            Xs = [k.sb(sA, "Xa%d" % i, [128, 8, 512], F32) for i in range(1)]
            sq = k.sb(sA, "sqA", [128, 8, 512], BF16)
            rstd = k.sb(sA, "rstdA", [128, 512], F32)
            tmp = k.sb(sA, "tmpA", [128, 2, 512], F32)
            Hs = [k.sb(sA, "Ha%d" % i, [128, 8, 512], BF16) for i in range(1)]
            ZT = [k.sb(sA, "ZT%d" % i, [128, 29, 512], BF16) for i in range(1)]
            VT = [k.sb(sA, "VT%d" % i, [128, 4, TM_COLS], BF16) for i in range(2)]
            GN = k.sb(sA, "GN", [32, 512], F32)
            GN2 = [k.sb(sA, "GN2%d" % i, [32, 512], F32) for i in range(2)]
            for qi in range(NQ):
                qs = slice(qi * 512, (qi + 1) * 512)
                X = Xs[0]
                H = Hs[0]
                Z = ZT[0]
                V = VT[qi % 2]
                G2 = GN2[qi % 2]
                k.load(X, X[:], xcur_b, xcur_v[:, :, qs], skey=qi)
                norm_tile(X, H, lambda c: A1[:, l * 8 + c:l * 8 + c + 1], lambda c: mcol(l, 0, c), sq, rstd, tmp)
                if qi == 0 and l == 0:
                    k.dump("H0", H, H[:], [128, 8, 512], BF16)
                    k.dump("rstd0", rstd, rstd[:], [128, 512], F32)
                    k.dump("X0", X, X[:], [128, 8, 512], F32)
                for j in range(30):
                    w = 128 if j < 29 else 32
                    pm = psum()
                    for kk in range(8):
                        k.op("pe", lambda e, kk=kk, j=j, w=w, pm=pm: e.matmul(
                            pm[0:w, :], Win[:, kk, j * 128:j * 128 + w], H[:, kk, :], start=(kk == 0), stop=(kk == 7)),
                            reads=[Win, (H, kk)], writes=[pm])
                    if j < 4 or 8 <= j < 12:
                        k.op("act", lambda e, j=j, pm=pm: e.activation(out=Z[:, j, :], in_=pm[:, :], func=AF.Copy, scale=0.125),
                             reads=[pm], writes=[(Z, j)])
                    elif j < 13:
                        k.op("dve", lambda e, j=j, pm=pm: e.tensor_copy(out=Z[:, j, :], in_=pm[:, :]), reads=[pm], writes=[(Z, j)])
                    elif j < 29:
                        k.op("act", lambda e, j=j, pm=pm: e.activation(out=Z[:, j, :], in_=pm[:, :], func=AF.Sigmoid),
                             reads=[pm], writes=[(Z, j)])
                    else:
                        k.op("act", lambda e, pm=pm: e.activation(out=GN[:, :], in_=pm[0:32, :], func=AF.Sigmoid),
                             reads=[pm], writes=[GN])
                        k.op("act", lambda e: e.activation(out=G2[:, :], in_=GN[:, :], func=AF.Ln), reads=[GN], writes=[G2])
                for q4 in range(4):
                    pm = psum()
                    for kk in range(8):
                        k.op("pe", lambda e, kk=kk, q4=q4, pm=pm: e.matmul(
                            pm[:, 0:TM_COLS], H[:, kk, q4 * 128:(q4 + 1) * 128], Win[:, kk, FM_COLS:FM_COLS + TM_COLS],
                            start=(kk == 0), stop=(kk == 7)), reads=[Win, (H, kk)], writes=[pm])
                    k.op("dve", lambda e, q4=q4, pm=pm: e.tensor_copy(out=V[:, q4, :], in_=pm[:, 0:TM_COLS]),
                         reads=[pm], writes=[(V, q4)])
                k.store(ZFM, ZFM.t[:, :, qs].rearrange("j p t -> p j t"), Z, Z[:], dkey=qi)
                k.store(ZTM, ZTM.t[qs, :].rearrange("(q p) c -> p q c", p=128), V, V[:], dkey=qi)
                k.store(LNG, LNG.t[:, qs], G2, G2[:], dkey=qi)

        k.barrier()
        with ExitStack() as sC:
            KCMP = k.sb(sC, "KCMP", [128, 512], BF16)
            VCMP = k.sb(sC, "VCMP", [128, NCC, 2, 65], BF16)
            k.op("dve", lambda e: e.memset(KCMP[:], 0.0), writes=[KCMP])
            k.op("dve", lambda e: e.memset(VCMP[:], 0.0), writes=[VCMP])
            with ExitStack() as sB:
                RT = k.sb(sB, "RT", [128, T], BF16)
                W1d = k.sb(sB, "W1d", [128, 32, 256], BF16)
                posd = k.sb(sB, "posd", [128, 32], BF16)
                W2t = k.sb(sB, "W2t", [128, 2, 64], BF16)
                posb = k.sb(sB, "posb", [128, 1], F32)
                U = k.sb(sB, "U", [128, 512], F32)
                U2 = k.sb(sB, "U2", [128, 512], F32)
                SG = k.sb(sB, "SGB", [128, 512], F32)
                HT = k.sb(sB, "HT", [128, 2, 512], BF16)
                for kv in range(2):
                    k.load(RT, RT[:], ZFM, ZFM.t[4 + kv, :, :])
                    for half in range(2):
                        k.loadc(W1d, W1d[half * 64:(half + 1) * 64, :, :], w1R,
                                w1R[l, kv, :, :].rearrange("d (l h) -> d l h", h=256), dkey=half)
                        k.loadc(posd, posd[half * 64:(half + 1) * 64, :], posT, posT[l, kv, :, :], dkey=half)
                    k.loadc(W2t, W2t[:], w2, w2[l, kv, :, :].rearrange("(c p) d -> p c d", p=128))
                    for g in range(2):
                        gs = slice(64 * g, 64 * g + 64)
                        for hc in range(2):
                            pb = psum()
                            for ll in range(32):
                                k.op("pe", lambda e, ll=ll, hc=hc, gs=gs, pb=pb: e.matmul(
                                    pb[:, 0:1], W1d[gs, ll, hc * 128:(hc + 1) * 128], posd[gs, ll:ll + 1],
                                    start=(ll == 0), stop=(ll == 31)), reads=[W1d, posd], writes=[pb])
                            k.op("dve", lambda e, pb=pb: e.tensor_copy(out=posb[:], in_=pb[:, 0:1]), reads=[pb], writes=[posb])
                            pm = psum()
                            for ll in range(32):
                                k.op("pe", lambda e, ll=ll, hc=hc, gs=gs, pm=pm: e.matmul(
                                    pm[:, 0:NCMP], W1d[gs, ll, hc * 128:(hc + 1) * 128],
                                    RT[gs, ll:ll + 16 * (NCMP - 1) + 1:16],
                                    start=(ll == 0), stop=(ll == 31)), reads=[W1d, RT], writes=[pm])
                            k.op("act", lambda e, pm=pm: e.activation(out=U[:, 0:NCMP], in_=pm[:, 0:NCMP], func=AF.Identity, bias=posb[:, 0:1]),
                                 reads=[pm, posb], writes=[U])
                            k.op("dve", lambda e: e.tensor_tensor(out=U2[:, 0:NCMP], in0=U[:, 0:NCMP], in1=U[:, 0:NCMP], op=ALU.mult),
                                 reads=[U], writes=[U2])
                            k.op("dve", lambda e: e.tensor_scalar(out=U2[:, 0:NCMP], in0=U2[:, 0:NCMP], scalar1=0.044715, scalar2=1.0,
                                                                  op0=ALU.mult, op1=ALU.add), reads=[U2], writes=[U2])
                            k.op("dve", lambda e: e.tensor_tensor(out=U2[:, 0:NCMP], in0=U2[:, 0:NCMP], in1=U[:, 0:NCMP], op=ALU.mult),
                                 reads=[U2, U], writes=[U2])
                            k.op("act", lambda e: e.activation(out=SG[:, 0:NCMP], in_=U2[:, 0:NCMP], func=AF.Sigmoid, scale=1.5957691216057308),
                                 reads=[U2], writes=[SG])
                            k.op("dve", lambda e, hc=hc: e.tensor_tensor(out=HT[:, hc, 0:NCMP], in0=U[:, 0:NCMP], in1=SG[:, 0:NCMP], op=ALU.mult),
                                 reads=[U, SG], writes=[(HT, hc)])
                        if kv == 0:
                            pm = psum()
                            for hc in range(2):
                                k.op("pe", lambda e, hc=hc, pm=pm: e.matmul(pm[0:64, 0:NCMP], W2t[:, hc, :], HT[:, hc, 0:NCMP],
                                                                            start=(hc == 0), stop=(hc == 1)),
                                     reads=[W2t, (HT, hc)], writes=[pm])
                            k.op("dve", lambda e, gs=gs, pm=pm: e.tensor_copy(out=KCMP[gs, 0:NCMP], in_=pm[0:64, 0:NCMP]),
                                 reads=[pm], writes=[(KCMP, g)])
                        else:
                            for c in range(NCC):
                                n0 = c * 128
                                nn = min(128, NCMP - n0)
                                pm = psum()
                                for hc in range(2):
                                    k.op("pe", lambda e, hc=hc, pm=pm, n0=n0, nn=nn: e.matmul(
                                        pm[0:nn, 0:64], HT[:, hc, n0:n0 + nn], W2t[:, hc, :], start=(hc == 0), stop=(hc == 1)),
                                        reads=[W2t, (HT, hc)], writes=[pm])
                                k.op("dve", lambda e, pm=pm, c=c, g=g, nn=nn: e.tensor_copy(out=VCMP[0:nn, c, g, 0:64], in_=pm[0:nn, 0:64]),
                                     reads=[pm], writes=[(VCMP, (c, g))])
                                k.op("dve", lambda e, c=c, g=g, nn=nn: e.memset(VCMP[0:nn, c, g, 64:65], 1.0), writes=[(VCMP, (c, g, 1))])

            k.barrier()
            KSL = k.sb(sC, "KSL", [128, T], BF16)
            VSL = k.sb(sC, "VSL", [128, NKC, 2, 65], BF16)
            TABN = k.sb(sC, "TABN", [128, 8, 1024], BF16)
            EM = k.sb(sC, "EM", [128, T], BF16)
            WT = k.sb(sC, "WTm", [128, NCC, NB + 1], BF16)
            FT = k.sb(sC, "FT", [128, 2 * NB], F32)
            SELG = k.sb(sC, "SELG", [32, 24 * 64], F32)
            k.load(KSL, KSL[:], ZFM, ZFM.t[6, :, :])
            k.op("pool", lambda e: e.memset(VSL[:], 1.0), writes=[VSL])
            for g in range(2):
                k.load(VSL, VSL[:, :, g, 0:64], ZTM, ZTM.t[:, g * 64:(g + 1) * 64].rearrange("(c p) d -> p c d", p=128), dkey=g)
            k.loadc(TABN, TABN[:], tabN, tabN[:, :, :])
            k.loadc(EM, EM[0:NB, :], emat, emat[:, :])
            k.loadc(WT, WT[:], wmap, wmap.t.rearrange("(c p) j -> p c j", p=128))
            k.load(FT, FT[:], ftab, ftab[:, :])
            k.load(SELG, SELG[:], selg, selg[:, :])
            QTs = [k.sb(sC, "QT%d" % i, [128, 4, 512], BF16) for i in range(2)]
            LGs = [k.sb(sC, "LG%d" % i, [32, 512], F32) for i in range(2)]
            EC = k.sb(sC, "EC", [128, 4, NCC, 512], BF16)
            ETs = [k.sb(sC, "ET%d" % i, [128, 512], BF16) for i in range(4)]
            PENT = k.sb(sC, "PENT", [128, 2, 512], BF16)
            OCS = [k.sb(sC, "OCS%d" % i, [128, 4, 512], F32) for i in range(2)]
            drow = k.sb(sC, "drow", [128, 512], F32)
            lnrow = k.sb(sC, "lnrow", [128, 512], F32)
            EB = k.sb(sC, "EB", [64, 512], F32)
            TMPF = k.sb(sC, "TMPF", [128, 512], F32)
            acc = k.sb(sC, "acc", [128, 128], F32)
            sc2 = k.sb(sC, "sc2", [128, 128], F32)
            m8a = k.sb(sC, "m8a", [128, 8], F32)
            m8b = k.sb(sC, "m8b", [128, 8], F32)
            rden = k.sb(sC, "rden", [128, 1], F32)
            pen = k.sb(sC, "pen", [128, 128], F32)

            def finalize(Ops, dest, g, hg, first, LGT, r=None, sink_ap=None, out_bf=False):
                gs = slice(64 * g, 64 * g + 64)
                k.op("dve", lambda e: e.tensor_scalar_max(out=drow[64:65, :], in0=Ops[64:65, :], scalar1=1e-30),
                     reads=[Ops], writes=[drow])
                if sink_ap is None:
                    k.op("act", lambda e: e.activation(out=lnrow[64:65, :], in_=drow[64:65, :], func=AF.Ln), reads=[drow], writes=[lnrow])
                else:
                    k.op("act", lambda e: e.activation(out=lnrow[64:65, :], in_=drow[64:65, :], func=AF.Ln, bias=sink_ap),
                         reads=[drow], writes=[lnrow])
                pb = psum()
                if r is not None:
                    k.op("pe", lambda e: e.matmul(pb[0:64, :], SELG[0:32, r * 64:(r + 1) * 64], LGT[0:32, :], start=True, stop=False),
                         reads=[SELG, LGT], writes=[pb])
                k.op("pe", lambda e: e.matmul(pb[0:64, :], neg1_f[64:65, 0:64], lnrow[64:65, :], start=(r is None), stop=True),
                     reads=[neg1_f, lnrow], writes=[pb])
                k.op("act", lambda e: e.activation(out=EB[:, :], in_=pb[0:64, :], func=AF.Exp), reads=[pb], writes=[EB])
                if first:
                    k.op("dve", lambda e: e.tensor_tensor(out=dest[gs, hg, :], in0=Ops[0:64, :], in1=EB[:, :], op=ALU.mult),
                         reads=[Ops, EB], writes=[(dest, (g, hg))])
                else:
                    k.op("dve", lambda e: e.tensor_tensor(out=TMPF[gs, :], in0=Ops[0:64, :], in1=EB[:, :], op=ALU.mult),
                         reads=[Ops, EB], writes=[TMPF])
                    k.op("pool", lambda e: e.tensor_tensor(out=dest[gs, hg, :], in0=dest[gs, hg, :], in1=TMPF[gs, :], op=ALU.add),
                         reads=[TMPF, (dest, (g, hg))], writes=[(dest, (g, hg))])

            def attn_chunks(chunks, Kt, Vt, Qt, g, hg, extra, bias_far):
                gs = slice(64 * g, 64 * g + 64)
                Ops = psum_acc()
                pend = []
                n = len(chunks)
                for i, (kc, dl) in enumerate(chunks):
                    pm = psum()
                    ex = extra(kc, dl)
                    k.op("pe", lambda e, pm=pm, kc=kc, ex=ex: e.matmul(pm[:, :], Kt[gs, kc * 128:(kc + 1) * 128], Qt[gs, hg, :],
                                                                          start=True, stop=(len(ex) == 0)),
                         reads=[Kt, Qt], writes=[pm])
                    for xi, (la, ra, bufs) in enumerate(ex):
                        k.op("pe", lambda e, pm=pm, la=la, ra=ra, xi=xi, ex=ex: e.matmul(pm[:, :], la, ra, start=False, stop=(xi == len(ex) - 1)),
                             reads=bufs, writes=[pm])
                    ET = ETs[i % 4]
                    bf = bias_far(dl)
                    if bf is None:
                        k.op("act", lambda e, pm=pm, ET=ET: e.activation(out=ET[:, :], in_=pm[:, :], func=AF.Exp), reads=[pm], writes=[ET])
                    else:
                        k.op("act", lambda e, pm=pm, ET=ET, bf=bf: e.activation(out=ET[:, :], in_=pm[:, :], func=AF.Exp, bias=bf),
                             reads=[pm, b31t], writes=[ET])
                    pend.append((kc, ET, i))
                    if len(pend) > 2:
                        kc2, ET2, i2 = pend.pop(0)
                        k.op("pe", lambda e, kc2=kc2, ET2=ET2, i2=i2: e.matmul(Ops[0:65, :], Vt[:, kc2, g, 0:65], ET2[:, :],
                                                                                 start=(i2 == 0), stop=(i2 == n - 1)),
                             reads=[Vt, ET2], writes=[Ops])
                for kc2, ET2, i2 in pend:
                    k.op("pe", lambda e, kc2=kc2, ET2=ET2, i2=i2: e.matmul(Ops[0:65, :], Vt[:, kc2, g, 0:65], ET2[:, :],
                                                                             start=(i2 == 0), stop=(i2 == n - 1)),
                         reads=[Vt, ET2], writes=[Ops])
                return Ops

            for qi in range(NQ):
                q0 = qi * 512
                qs = slice(q0, q0 + 512)
                QT = QTs[qi % 2]
                LGT = LGs[qi % 2]
                OC = OCS[qi % 2]
                k.load(QT, QT[:], ZFM, ZFM.t[0:4, :, qs].rearrange("j p t -> p j t"), skey=qi)
                k.load(LGT, LGT[:], LNG, LNG.t[:, qs], skey=qi)
                NCV = min(NCC, (q0 + 480) // 2048 + 1)
                for g in range(2):
                    gs = slice(64 * g, 64 * g + 64)
                    for hg in range(4):
                        for c in range(NCV):
                            pm = psum()
                            k.op("pe", lambda e, pm=pm, c=c: e.matmul(pm[:, :], KCMP[gs, c * 128:(c + 1) * 128], QT[gs, hg, :], start=True, stop=True),
                                 reads=[KCMP, QT], writes=[pm])
                            k.op("act", lambda e, pm=pm, c=c: e.activation(out=EC[:, hg, c, :], in_=pm[:, :], func=AF.Exp),
                                 reads=[pm], writes=[(EC, (hg, c))])
                            if 2048 * c + 2063 > q0:
                                base = q0 - 2048 * c - 31
                                k.op("pool", lambda e, c=c, base=base: e.affine_select(
                                    out=EC[:, hg, c, :], in_=EC[:, hg, c, :], pattern=[[1, 512]], compare_op=ALU.is_ge,
                                    fill=0.0, base=base, channel_multiplier=-16), reads=[(EC, (hg, c))], writes=[(EC, (hg, c))])
                        Ops = psum_acc()
                        for c in range(NCV):
                            k.op("pe", lambda e, c=c: e.matmul(Ops[0:65, :], VCMP[:, c, g, 0:65], EC[:, hg, c, :], start=(c == 0), stop=(c == NCV - 1)),
                                 reads=[VCMP, (EC, (hg, c))], writes=[Ops])
                        finalize(Ops, OC, g, hg, True, LGT, r=0 * 8 + g * 4 + hg)
                    for q4 in range(4):
                        t128 = qi * 4 + q4
                        for hg in range(4):
                            pi = psum()
                            for c in range(NCV):
                                k.op("pe", lambda e, c=c, pi=pi: e.matmul(pi[:, 0:NB + 1], EC[:, hg, c, q4 * 128:(q4 + 1) * 128], WT[:, c, :],
                                                                          start=(c == 0), stop=(c == NCV - 1)),
                                     reads=[WT, (EC, (hg, c))], writes=[pi])
                            k.op("dve", lambda e, pi=pi: e.tensor_scalar_max(out=rden[:], in0=pi[:, NB:NB + 1], scalar1=1e-30), reads=[pi], writes=[rden])
                            k.op("dve", lambda e: e.reciprocal(out=rden[:], in_=rden[:]), reads=[rden], writes=[rden])
                            if hg == 0:
                                k.op("dve", lambda e, pi=pi: e.tensor_scalar(out=acc[:, 0:NB], in0=pi[:, 0:NB], scalar1=rden[:, 0:1], scalar2=None, op0=ALU.mult),
                                     reads=[pi, rden], writes=[acc])
                            else:
                                k.op("dve", lambda e, pi=pi: e.scalar_tensor_tensor(out=acc[:, 0:NB], in0=pi[:, 0:NB], scalar=rden[:, 0:1], in1=acc[:, 0:NB],
                                                                                    op0=ALU.mult, op1=ALU.add), reads=[pi, rden, acc], writes=[acc])
                        fo = NB - 2 * t128
                        k.op("dve", lambda e, fo=fo: e.tensor_tensor(out=acc[:, 0:NB], in0=acc[:, 0:NB], in1=FT[:, fo:fo + NB], op=ALU.add),
                             reads=[acc, FT], writes=[acc])
                        k.op("dve", lambda e: e.tensor_scalar_add(out=acc[:, 0:1], in0=acc[:, 0:1], scalar1=100.0), reads=[acc], writes=[acc])
                        k.op("dve", lambda e: e.max(out=m8a[:], in_=acc[:, 0:NB]), reads=[acc], writes=[m8a])
                        k.op("dve", lambda e: e.match_replace(out=sc2[:, 0:NB], in_to_replace=m8a[:], in_values=acc[:, 0:NB], imm_value=-1e30),
                             reads=[acc, m8a], writes=[sc2])
                        k.op("dve", lambda e: e.max(out=m8b[:], in_=sc2[:, 0:NB]), reads=[sc2], writes=[m8b])
                        k.op("dve", lambda e: e.tensor_scalar(out=pen[:, 0:NB], in0=acc[:, 0:NB], scalar1=m8b[:, 7:8], scalar2=1.0,
                                                              op0=ALU.is_ge, op1=ALU.subtract), reads=[acc, m8b], writes=[pen])
                        pt = psum()
                        k.op("pe", lambda e, pt=pt: e.transpose(pt[0:NB, 0:128], pen[:, 0:NB], ident_f[:, :]), reads=[pen, ident_f], writes=[pt])
                        k.op("act", lambda e, pt=pt, q4=q4: e.activation(out=PENT[0:NB, g, q4 * 128:(q4 + 1) * 128], in_=pt[0:NB, 0:128],
                                                                          func=AF.Copy, scale=-NEG),
                             reads=[pt], writes=[(PENT, (g, q4))])
                    for hg in range(4):
                        h = g * 4 + hg
                        chunks = [(kc, q0 - 128 * kc) for kc in range(0, 4 * qi + 4)]

                        def extra(kc, dl, h=h, g=g):
                            ex = [(EM[0:NB, kc * 128:(kc + 1) * 128], PENT[0:NB, g, :], [EM, PENT])]
                            if dl <= 128:
                                ex.append((ident_bf[:, :], TABN[:, h, dl + 384:dl + 384 + 512], [ident_bf, TABN]))
                            return ex

                        Ops = attn_chunks(chunks, KSL, VSL, QT, g, hg, extra,
                                          lambda dl, h=h: (b31t[:, h:h + 1] if dl >= 256 else None))
                        finalize(Ops, OC, g, hg, False, LGT, r=1 * 8 + g * 4 + hg)
                k.store(OCSD, OCSD.t[:, :, qs].rearrange("j p t -> p j t"), OC, OC[:], dkey=qi)

        k.barrier()
        with ExitStack() as sD:
            KW = k.sb(sD, "KW", [128, T], BF16)
            VW = k.sb(sD, "VW", [128, NKC, 2, 65], BF16)
            TABN = k.sb(sD, "TABN2", [128, 8, 1024], BF16)
            MW = k.sb(sD, "MW", [128, 1408], BF16)
            SELG = k.sb(sD, "SELG2", [32, 24 * 64], F32)
            k.load(KW, KW[:], ZFM, ZFM.t[7, :, :])
            k.op("pool", lambda e: e.memset(VW[:], 1.0), writes=[VW])
            for g in range(2):
                k.load(VW, VW[:, :, g, 0:64], ZTM, ZTM.t[:, 128 + g * 64:128 + (g + 1) * 64].rearrange("(c p) d -> p c d", p=128), dkey=g)
            k.loadc(TABN, TABN[:], tabN, tabN[:, :, :])
            k.loadc(MW, MW[:], mskW, mskW[:, :])
            k.load(SELG, SELG[:], selg, selg[:, :])
            QTs = [k.sb(sD, "QTw%d" % i, [128, 4, 512], BF16) for i in range(2)]
            LGs = [k.sb(sD, "LGw%d" % i, [32, 512], F32) for i in range(2)]
            OCs = [k.sb(sD, "OCw%d" % i, [128, 4, 512], F32) for i in range(2)]
            ONs = [k.sb(sD, "ONs%d" % i, [128, 4, 512], BF16) for i in range(2)]
            ETs = [k.sb(sD, "ETw%d" % i, [128, 512], BF16) for i in range(4)]
            drow = k.sb(sD, "droww", [128, 512], F32)
            lnrow = k.sb(sD, "lnroww", [128, 512], F32)
            EB = k.sb(sD, "EBw", [64, 512], F32)
            TMPF = k.sb(sD, "TMPFw", [128, 512], F32)
            for qi in range(NQ):
                q0 = qi * 512
                qs = slice(q0, q0 + 512)
                QT, LGT, OC, ON = QTs[qi % 2], LGs[qi % 2], OCs[qi % 2], ONs[qi % 2]
                k.load(QT, QT[:], ZFM, ZFM.t[0:4, :, qs].rearrange("j p t -> p j t"), skey=qi)
                k.load(LGT, LGT[:], LNG, LNG.t[:, qs], skey=qi)
                k.load(OC, OC[:], OCSD, OCSD.t[:, :, qs].rearrange("j p t -> p j t"), skey=qi)
                for g in range(2):
                    for hg in range(4):
                        h = g * 4 + hg
                        chunks = [(kc, q0 - 128 * kc) for kc in range(max(0, 4 * qi - 4), 4 * qi + 4)]

                        def extra(kc, dl, h=h):
                            ex = []
                            if dl <= 128:
                                ex.append((ident_bf[:, :], TABN[:, h, dl + 384:dl + 384 + 512], [ident_bf, TABN]))
                            if dl >= 128:
                                ex.append((ident_bf[:, :], MW[:, dl + 384:dl + 384 + 512], [ident_bf, MW]))
                            return ex

                        Ops = attn_chunks(chunks, KW, VW, QT, g, hg, extra, lambda dl, h=h: (b31t[:, h:h + 1] if dl >= 256 else None))
                        finalize(Ops, OC, g, hg, False, LGT, r=2 * 8 + g * 4 + hg)
                k.op("act", lambda e: e.copy(out=ON[:], in_=OC[:]), reads=[OC], writes=[ON])
                k.store(ONB, ONB.t[:, :, qs].rearrange("j p t -> p j t"), ON, ON[:], dkey=qi)

        k.barrier()
        with ExitStack() as sD:
            KS = k.sb(sD, "KS", [128, T], BF16)
            VS = k.sb(sD, "VS", [128, NKC, 2, 65], BF16)
            TABS = k.sb(sD, "TABS", [128, 8, 1024], BF16)
            MS = k.sb(sD, "MS", [128, 1024], BF16)
            WB = k.sb(sD, "WB", [128, 2, 4, D], BF16)
            WO = k.sb(sD, "WO", [128, 8, D], BF16)
            snk = k.sb(sD, "snk", [128, 8], F32)
            esnk = k.sb(sD, "esnk", [128, 8], F32)
            k.load(KS, KS[:], ZFM, ZFM.t[12, :, :])
            k.op("pool", lambda e: e.memset(VS[:], 1.0), writes=[VS])
            for g in range(2):
                k.load(VS, VS[:, :, g, 0:64], ZTM, ZTM.t[:, 256 + g * 64:256 + (g + 1) * 64].rearrange("(c p) d -> p c d", p=128), dkey=g)
            k.loadc(TABS, TABS[:], tabS, tabS[:, :, :])
            k.loadc(MS, MS[:], mskS, mskS[:, :])
            for b in range(2):
                for hg in range(4):
                    k.loadc(WB, WB[:, b, hg, :], w_brR, w_brR[l, b, :, hg * D:(hg + 1) * D], dkey=(b, hg))
            for kk in range(8):
                k.loadc(WO, WO[:, kk, :], w_out, w_out[l, kk * 128:(kk + 1) * 128, :], dkey=kk)
            k.load(snk, snk[:], sinksB, sinksB[l, :, :])
            k.op("act", lambda e: e.activation(out=esnk[:], in_=snk[:], func=AF.Exp), reads=[snk], writes=[esnk])
            QS = k.sb(sD, "QSs", [128, 4, 512], BF16)
            GM = k.sb(sD, "GM", [128, 16, 512], BF16)
            X = k.sb(sD, "Xc", [128, 8, 512], F32)
            ONb = k.sb(sD, "ONb", [128, 4, 512], BF16)
            OSb = k.sb(sD, "OSb", [128, 4, 512], BF16)
            MG = k.sb(sD, "MG", [128, 8, 512], BF16)
            M1 = k.sb(sD, "M1", [128, 512], F32)
            M2 = k.sb(sD, "M2", [128, 512], F32)
            ETs = [k.sb(sD, "ETs%d" % i, [128, 512], BF16) for i in range(4)]
            drow = k.sb(sD, "drows", [128, 512], F32)
            lnrow = k.sb(sD, "lnrows", [128, 512], F32)
            EB = k.sb(sD, "EBs", [64, 512], F32)
            TMPF = k.sb(sD, "TMPFs", [128, 512], F32)
            for qi in range(NQ):
                q0 = qi * 512
                qs = slice(q0, q0 + 512)
                k.load(QS, QS[:], ZFM, ZFM.t[8:12, :, qs].rearrange("j p t -> p j t"), skey=qi)
                k.load(GM, GM[:], ZFM, ZFM.t[13:29, :, qs].rearrange("j p t -> p j t"), skey=qi)
                k.load(ONb, ONb[:], ONB, ONB.t[:, :, qs].rearrange("j p t -> p j t"), skey=qi)
                k.load(X, X[:], xcur_b, xcur_v[:, :, qs], skey=qi)
                for g in range(2):
                    for hg in range(4):
                        h = g * 4 + hg
                        chunks = [(kc, q0 - 128 * kc) for kc in range(max(0, 4 * qi - 1), 4 * qi + 4)]

                        def extra(kc, dl, h=h):
                            ex = [(ident_bf[:, :], TABS[:, h, dl + 384:dl + 384 + 512], [ident_bf, TABS])]
                            if dl >= -256:
                                ex.append((ident_bf[:, :], MS[:, dl + 384:dl + 384 + 512], [ident_bf, MS]))
                            return ex

                        Ops = attn_chunks(chunks, KS, VS, QS, g, hg, extra, lambda dl: None)
                        finalize(Ops, OSb, g, hg, True, None, r=None, sink_ap=esnk[64:65, h:h + 1])
                for cc in range(8):
                    pa = psum()
                    pb = psum()
                    for hg in range(4):
                        k.op("pe", lambda e, hg=hg, cc=cc, pa=pa: e.matmul(pa[:, :], WB[:, 0, hg, cc * 128:(cc + 1) * 128], ONb[:, hg, :],
                                                                           start=(hg == 0), stop=(hg == 3)), reads=[WB, ONb], writes=[pa])
                    for hg in range(4):
                        k.op("pe", lambda e, hg=hg, cc=cc, pb=pb: e.matmul(pb[:, :], WB[:, 1, hg, cc * 128:(cc + 1) * 128], OSb[:, hg, :],
                                                                           start=(hg == 0), stop=(hg == 3)), reads=[WB, OSb], writes=[pb])
                    k.op("dve", lambda e, cc=cc, pa=pa: e.tensor_tensor(out=M1[:, :], in0=pa[:, :], in1=GM[:, cc, :], op=ALU.mult),
                         reads=[pa, GM], writes=[M1])
                    k.op("dve", lambda e, cc=cc, pb=pb: e.tensor_tensor(out=M2[:, :], in0=pb[:, :], in1=GM[:, 8 + cc, :], op=ALU.mult),
                         reads=[pb, GM], writes=[M2])
                    k.op("pool", lambda e, cc=cc: e.tensor_tensor(out=MG[:, cc, :], in0=M1[:, :], in1=M2[:, :], op=ALU.add),
                         reads=[M1, M2], writes=[(MG, cc)])
                for co in range(8):
                    pm = psum()
                    for cc in range(8):
                        k.op("pe", lambda e, cc=cc, co=co, pm=pm: e.matmul(pm[:, :], WO[:, cc, co * 128:(co + 1) * 128], MG[:, cc, :],
                                                                           start=(cc == 0), stop=(cc == 7)), reads=[WO, (MG, cc)], writes=[pm])
                    k.op("dve", lambda e, co=co, pm=pm: e.scalar_tensor_tensor(out=X[:, co, :], in0=pm[:, :], scalar=mcol(l, 2, co), in1=X[:, co, :],
                                                                                op0=ALU.mult, op1=ALU.add), reads=[pm, modT, (X, co)], writes=[(X, co)])
                k.store(xresA, xresA.t[:, :, qs].rearrange("c p t -> p c t"), X, X[:], dkey=qi)

        k.barrier()
        with ExitStack() as sE:
            WU = [k.sb(sE, "WU%d" % i, [128, 8, 2 * D], BF16) for i in range(2)]
            WDn = [k.sb(sE, "WD%d" % i, [128, 8, D], BF16) for i in range(2)]
            BU = [k.sb(sE, "BU%d" % i, [128, 16], F32) for i in range(2)]
            BD = [k.sb(sE, "BD%d" % i, [1, D], BF16) for i in range(2)]
            WR = k.sb(sE, "WR", [128, 8, NEXP], F32)
            BR = k.sb(sE, "BR", [1, NEXP], F32)
            H2 = k.sb(sE, "H2", [128, 8, 1024], BF16)
            YACC = k.sb(sE, "YACC", [128, 8, D], F32)
            GATES = k.sb(sE, "GATES", [128, 8, NEXP], F32)
            k.load(WR, WR[:], w_router, w_router[l, :, :].rearrange("(k p) e -> p k e", p=128))
            k.load(BR, BR[:], b_router, b_router[l, :, :])
            for gi in range(NG):
                t0 = gi * 1024
                k.barrier()
                with ExitStack() as sP:
                    XT = k.sb(sP, "XTm", [128, 8, 512], F32)
                    HF = XT
                    sq = k.sb(sP, "sqM", [128, 8, 512], BF16)
                    rstd = k.sb(sP, "rstdM", [128, 512], F32)
                    tmp = k.sb(sP, "tmpM", [128, 2, 512], F32)
                    LGm = k.sb(sP, "LGm", [128, NEXP], F32)
                    Pm = k.sb(sP, "Pm", [128, NEXP], F32)
                    Mk = k.sb(sP, "Mk", [128, NEXP], F32)
                    m8 = k.sb(sP, "m8", [128, 8], F32)
                    e4 = k.sb(sP, "e4", [128, 4], F32)
                    nmx = k.sb(sP, "nmx", [128, 1], F32)
                    den = k.sb(sP, "den", [128, 1], F32)
                    for hf in range(2):
                        qs = slice(t0 + hf * 512, t0 + hf * 512 + 512)
                        k.load(XT, XT[:], xresA, xresA.t[:, :, qs].rearrange("c p t -> p c t"), skey=(t0 + hf * 512) // 512)
                        H2half = _HalfView(H2, hf)
                        norm_tile(XT, H2half, lambda c: A2[:, l * 8 + c:l * 8 + c + 1], lambda c: mcol(l, 3, c), sq, rstd, tmp, Hf=HF)
                        for q4 in range(4):
                            sub = hf * 4 + q4
                            pm = psum()
                            for kk in range(8):
                                k.op("pe", lambda e, kk=kk, q4=q4, pm=pm: e.matmul(pm[:, 0:NEXP], HF[:, kk, q4 * 128:(q4 + 1) * 128], WR[:, kk, :],
                                                                                   start=(kk == 0), stop=False), reads=[(HF, kk), WR], writes=[pm])
                            k.op("pe", lambda e, pm=pm: e.matmul(pm[:, 0:NEXP], ones_f[0:1, 0:128], BR[0:1, :], start=False, stop=True),
                                 reads=[ones_f, BR], writes=[pm])
                            k.op("dve", lambda e, pm=pm: e.tensor_copy(out=LGm[:], in_=pm[:, 0:NEXP]), reads=[pm], writes=[LGm])
                            k.op("dve", lambda e: e.max(out=m8[:], in_=LGm[:]), reads=[LGm], writes=[m8])
                            k.op("dve", lambda e: e.tensor_scalar(out=nmx[:], in0=m8[:, 0:1], scalar1=-1.0, scalar2=None, op0=ALU.mult), reads=[m8], writes=[nmx])
                            k.op("act", lambda e: e.activation(out=e4[:], in_=m8[:, 0:4], func=AF.Exp, bias=nmx[:, 0:1]), reads=[m8, nmx], writes=[e4])
                            k.op("dve", lambda e: e.reduce_sum(out=den[:], in_=e4[:], axis=mybir.AxisListType.X), reads=[e4], writes=[den])
                            k.op("dve", lambda e: e.reciprocal(out=den[:], in_=den[:]), reads=[den], writes=[den])
                            k.op("act", lambda e: e.activation(out=Pm[:], in_=LGm[:], func=AF.Exp, bias=nmx[:, 0:1]), reads=[LGm, nmx], writes=[Pm])
                            k.op("dve", lambda e: e.tensor_scalar(out=Mk[:], in0=LGm[:], scalar1=m8[:, 3:4], scalar2=None, op0=ALU.is_ge), reads=[LGm, m8], writes=[Mk])
                            k.op("dve", lambda e, sub=sub: e.scalar_tensor_tensor(out=GATES[:, sub, :], in0=Pm[:], scalar=den[:, 0:1], in1=Mk[:],
                                                                                   op0=ALU.mult, op1=ALU.mult), reads=[Pm, den, Mk], writes=[(GATES, sub)])
                k.barrier()
                with ExitStack() as sX:
                    STG = [k.sb(sX, "STG%d" % i, [128, 2 * D], F32) for i in range(2)]
                    G1 = [k.sb(sX, "G1%d" % i, [128, 512], F32) for i in range(2)]
                    S1 = [k.sb(sX, "S1%d" % i, [128, 512], F32) for i in range(2)]
                    L1 = [k.sb(sX, "L1%d" % i, [128, 512], F32) for i in range(2)]
                    HID = k.sb(sX, "HID", [128, 8, 1024], BF16)
                    sti = [0]

                    def emit_load(ex, chunk):
                        st = STG[sti[0] % 2]
                        sti[0] += 1
                        if chunk == 0:
                            k.load(BU[ex % 2], BU[ex % 2][:], b_upR, b_upR[l, ex, :, :])
                            k.loadc(BD[ex % 2], BD[ex % 2][:], b_down, b_down[l, ex, :, :])
                        if chunk < 8:
                            kk = chunk
                            wu = WU[ex % 2]
                            k.load(st, st[:, :], w_upR, w_upR[l, ex, kk * 128:(kk + 1) * 128, :])
                            k.op("act", lambda e: e.copy(out=wu[:, kk, :], in_=st[:, :]), reads=[st], writes=[(wu, kk)])
                        else:
                            kk = chunk - 8
                            wd = WDn[ex % 2]
                            k.load(st, st[:, 0:D], w_down, w_down[l, ex, kk * 128:(kk + 1) * 128, :])
                            k.op("act", lambda e: e.copy(out=wd[:, kk, :], in_=st[:, 0:D]), reads=[st], writes=[(wd, kk)])

                    for ch in range(16):
                        emit_load(0, ch)
                    for ex in range(NEXP):
                        wu, wd, bu, bd = WU[ex % 2], WDn[ex % 2], BU[ex % 2], BD[ex % 2]
                        for hf in range(2):
                            hs = slice(hf * 512, hf * 512 + 512)
                            for jc in range(8):
                                pg = psum()
                                pl = psum()
                                g1, s1, l1 = G1[jc % 2], S1[jc % 2], L1[jc % 2]
                                for kk in range(8):
                                    k.op("pe", lambda e, kk=kk, jc=jc, pg=pg: e.matmul(pg[:, :], wu[:, kk, jc * 128:(jc + 1) * 128], H2[:, kk, hs],
                                                                                       start=(kk == 0), stop=(kk == 7)), reads=[(wu, kk), H2], writes=[pg])
                                for kk in range(8):
                                    k.op("pe", lambda e, kk=kk, jc=jc, pl=pl: e.matmul(pl[:, :], wu[:, kk, D + jc * 128:D + (jc + 1) * 128], H2[:, kk, hs],
                                                                                       start=(kk == 0), stop=(kk == 7)), reads=[(wu, kk), H2], writes=[pl])
                                k.op("dve", lambda e, jc=jc, pg=pg, g1=g1: e.tensor_scalar(out=g1[:], in0=pg[:, :], scalar1=bu[:, jc:jc + 1], scalar2=7.0,
                                                                                           op0=ALU.add, op1=ALU.min), reads=[pg, bu], writes=[g1])
                                k.op("act", lambda e, g1=g1, s1=s1: e.activation(out=s1[:], in_=g1[:], func=AF.Silu, scale=1.702), reads=[g1], writes=[s1])
                                k.op("dve", lambda e, jc=jc, pl=pl, l1=l1: e.tensor_scalar(out=l1[:], in0=pl[:, :], scalar1=bu[:, 8 + jc:9 + jc], scalar2=7.0,
                                                                                           op0=ALU.add, op1=ALU.min), reads=[pl, bu], writes=[l1])
                                k.op("dve", lambda e, l1=l1: e.tensor_scalar(out=l1[:], in0=l1[:], scalar1=-7.0, scalar2=1.0, op0=ALU.max, op1=ALU.add),
                                     reads=[l1], writes=[l1])
                                k.op("dve", lambda e, jc=jc, s1=s1, l1=l1, hs=hs: e.scalar_tensor_tensor(out=HID[:, jc, hs], in0=s1[:], scalar=1.0 / 1.702, in1=l1[:],
                                                                                                          op0=ALU.mult, op1=ALU.mult),
                                     reads=[s1, l1], writes=[(HID, (jc, hf))])
                                if ex + 1 < NEXP:
                                    emit_load(ex + 1, hf * 8 + jc)
                        for hf in range(2):
                            for q4 in range(4):
                                sub = hf * 4 + q4
                                for ch in range(2):
                                    py = psum()
                                    for jc in range(8):
                                        k.op("pe", lambda e, jc=jc, q4=q4, ch=ch, py=py, hf=hf: e.matmul(
                                            py[:, :], HID[:, jc, hf * 512 + q4 * 128:hf * 512 + (q4 + 1) * 128], wd[:, jc, ch * 512:(ch + 1) * 512],
                                            start=(jc == 0), stop=False), reads=[(HID, (jc, hf)), (wd, jc)], writes=[py])
                                    k.op("pe", lambda e, ch=ch, py=py: e.matmul(py[:, :], ones_bf[0:1, 0:128], bd[0:1, ch * 512:(ch + 1) * 512], start=False, stop=True),
                                         reads=[ones_bf, bd], writes=[py])
                                    if ex == 0:
                                        k.op("dve", lambda e, sub=sub, ch=ch, py=py: e.tensor_scalar(out=YACC[:, sub, ch * 512:(ch + 1) * 512], in0=py[:, :],
                                                                                                     scalar1=GATES[:, sub, ex:ex + 1], scalar2=None, op0=ALU.mult),
                                             reads=[py, (GATES, sub)], writes=[(YACC, (sub, ch))])
                                    else:
                                        k.op("dve", lambda e, sub=sub, ch=ch, py=py, ex=ex: e.scalar_tensor_tensor(
                                            out=YACC[:, sub, ch * 512:(ch + 1) * 512], in0=py[:, :], scalar=GATES[:, sub, ex:ex + 1],
                                            in1=YACC[:, sub, ch * 512:(ch + 1) * 512], op0=ALU.mult, op1=ALU.add),
                                            reads=[py, (GATES, sub), (YACC, (sub, ch))], writes=[(YACC, (sub, ch))])
                k.barrier()
                with ExitStack() as sQ:
                    XTs = [k.sb(sQ, "XTe%d" % i, [128, 8, 512], F32) for i in range(2)]
                    for hf in range(2):
                        XT = XTs[hf]
                        qs = slice(t0 + hf * 512, t0 + hf * 512 + 512)
                        k.load(XT, XT[:], xresA, xresA.t[:, :, qs].rearrange("c p t -> p c t"), skey=(t0 + hf * 512) // 512)
                        for cc in range(8):
                            pm = psum()
                            for q4 in range(4):
                                sub = hf * 4 + q4
                                k.op("pe", lambda e, q4=q4, sub=sub, cc=cc, pm=pm: e.transpose(pm[:, q4 * 128:(q4 + 1) * 128], YACC[:, sub, cc * 128:(cc + 1) * 128], ident_f[:, :]),
                                     reads=[YACC, ident_f], writes=[pm])
                            k.op("dve", lambda e, cc=cc, pm=pm, XT=XT: e.scalar_tensor_tensor(out=XT[:, cc, :], in0=pm[:, :], scalar=mcol(l, 5, cc), in1=XT[:, cc, :],
                                                                                        op0=ALU.mult, op1=ALU.add), reads=[pm, modT, (XT, cc)], writes=[(XT, cc)])
                        k.store(xresB, xresB.t[:, :, qs].rearrange("c p t -> p c t"), XT, XT[:], dkey=(t0 + hf * 512) // 512)
        xcur_b = xresB
        xcur_v = xresB.t.rearrange("c p t -> p c t")

    k.barrier()
    with ExitStack() as sF:
        Xs = [k.sb(sF, "Xf%d" % i, [128, 8, 512], F32) for i in range(2)]
        Os = [k.sb(sF, "Of%d" % i, [128, 8, 512], F32) for i in range(2)]
        sq = k.sb(sF, "sqF", [128, 8, 512], BF16)
        rstd = k.sb(sF, "rstdF", [128, 512], F32)
        tmp = k.sb(sF, "tmpF", [128, 2, 512], F32)
        for qi in range(NQ):
            qs = slice(qi * 512, (qi + 1) * 512)
            X, O = Xs[qi % 2], Os[qi % 2]
            k.load(X, X[:], xcur_b, xcur_v[:, :, qs], skey=qi)
            norm_tile(X, O, lambda c: gv[:, 2 * L * 8 + c:2 * L * 8 + c + 1], None, sq, rstd, tmp)
            k.store(outT, outT_v[:, :, qs], O, O[:], dkey=qi)
        k.wait_all("sp", [outT])
    es.close()
    return nc, k


class _HalfView:
    def __init__(self, base, hf):
        self.base = base
        self.hf = hf
        self.name = base.name
        self.regs = base.regs
        self.dsem = None

    def __getitem__(self, idx):
        p, c, t = idx
        assert t == slice(None, None, None)
        return self.base.t[p, c, self.hf * 512:(self.hf + 1) * 512]


def prep_shared(inp, T, L):
    f = np.float32
    NB = T // 64
    NCMP = T // 16 - 1
    NCC = (NCMP + 127) // 128
    sh = {}
    sh["w_ada"] = np.ascontiguousarray(inp["w_ada"][:L], f)
    sh["b_adaT"] = np.ascontiguousarray(inp["b_ada"][:L].reshape(L, 48, 128).transpose(0, 2, 1), f)
    gl = [inp["g_mix"][l] for l in range(L)] + [inp["g_ffn"][l] for l in range(L)] + [inp["g_final"]]
    sh["gvec"] = np.ascontiguousarray(np.concatenate([np.asarray(v, f).reshape(8, 128).T for v in gl], axis=1), f)
    w_in = np.asarray(inp["w_in"][:L], f)
    o_qn, o_kv, o_gn, o_qs, o_kvs, o_gm = 0, 512, 1280, 1304, 1816, 2072
    cols = []
    for hg in range(4):
        for g in range(2):
            h = g * 4 + hg
            cols += list(range(o_qn + h * 64, o_qn + (h + 1) * 64))
    cols += list(range(o_kv + 0, o_kv + 128))
    cols += list(range(o_kv + 128, o_kv + 256))
    cols += list(range(o_kv + 256, o_kv + 384))
    cols += list(range(o_kv + 512, o_kv + 640))
    for hg in range(4):
        for g in range(2):
            h = g * 4 + hg
            cols += list(range(o_qs + h * 64, o_qs + (h + 1) * 64))
    cols += list(range(o_kvs, o_kvs + 128))
    cols += list(range(o_gm, o_gm + 2048))
    cols += list(range(o_gn, o_gn + 24))
    fm = w_in[:, :, cols]
    fm = np.concatenate([fm, np.zeros((L, D, 8), f)], axis=2)
    tmc = list(range(o_kv + 384, o_kv + 512)) + list(range(o_kv + 640, o_kv + 768)) + list(range(o_kvs + 128, o_kvs + 256))
    sh["w_inR"] = np.ascontiguousarray(np.concatenate([fm, w_in[:, :, tmc]], axis=2), f)
    assert sh["w_inR"].shape[2] == FM_COLS + TM_COLS
    sh["posT"] = np.ascontiguousarray(np.asarray(inp["cmp_pos"][:L], f).transpose(0, 1, 3, 2))
    sh["w1R"] = np.ascontiguousarray(np.asarray(inp["cmp_w1"][:L], f).reshape(L, 2, 32, 64, 256).transpose(0, 1, 3, 2, 4).reshape(L, 2, 64, 32 * 256))
    sh["w2"] = np.ascontiguousarray(inp["cmp_w2"][:L], f)
    sh["sinksB"] = np.ascontiguousarray(np.broadcast_to(np.asarray(inp["sinks"][:L], f)[:, None, :], (L, 128, 8)))
    rel = np.asarray(inp["rel_tab"], f)
    sh["tabN"] = np.ascontiguousarray(np.stack([near_table(rel[:, h]) for h in range(8)], axis=1))
    sh["tabS"] = np.ascontiguousarray(np.stack([near_table(rel[:, 8 + h]) for h in range(8)], axis=1))
    sh["mskW"] = win_mask(1408, 512)
    sh["mskS"] = win_mask(1024, 128)
    sh["b31"] = np.ascontiguousarray(np.broadcast_to(rel[31][None, :], (128, 16)))
    wm = np.zeros((NCC * 128, NB + 1), f)
    for j in range(NB):
        for m in range(4):
            for n in range(2):
                i = 4 * j - m - n
                if 0 <= i < NCMP:
                    wm[i, j] += 1.0
    wm[:NCMP, NB] = 1.0
    sh["wmap"] = wm
    ft = np.zeros((128, 2 * NB), f)
    for qp in range(128):
        s = 1 if qp >= 64 else 0
        for y in range(2 * NB):
            x = y - NB
            if x == s or x == s - 1:
                ft[qp, y] = 100.0
            elif x > s:
                ft[qp, y] = -100.0
    sh["ftab"] = ft
    em = np.zeros((NB, T), f)
    for j in range(NB):
        em[j, j * 64:(j + 1) * 64] = 1.0
    sh["emat"] = em
    sg = np.zeros((32, 24 * 64), f)
    for r in range(24):
        sg[r, r * 64:(r + 1) * 64] = 1.0
    sh["selg"] = sg
    sh["identF"] = np.eye(128, dtype=f)
    wb = np.asarray(inp["w_branch"][:L], f).reshape(L, 2, 2, 4, 64, D)
    sh["w_brR"] = np.ascontiguousarray(wb.transpose(0, 1, 2, 4, 3, 5).reshape(L, 2, 128, 4 * D))
    sh["w_out"] = np.ascontiguousarray(inp["w_out"][:L], f)
    sh["w_router"] = np.ascontiguousarray(inp["w_router"][:L], f)
    sh["b_router"] = np.ascontiguousarray(np.asarray(inp["b_router"][:L], f)[:, None, :])
    wu = np.asarray(inp["w_up"][:L], f)
    sh["w_upR"] = np.ascontiguousarray(np.concatenate([wu[..., 0::2], wu[..., 1::2]], axis=-1))
    bu = np.asarray(inp["b_up"][:L], f)
    bu = np.concatenate([bu[..., 0::2], bu[..., 1::2]], axis=-1)
    sh["b_upR"] = np.ascontiguousarray(bu.reshape(L, NEXP, 16, 128).transpose(0, 1, 3, 2))
    sh["w_down"] = np.ascontiguousarray(inp["w_down"][:L], f)
    sh["b_down"] = np.ascontiguousarray(np.asarray(inp["b_down"][:L], f)[:, :, None, :])
    return sh


def run_model(inp, T, L, n_cores=8, dbg=False):
    x = np.asarray(inp["x"], np.float32)
    c = np.asarray(inp["c"], np.float32)
    B = x.shape[0]
    sh = prep_shared(inp, T, L)
    nc, kb = build(T, L, dbg)
    in_maps = []
    for core in range(n_cores):
        b = core % B
        m = dict(sh)
        m["xT"] = np.ascontiguousarray(x[b].T)
        m["cT"] = np.ascontiguousarray(c[b].reshape(8, 128).T)
        in_maps.append(m)
    res = run_bass_kernel_spmd(nc, in_maps, core_ids=list(range(n_cores)))
    out = np.stack([np.ascontiguousarray(res.results[b]["outT"].T) for b in range(B)], axis=0)
    if dbg:
        return out.astype(np.float32), res.results[0]
    return out.astype(np.float32)


def kernel(**inputs):
    return run_model(inputs, 8192, DEPTH, 8)
```
